# Optimizing a Trainium2 kernel written in Bass

```python
import jax
import jax.numpy as jnp
from jax import lax
import numpy as np

D_MODEL = 1024
BATCH = 16
SEQ = 2048
DEPTH = 1

CTX_LEN = 256
GRID_W = 64
MIX_WIDTH = D_MODEL
HGRN_WIDTH = MIX_WIDTH // 2
HGRN_HEAD_DIM = 128
HGRN_HEADS = HGRN_WIDTH // HGRN_HEAD_DIM
CONV_WIDTH = MIX_WIDTH - HGRN_WIDTH
CONV_K = 31
CONV_PAD = CONV_K // 2
CHUNK = 64
IN_COLS = 5 * HGRN_WIDTH + 2 * CONV_WIDTH
N_GROUPS = 4
EXPERTS_PER_GROUP = 8
N_EXPERTS = N_GROUPS * EXPERTS_PER_GROUP
TOP_K = 2
D_EXPERT = D_MODEL // 2
EXPERT_BLOCK = 128
N_MOD = 6
EPS = 1e-6

kernel_name = 'hybrid_hgrn2_conformer_hmoe_dit_layer'


def _rmsnorm(x, g):
    xf = x.astype(jnp.float32)
    y = xf * lax.rsqrt(jnp.mean(xf * xf, axis=-1, keepdims=True) + EPS)
    return (y * g.astype(jnp.float32)).astype(x.dtype)


def _layernorm(x, g, b):
    xf = x.astype(jnp.float32)
    mu = jnp.mean(xf, axis=-1, keepdims=True)
    var = jnp.mean(jnp.square(xf - mu), axis=-1, keepdims=True)
    y = (xf - mu) * lax.rsqrt(var + EPS)
    return (y * g.astype(jnp.float32) + b.astype(jnp.float32)).astype(x.dtype)


def _modulate(h, shift, scale):
    return h * (1 + scale) + shift


def _lower_bound(lb_logits, layer):
    p = jax.nn.softmax(lb_logits.astype(jnp.float32), axis=0)
    return jnp.cumsum(p, axis=0)[layer]


def _heads(t):
    b, l, _ = t.shape
    return t.reshape(b, l, HGRN_HEADS, HGRN_HEAD_DIM).transpose(0, 2, 1, 3)


def _rev(t):
    return jnp.flip(t, axis=2)


def _forget(f_pre, lb):
    lb = lb.reshape(HGRN_HEADS, 1, HGRN_HEAD_DIM)
    f = lb + (1.0 - lb) * jax.nn.sigmoid(f_pre.astype(jnp.float32))
    return jnp.log(f), 1.0 - f


def _gla_chunked(q, k, v, logf, s0):
    b, h, l, dk = q.shape
    dv = v.shape[-1]
    n = l // CHUNK
    q = q.reshape(b, h, n, CHUNK, dk)
    k = k.reshape(b, h, n, CHUNK, dk)
    v = v.reshape(b, h, n, CHUNK, dv)
    cum = jnp.cumsum(logf.reshape(b, h, n, CHUNK, dk), axis=3)
    last = cum[:, :, :, -1:, :]
    q_dec = q * jnp.exp(cum)
    k_intra = k * jnp.exp(-cum)
    k_state = k * jnp.exp(last - cum)
    mask = jnp.tril(jnp.ones((CHUNK, CHUNK), dtype=bool))
    scores = jnp.where(mask, jnp.einsum('bhncd,bhnsd->bhncs', q_dec, k_intra), 0.0)
    o_intra = jnp.einsum('bhncs,bhnsv->bhncv', scores, v)
    kv = jnp.einsum('bhnsd,bhnsv->bhndv', k_state, v)
    decay = jnp.exp(last[:, :, :, 0, :])

    def step(s, inp):
        kv_n, d_n = inp
        return d_n[..., None] * s + kv_n, s

    s_final, s_prev = lax.scan(step, s0, (jnp.moveaxis(kv, 2, 0), jnp.moveaxis(decay, 2, 0)))
    s_prev = jnp.moveaxis(s_prev, 0, 2)
    o_inter = jnp.einsum('bhncd,bhndv->bhncv', q_dec, s_prev)
    return (o_intra + o_inter).reshape(b, h, l, dv), s_final


def _gla_final_state(k, v, logf):
    cum = jnp.cumsum(logf, axis=2)
    w = jnp.exp(cum[:, :, -1:, :] - cum)
    return jnp.einsum('bhld,bhlv->bhdv', k * w, v)


def _hgrn2_mix(z, lb_f, lb_b, s0_f, s0_b, g_norm):
    W = HGRN_WIDTH
    logf_f, k_f = _forget(_heads(z[..., 0:W]), lb_f)
    logf_b, k_b = _forget(_heads(z[..., W:2 * W]), lb_b)
    v = _heads(z[..., 2 * W:3 * W]).astype(jnp.float32)
    q = jax.nn.silu(_heads(z[..., 3 * W:4 * W]).astype(jnp.float32))
    gate = z[..., 4 * W:5 * W].astype(jnp.float32)
    o_f, s_f = _gla_chunked(q, k_f, v, logf_f, s0_f)
    o_b, s_b = _gla_chunked(_rev(q), _rev(k_b), _rev(v), _rev(logf_b), s0_b)
    o = (o_f + _rev(o_b)).transpose(0, 2, 1, 3)
    o = o * lax.rsqrt(jnp.mean(o * o, axis=-1, keepdims=True) + EPS) * g_norm.astype(jnp.float32)
    b, l = gate.shape[:2]
    o = o.reshape(b, l, W) * jax.nn.silu(gate)
    return o.astype(z.dtype), s_f, s_b


def _hgrn2_ctx_states(z, lb_f, lb_b):
    W = HGRN_WIDTH
    logf_f, k_f = _forget(_heads(z[..., 0:W]), lb_f)
    logf_b, k_b = _forget(_heads(z[..., W:2 * W]), lb_b)
    v = _heads(z[..., 2 * W:3 * W]).astype(jnp.float32)
    return _gla_final_state(k_f, v, logf_f), _gla_final_state(_rev(k_b), _rev(v), _rev(logf_b))


def _conv_module(z, dw_k, dw_b, ln_g, ln_b, n_seg, seg_len):
    a, gt = z[..., :CONV_WIDTH], z[..., CONV_WIDTH:]
    u = a * jax.nn.sigmoid(gt)
    b, l, ch = u.shape
    u = u.reshape(b * n_seg, seg_len, ch)
    u = lax.conv_general_dilated(u, dw_k[:, None, :].astype(u.dtype), window_strides=(1,),
                                 padding=[(CONV_PAD, CONV_PAD)],
                                 dimension_numbers=('NWC', 'WIO', 'NWC'), feature_group_count=ch)
    u = (u + dw_b.astype(u.dtype)).reshape(b, l, ch)
    return jax.nn.silu(_layernorm(u, ln_g, ln_b))


def _hier_moe(h, wg, bg, we, be, w_gate, w_up, w_down):
    n_tok, d = h.shape
    hf = h.astype(jnp.float32)
    g_logits = hf @ wg.astype(jnp.float32) + bg.astype(jnp.float32)
    grp = jnp.argmax(g_logits, axis=-1)
    p_grp = jnp.take_along_axis(jax.nn.softmax(g_logits, axis=-1), grp[:, None], axis=-1)
    e_logits = (hf @ we.astype(jnp.float32) + be.astype(jnp.float32)).reshape(n_tok, N_GROUPS, EXPERTS_PER_GROUP)
    e_logits = jnp.take_along_axis(e_logits, grp[:, None, None], axis=1)[:, 0]
    top_v, top_i = lax.top_k(e_logits, TOP_K)
    wts = (jax.nn.softmax(top_v, axis=-1) * p_grp).reshape(-1)
    eid = (grp[:, None] * EXPERTS_PER_GROUP + top_i).reshape(-1).astype(jnp.int32)
    n_asg = n_tok * TOP_K
    tok = jnp.arange(n_asg, dtype=jnp.int32) // TOP_K
    order = jnp.argsort(eid)
    e_s, tok_s, w_s = eid[order], tok[order], wts[order]
    counts = jax.ops.segment_sum(jnp.ones((n_asg,), jnp.int32), eid, num_segments=N_EXPERTS)
    starts = jnp.cumsum(counts) - counts
    pcounts = (counts + EXPERT_BLOCK - 1) // EXPERT_BLOCK * EXPERT_BLOCK
    pends = jnp.cumsum(pcounts)
    pstarts = pends - pcounts
    pos = pstarts[e_s] + jnp.arange(n_asg, dtype=jnp.int32) - starts[e_s]
    n_blk = -(-n_asg // EXPERT_BLOCK) + N_EXPERTS
    xbuf = jnp.zeros((n_blk * EXPERT_BLOCK, d), h.dtype).at[pos].set(h[tok_s])
    blk_e = jnp.minimum(jnp.searchsorted(pends, jnp.arange(n_blk, dtype=jnp.int32) * EXPERT_BLOCK, side='right'),
                        N_EXPERTS - 1)

    def expert_block(args):
        xb, e = args
        hid = jax.nn.silu(xb @ w_gate[e]) * (xb @ w_up[e])
        return hid @ w_down[e]

    ybuf = lax.map(expert_block, (xbuf.reshape(n_blk, EXPERT_BLOCK, d), blk_e)).reshape(-1, d)
    y = ybuf[pos] * w_s[:, None].astype(h.dtype)
    return jax.ops.segment_sum(y, tok_s, num_segments=n_tok)


def setup_inputs(seed: int = 0) -> dict:
    key = jax.random.key(seed)
    ks = jax.random.split(key, 24)
    f32 = jnp.float32
    D, W = D_MODEL, HGRN_WIDTH

    def nrm(k, shape, s):
        return s * jax.random.normal(k, shape, f32)

    return {
        'x': nrm(ks[0], (BATCH, SEQ, D), 1.0),
        'c': nrm(ks[1], (BATCH, D), 1.0),
        'ctx': nrm(ks[2], (BATCH, CTX_LEN, D), 1.0),
        'c_ctx': nrm(ks[3], (D,), 1.0),
        'w_ada': nrm(ks[4], (DEPTH, D, N_MOD * D), 0.5 * D ** -0.5),
        'b_ada': nrm(ks[5], (DEPTH, N_MOD * D), 0.01),
        'norm1_g': 1.0 + nrm(ks[6], (DEPTH, D), 0.05),
        'w_in': nrm(ks[7], (DEPTH, D, IN_COLS), D ** -0.5),
        'lb_logits': nrm(ks[8], (2, DEPTH + 1, W), 0.1),
        'hgrn_norm_g': 1.0 + nrm(ks[9], (DEPTH, HGRN_HEAD_DIM), 0.05),
        'dw_kernel': nrm(ks[10], (DEPTH, CONV_K, CONV_WIDTH), CONV_K ** -0.5),
        'dw_bias': nrm(ks[11], (DEPTH, CONV_WIDTH), 0.01),
        'conv_ln_g': 1.0 + nrm(ks[12], (DEPTH, CONV_WIDTH), 0.05),
        'conv_ln_b': nrm(ks[13], (DEPTH, CONV_WIDTH), 0.01),
        'w_out': nrm(ks[14], (DEPTH, MIX_WIDTH, D), MIX_WIDTH ** -0.5),
        'norm2_g': 1.0 + nrm(ks[15], (DEPTH, D), 0.05),
        'router_group_w': nrm(ks[16], (DEPTH, D, N_GROUPS), D ** -0.5),
        'router_group_b': nrm(ks[17], (DEPTH, N_GROUPS), 0.01),
        'router_expert_w': nrm(ks[18], (DEPTH, D, N_EXPERTS), D ** -0.5),
        'router_expert_b': nrm(ks[19], (DEPTH, N_EXPERTS), 0.01),
        'w_expert_gate': nrm(ks[20], (DEPTH, N_EXPERTS, D, D_EXPERT), D ** -0.5),
        'w_expert_up': nrm(ks[21], (DEPTH, N_EXPERTS, D, D_EXPERT), D ** -0.5),
        'w_expert_down': nrm(ks[22], (DEPTH, N_EXPERTS, D_EXPERT, D), D_EXPERT ** -0.5),
        'final_norm_g': 1.0 + nrm(ks[23], (D,), 0.05),
    }


def reference(x, c, ctx, c_ctx, w_ada, b_ada, norm1_g, w_in, lb_logits, hgrn_norm_g, dw_kernel, dw_bias,
              conv_ln_g, conv_ln_b, w_out, norm2_g, router_group_w, router_group_b, router_expert_w,
              router_expert_b, w_expert_gate, w_expert_up, w_expert_down, final_norm_g):
    W = HGRN_WIDTH
    bsz, n_lat, d = x.shape
    n_ctx = ctx.shape[1]
    rows = n_lat // GRID_W
    for l in range(DEPTH):
        update_ctx = l < DEPTH - 1
        mod = jax.nn.silu(c) @ w_ada[l] + b_ada[l]
        mod_c = jax.nn.silu(c_ctx) @ w_ada[l] + b_ada[l]
        sh1, sc1, g1, sh2, sc2, g2 = jnp.split(mod[:, None, :], N_MOD, axis=-1)
        csh1, csc1, cg1, csh2, csc2, cg2 = jnp.split(mod_c, N_MOD, axis=-1)
        lb_f = _lower_bound(lb_logits[0], l)
        lb_b = _lower_bound(lb_logits[1], l)
        hx = _modulate(_rmsnorm(x, norm1_g[l]), sh1, sc1)
        hc = _modulate(_rmsnorm(ctx, norm1_g[l]), csh1, csc1)
        zx = hx @ w_in[l]
        if update_ctx:
            zc = hc @ w_in[l]
            s0 = jnp.zeros((bsz, HGRN_HEADS, HGRN_HEAD_DIM, HGRN_HEAD_DIM), jnp.float32)
            o_c, s_ctx_f, s_ctx_b = _hgrn2_mix(zc[..., :5 * W], lb_f, lb_b, s0, s0, hgrn_norm_g[l])
            conv_c = _conv_module(zc[..., 5 * W:], dw_kernel[l], dw_bias[l], conv_ln_g[l], conv_ln_b[l], 1, n_ctx)
            ctx_new = ctx + cg1 * (jnp.concatenate([o_c, conv_c], axis=-1) @ w_out[l])
            hc2 = _modulate(_rmsnorm(ctx_new, norm2_g[l]), csh2, csc2)
            ctx_new = ctx_new + cg2 * _hier_moe(hc2.reshape(-1, d), router_group_w[l], router_group_b[l],
                                                router_expert_w[l], router_expert_b[l], w_expert_gate[l],
                                                w_expert_up[l], w_expert_down[l]).reshape(ctx.shape)
        else:
            zc = hc @ w_in[l][:, :3 * W]
            s_ctx_f, s_ctx_b = _hgrn2_ctx_states(zc, lb_f, lb_b)
        o_x, _, _ = _hgrn2_mix(zx[..., :5 * W], lb_f, lb_b, s_ctx_f, s_ctx_b, hgrn_norm_g[l])
        conv_x = _conv_module(zx[..., 5 * W:], dw_kernel[l], dw_bias[l], conv_ln_g[l], conv_ln_b[l], rows, GRID_W)
        x = x + g1 * (jnp.concatenate([o_x, conv_x], axis=-1) @ w_out[l])
        hx2 = _modulate(_rmsnorm(x, norm2_g[l]), sh2, sc2)
        x = x + g2 * _hier_moe(hx2.reshape(-1, d), router_group_w[l], router_group_b[l], router_expert_w[l],
                               router_expert_b[l], w_expert_gate[l], w_expert_up[l],
                               w_expert_down[l]).reshape(x.shape)
        if update_ctx:
            ctx = ctx_new
    return _rmsnorm(x, final_norm_g)
```

```python
import numpy as np
from contextlib import ExitStack
import concourse.bass as bass
import concourse.mybir as mybir
from concourse.bass_utils import run_bass_kernel_spmd

F32 = mybir.dt.float32
BF16 = mybir.dt.bfloat16
I32 = mybir.dt.int32
AF = mybir.ActivationFunctionType
ALU = mybir.AluOpType
AX = mybir.AxisListType

D = 1024
W = 512
NCOL = 3584
NE = 32
EPS = 1e-6
N_CORES = 8


class _Stop(Exception):
    pass


class Sched:
    NDS = 8

    def __init__(self, nc, es):
        self.nc = nc
        self.eng = {'pe': nc.tensor, 'act': nc.scalar, 'dve': nc.vector, 'pool': nc.gpsimd, 'sp': nc.sync}
        self.esem = {k: es.enter_context(nc.semaphore('sem_' + k)) for k in self.eng}
        self.ecnt = {k: 0 for k in self.eng}
        self.dsem = {q: [es.enter_context(nc.semaphore(f'd{q}{i}')) for i in range(self.NDS)] for q in ('sp', 'pool')}
        self.dcnt = {q: [0] * self.NDS for q in ('sp', 'pool')}
        self.drr = {'sp': 0, 'pool': 0}
        self.known = {k: {} for k in self.eng}
        self.lastw = {}
        self.rd = {}
        self.alias = {}
        self.pool_hist = []
        self.pool_depth = 4

    def _wait(self, e, ev):
        key, sem, val, src = ev
        if self.known[e].get(key, 0) >= val:
            return
        self.eng[e].wait_ge(sem, val)
        self.known[e][key] = val

    def _deps(self, e, reads, writes):
        evs = []
        for b in reads:
            if b in self.lastw:
                evs.append(self.lastw[b])
        for b in writes:
            if b in self.lastw:
                evs.append(self.lastw[b])
            evs.extend(self.rd.get(b, {}).values())
        for ev in evs:
            if ev[3] == 'pe' and e == 'pe':
                continue
            self._wait(e, ev)

    def _commit(self, ev, reads, writes):
        for b in reads:
            self.rd.setdefault(b, {})[ev[0]] = ev
        for b in writes:
            self.lastw[b] = ev
            self.rd[b] = {}

    PSUM_NAMES = ('PA', 'PB', 'PC', 'PD', 'PT', 'PS', 'PO')

    def op(self, e, fn, reads=(), writes=()):
        reads = [self.alias.get(b, b) for b in reads]
        writes = [self.alias.get(b, b) for b in writes]
        writes = writes + [b for b in reads if b in self.PSUM_NAMES and b not in writes]
        reads = [b for b in reads if b not in self.PSUM_NAMES]
        self._deps(e, reads, writes)
        ins = fn(self.eng[e])
        self.ecnt[e] += 1
        ins.then_inc(self.esem[e], 1)
        self._commit(('e_' + e, self.esem[e], self.ecnt[e], e), reads, writes)

    def dma(self, q, fn, reads=(), writes=()):
        reads = [self.alias.get(b, b) for b in reads]
        writes = [self.alias.get(b, b) for b in writes]
        i = self.drr[q]
        self.drr[q] = (i + 1) % self.NDS
        sem = self.dsem[q][i]
        key = f'd_{q}{i}'
        if self.dcnt[q][i] > 0:
            self._wait(q, (key, sem, self.dcnt[q][i], 'dma'))
        if q == 'pool' and len(self.pool_hist) >= self.pool_depth:
            self._wait(q, self.pool_hist[-self.pool_depth])
        self._deps(q, reads, writes)
        ins = fn(self.eng[q])
        self.dcnt[q][i] += 16
        ins.then_inc(sem, 16)
        ev = (key, sem, self.dcnt[q][i], 'dma')
        if q == 'pool':
            self.pool_hist.append(ev)
        self._commit(ev, reads, writes)

    def barrier(self, engines=None):
        engines = engines or list(self.eng)
        for e in engines:
            for k in self.eng:
                if k != e and self.ecnt[k] > 0:
                    self._wait(e, ('e_' + k, self.esem[k], self.ecnt[k], k))
            for q in ('sp', 'pool'):
                for i in range(self.NDS):
                    if self.dcnt[q][i] > 0:
                        self._wait(e, (f'd_{q}{i}', self.dsem[q][i], self.dcnt[q][i], 'dma'))


def make_consts():
    s = np.arange(128)[:, None]
    t = np.arange(128)[None, :]
    c = {}
    c['IDF'] = (s == t)
    c['MF'] = ((s >= 64) & (s <= t)) * 1.0 - ((s > t) & (s <= 63)) * 1.0
    c['RF'] = (s > t)
    c['MB'] = ((s >= 64) & (s < t)) * 1.0 - ((s >= t) & (s <= 63)) * 1.0
    c['RB'] = (s < t)
    c['MASKF'] = np.tile((s <= t) * 1.0, (1, 4))
    c['MASKB'] = np.tile((s >= t) * 1.0, (1, 4))
    c['ONES'] = np.ones((128, 128))
    cv = np.zeros((128, 4))
    cv[:, 0] = 1.0
    cv[:64, 1] = 1.0
    cv[64:, 2] = 1.0
    c['CV'] = cv
    c['IOTA'] = np.tile(np.arange(128)[None, :], (128, 1))
    c['PCOL'] = np.tile(np.arange(128)[:, None], (1, 2))
    offs = {}
    cols = []
    o = 0
    for k, v in c.items():
        v = np.asarray(v, np.float32)
        offs[k] = (o, v.shape[1])
        cols.append(v)
        o += v.shape[1]
    return np.concatenate(cols, axis=1).astype(np.float32), offs


CONSTS, COFF = make_consts()
NCONST = CONSTS.shape[1]
SV_DWB, SV_LNG, SV_LNB, SV_DWK, NSV = 0, 4, 8, 12, 12 + 124


def build(NB=2, T=16, CT=2, debug=False, upto=9):
    NT = NB * T
    NBLK = NT * 2 + NE
    nc = bass.Bass("TRN2", target_bir_lowering=False)

    def din(name, shape, dt=F32):
        return nc.dram_tensor(name, shape, dt, kind="ExternalInput").ap()

    x_d = din("x", [NB, T * 128, D])
    ctx_d = din("ctx", [NB, CT * 128, D])
    ct_d = din("cT", [128, 8, 3])
    wada_d = din("w_ada", [D, 6 * D])
    bada_d = din("b_ada", [1, 6 * D])
    win_d = din("w_in", [D, NCOL])
    wout_d = din("w_out", [D, D])
    n1g_d = din("n1g", [1, D])
    n2g_d = din("n2g", [1, D])
    fng_d = din("fng", [1, D])
    lbl_d = din("lbl", [1, 4 * W])
    gn4_d = din("gn4", [1, W])
    sv_d = din("smallv", [128, NSV])
    wr_d = din("wr", [D, 36])
    rb_d = din("rb", [1, 36])
    weg_d = din("weg", [NE, D, W])
    weu_d = din("weu", [NE, D, W])
    wed_d = din("wed", [NE, W, D])
    cst_d = din("consts", [128, NCONST])
    out_d = nc.dram_tensor("out", [NB, T * 128, D], F32, kind="ExternalOutput").ap()
    dk = "ExternalOutput" if debug else "Internal"
    x1_d = nc.dram_tensor("x1d", [NT * 128, D], F32, kind=dk).ap()
    hx2_d = nc.dram_tensor("hx2d", [NT * 128, D], BF16, kind=dk).ap()
    modbc_d = nc.dram_tensor("modbc", [NB, 4, 128, D], F32, kind="Internal").ap()
    xbuf_d = nc.dram_tensor("xbuf", [NBLK * 128, D], BF16, kind="Internal").ap()
    ybuf_d = nc.dram_tensor("ybuf", [NBLK * 128, D], F32, kind=dk).ap()
    if debug:
        dbg_d = nc.dram_tensor("dbg", [128, 4096], F32, kind="ExternalOutput").ap()
        dbgi_d = nc.dram_tensor("dbgi", [128, 1024], I32, kind="ExternalOutput").ap()

    try:
      with ExitStack() as es:
        S = Sched(nc, es)

        dbg_list = []

        def dump(ap, name, off, n):
            if debug:
                S.dma('sp', lambda e: e.dma_start(out=dbg_d[:, off:off + n], in_=ap), reads=[name], writes=['dbg'])

        MARK = es.enter_context(nc.sbuf_tensor("MARK", [128, 128], F32))
        DBGT = es.enter_context(nc.sbuf_tensor("DBGT", [128, 512], F32)) if debug else None

        def dump_bf(ap, name, off, n):
            if debug:
                S.op('dve', lambda e: e.tensor_copy(out=DBGT[:, 0:n], in_=ap), reads=[name], writes=['DBGT'])
                dump(DBGT[:, 0:n], 'DBGT', off, n)

        def chk(n):
            if debug and upto == -n:
                S.op('dve', lambda e: e.memset(MARK[:], float(n)), writes=['MARK'])
                dump(MARK[:], 'MARK', 3968, 128)
                S.barrier()
                raise _Stop()

        def sb(name, shape, dt=F32):
            return es.enter_context(nc.sbuf_tensor(name, shape, dt))

        def ps(name, shape, dt=F32):
            return es.enter_context(nc.psum_tensor(name, shape, dt))

        CST = sb("CST", [128, NCONST])

        def C(k, lo=0, n=None):
            o, w = COFF[k]
            n = w if n is None else n
            return CST[:, o + lo:o + lo + n]

        IDB = sb("IDB", [128, 128], BF16)
        ONEB = sb("ONEB", [128, 128], BF16)
        SV = sb("SV", [128, NSV])
        LBB = sb("LBB", [128, 4, W])
        GNB = sb("GNB", [128, W])
        A1T = sb("A1T", [128, 3, 8])
        S1T = sb("S1T", [128, 3, 8])
        WR = sb("WR", [128, 8, 36])
        RBB = sb("RBB", [128, 36])
        SELS = sb("SELS", [128, NT, 64], BF16)
        WTS = sb("WTS", [128, NT, 2])
        IDX = sb("IDX", [128, NT, 2], I32)
        BEI = sb("BEI", [128, 2 * NBLK], I32)
        IDXW = sb("IDXW", [128, NBLK], I32)
        ROW = sb("ROW", [1, 512])
        PA = ps("PA", [128, 512])
        PB = ps("PB", [128, 512])
        PC = ps("PC", [128, 512])
        PD = ps("PD", [128, 512])
        PT = ps("PT", [128, 1024], BF16)
        PS = ps("PS", [128, 1024])
        PO = ps("PO", [128, 512])

        S.dma('sp', lambda e: e.dma_start(out=CST[:], in_=cst_d), writes=['CST'])
        S.dma('sp', lambda e: e.dma_start(out=SV[:], in_=sv_d), writes=['SV'])
        S.dma('sp', lambda e: e.dma_start(out=WR[:], in_=wr_d.rearrange("(k p) n -> p k n", p=128)), writes=['WR'])
        S.op('dve', lambda e: e.tensor_copy(out=IDB[:], in_=C('IDF')), reads=['CST'], writes=['IDB'])
        S.op('dve', lambda e: e.tensor_copy(out=ONEB[:], in_=C('ONES')), reads=['CST'], writes=['ONEB'])

        def bcast_row(dst_ap, n, src_dram_ap, dstname, post=None):
            for h in range(0, n, 512):
                m = min(512, n - h)
                S.dma('sp', lambda e: e.dma_start(out=ROW[0:1, 0:m], in_=src_dram_ap[:, h:h + m]), writes=['ROW'])
                S.op('pe', lambda e: e.matmul(PC[:, 0:m], lhsT=C('ONES')[0:1, :], rhs=ROW[0:1, 0:m],
                                              start=True, stop=True), reads=['ROW', 'CST'], writes=['PC'])
                S.op('dve', lambda e: e.tensor_copy(out=dst_ap[:, h:h + m], in_=PC[:, 0:m]), reads=['PC'],
                     writes=[dstname])

        def rsqrt_col(dst, src, n, scale, cols=1, name_d=None, name_s=None):
            S.op('act', lambda e: e.activation(out=dst, in_=src, func=AF.Sqrt, scale=scale, bias=EPSB[:, 0:1]),
                 reads=[name_s, 'EPSB'], writes=[name_d])
            S.op('dve', lambda e: e.reciprocal(out=dst, in_=dst), reads=[name_d], writes=[name_d])

        EPSB = sb("EPSB", [128, 1])
        S.op('dve', lambda e: e.memset(EPSB[:], EPS), writes=['EPSB'])
        dump(CST[:, 0:512], 'CST', 0, 512)
        if debug:
            MARK2 = sb("MARK2", [128, 128])
            S.op('dve', lambda e: e.memset(MARK2[:], float(-upto)), writes=['MARK2'])
            dump(MARK2[:], 'MARK2', 3840, 128)
        chk(1)

        import os as _os2
        with ExitStack() as es0:
            def sb0(name, shape, dt=F32):
                return (es if _os2.environ.get('NOSCOPE') else es0).enter_context(nc.sbuf_tensor(name, shape, dt))
            CTt = sb0("CTt", [128, 8, 3])
            LT = sb0("LT", [128, 3, 8, 128], BF16)
            SCt = sb0("SCt", [128, 8, 3])
            WA = [sb0(f"WA{i}", [128, 8, 512], BF16) for i in range(2)]
            BAR = [sb0(f"BAR{i}", [1, 512]) for i in range(2)]
            MT = sb0("MT", [128, 3, D])
            NG = sb0("NG", [128, 2, D])
            LR = sb0("LR", [1, 4 * W])
            LR2 = sb0("LR2", [1, 4 * W])
            TMPB = sb0("TMPB", [128, D])

            S.dma('sp', lambda e: e.dma_start(out=LR[:], in_=lbl_d), writes=['LR'])
            for d in range(2):
                S.op('dve', lambda e: e.tensor_tensor(out=LR2[0:1, d * 2 * W:d * 2 * W + W],
                                                      in0=LR[0:1, d * 2 * W:d * 2 * W + W],
                                                      in1=LR[0:1, d * 2 * W + W:d * 2 * W + 2 * W], op=ALU.subtract),
                     reads=['LR'], writes=['LR2'])
                S.op('act', lambda e: e.activation(out=LR2[0:1, d * 2 * W:d * 2 * W + W],
                                                   in_=LR2[0:1, d * 2 * W:d * 2 * W + W], func=AF.Sigmoid),
                     reads=['LR2'], writes=['LR2'])
                S.op('dve', lambda e: e.tensor_scalar(out=LR2[0:1, d * 2 * W + W:d * 2 * W + 2 * W],
                                                      in0=LR2[0:1, d * 2 * W:d * 2 * W + W], scalar1=-1.0, scalar2=1.0,
                                                      op0=ALU.mult, op1=ALU.add), reads=['LR2'], writes=['LR2'])
            for q in range(4):
                S.op('pe', lambda e: e.matmul(PC[:, :], lhsT=C('ONES')[0:1, :], rhs=LR2[0:1, q * W:(q + 1) * W],
                                              start=True, stop=True), reads=['LR2', 'CST'], writes=['PC'])
                S.op('dve', lambda e: e.tensor_copy(out=LBB[:, q, :], in_=PC[:, :]), reads=['PC'], writes=['LBB'])
            chk(2)
            bcast_row(GNB, W, gn4_d, 'GNB')
            bcast_row(RBB, 36, rb_d, 'RBB')
            bcast_row(NG[:, 0, :], D, n1g_d, 'NG')
            bcast_row(NG[:, 1, :], D, n2g_d, 'NG')

            chk(3)
            S.dma('sp', lambda e: e.dma_start(out=CTt[:], in_=ct_d), writes=['CTt'])
            S.op('act', lambda e: e.activation(out=SCt[:], in_=CTt[:], func=AF.Silu), reads=['CTt'], writes=['SCt'])
            for b in range(3):
                for k in range(8):
                    S.op('dve', lambda e: e.tensor_scalar(out=LT[:, b, k, :], in0=C('ONES'), scalar1=SCt[:, k, b:b + 1],
                                                          scalar2=None, op0=ALU.mult), reads=['SCt', 'CST'],
                         writes=['LT'])

            chk(4)

            def bc_to_cols(dst, src_bc, srcname, dstname):
                for k in range(8):
                    S.op('pe', lambda e: e.transpose(out=PS[:, k * 128:(k + 1) * 128], in_=src_bc[:, k * 128:(k + 1) * 128],
                                                     identity=C('IDF')), reads=[srcname, 'CST'], writes=['PS'])
                S.op('dve', lambda e: e.tensor_copy(out=dst, in_=PS[:, 0:1024:128]), reads=['PS'], writes=[dstname])

            for m in range(6):
                nb_needed = 3 if m < 2 else 2
                for jj in range(2):
                    j = 2 * m + jj
                    wa = WA[j % 2]
                    ba = BAR[j % 2]
                    S.dma('pool', lambda e: e.dma_start(out=wa[:], in_=wada_d[:, j * 512:(j + 1) * 512]
                                                        .rearrange("(k p) n -> p k n", p=128)), writes=[f'WA{j % 2}'])
                    S.dma('sp', lambda e: e.dma_start(out=ba[:], in_=bada_d[:, j * 512:(j + 1) * 512]),
                          writes=[f'BAR{j % 2}'])
                    for b in range(nb_needed):
                        pz = PA if (b % 2 == 0) else PB
                        pzn = 'PA' if (b % 2 == 0) else 'PB'
                        for k in range(8):
                            S.op('pe', lambda e: e.matmul(pz[:, :], lhsT=LT[:, b, k, :], rhs=wa[:, k, :],
                                                          start=(k == 0), stop=False),
                                 reads=['LT', f'WA{j % 2}'], writes=[pzn])
                        S.op('pe', lambda e: e.matmul(pz[:, :], lhsT=C('ONES')[0:1, :], rhs=ba[0:1, :],
                                                      start=False, stop=True), reads=['CST', f'BAR{j % 2}'],
                             writes=[pzn])
                        S.op('act', lambda e: e.activation(out=MT[:, b, jj * 512:(jj + 1) * 512], in_=pz[:, :],
                                                           func=AF.Identity), reads=[pzn], writes=[f'MT{b}'])
                if m == 0:
                    chk(5)
                for b in range(nb_needed):
                    if m == 0:
                        bc_to_cols(S1T[:, b, :], MT[:, b, :], f'MT{b}', 'S1T')
                        chk(6)
                    elif m == 1:
                        S.op('dve', lambda e: e.scalar_tensor_tensor(out=TMPB[:], in0=MT[:, b, :], scalar=1.0,
                                                                     in1=NG[:, 0, :], op0=ALU.add, op1=ALU.mult),
                             reads=[f'MT{b}', 'NG'], writes=['TMPB'])
                        bc_to_cols(A1T[:, b, :], TMPB, 'TMPB', 'A1T')
                    elif m == 4:
                        S.op('dve', lambda e: e.scalar_tensor_tensor(out=TMPB[:], in0=MT[:, b, :], scalar=1.0,
                                                                     in1=NG[:, 1, :], op0=ALU.add, op1=ALU.mult),
                             reads=[f'MT{b}', 'NG'], writes=['TMPB'])
                        S.dma('sp', lambda e: e.dma_start(out=modbc_d[b, 1], in_=TMPB[:]), reads=['TMPB'],
                              writes=[f'modbc{b}.1'])
                    else:
                        slot = {2: 0, 3: 2, 5: 3}[m]
                        S.dma('sp', lambda e: e.dma_start(out=modbc_d[b, slot], in_=MT[:, b, :]), reads=[f'MT{b}'],
                              writes=[f'modbc{b}.{slot}'])
                chk(60 + m)
            chk(67)
            S.barrier()
            chk(66)
        if debug and upto == 0:
            dump(LBB[:, 0, :], 'LBB', 512, 512)
            dump(A1T[:].rearrange("p b k -> p (b k)"), 'A1T', 1024, 24)
            dump(S1T[:].rearrange("p b k -> p (b k)"), 'S1T', 1056, 24)
            dump(GNB[:], 'GNB', 1536, 512)
            S.barrier()
            return nc
        chk(69)
        with ExitStack() as es1:
            chk(68)
            def sb1(name, shape, dt=F32):
                return es1.enter_context(nc.sbuf_tensor(name, shape, dt))
            UP = sb1("UP", [128, 4, 2, 96], BF16)
            X1 = sb1("X1", [128, D])
            RT = sb1("RT", [128, 128])
            WIN = sb1("WIN", [128, 8, NCOL], BF16)
            WOUT = sb1("WOUT", [128, 8, D], BF16)
            import os as _os
            DG = sb1("DG", [128, int(_os.environ.get("DGN", "124")), 128], BF16)
            SBS = sb1("SBS", [128, T, 4, 128], BF16)
            MBC = sb1("MBC", [128, 3, D])
            XT = sb1("XT", [128, D])
            XN = sb1("XN", [128, D], BF16)
            HXT = sb1("HXT", [128, 8, 128], BF16)
            SSc = sb1("SSc", [128, 8])
            T1 = sb1("T1", [128, W])
            T2 = sb1("T2", [128, W])
            T3 = sb1("T3", [128, W])
            LGF = sb1("LGF", [128, W])
            KK = sb1("KK", [128, W])
            SQ = sb1("SQ", [128, W])
            V = sb1("V", [128, W], BF16)
            KST = sb1("KST", [128, W], BF16)
            QK = sb1("QK", [128, 2, 2, W], BF16)
            QKT = sb1("QKT", [128, 2, 2, 4, 128], BF16)
            EV = sb1("EV", [128, 2, 16])
            SST = sb1("SST", [128, 2, W])
            SFS = sb1("SFS", [128, 4, 128], BF16)
            MIX = sb1("MIX", [128, W], BF16)
            U = sb1("U", [128, W], BF16)
            SGG = T2
            PM = QK[:, 0].rearrange("p a (h t) -> p a h t", h=4)
            MIXT = HXT
            CVS = LGF[:].rearrange("p (c t) -> p c t", c=4)
            SQC = T1[:].rearrange("p (c t) -> p c t", c=4)
            ST = T3[:].rearrange("p (c t) -> p c t", c=4)
            HX2 = XT
            HX2B = XN
            HX2T = X1[:].rearrange("p (k t) -> p k t", k=8)
            S.alias.update({'SGG': 'T2', 'PM0': 'QK0', 'PM1': 'QK0', 'MIXTa': 'HXT', 'MIXTb': 'HXT', 'CVS': 'LGF',
                            'SQC': 'T1', 'ST': 'T3', 'HX2': 'XT', 'HX2B': 'XN', 'HX2T': 'X1'})

            chk(70)
            for g in range(7):
                if g == 1:
                    chk(71)
                S.dma('pool', lambda e: e.dma_start(out=WIN[:, :, g * 512:(g + 1) * 512],
                                                    in_=win_d[:, g * 512:(g + 1) * 512]
                                                    .rearrange("(k p) n -> p k n", p=128)), writes=[f'WIN{g}'])
            chk(7)
            for g in range(2):
                S.dma('pool', lambda e: e.dma_start(out=WOUT[:, :, g * 512:(g + 1) * 512],
                                                    in_=wout_d[:, g * 512:(g + 1) * 512]
                                                    .rearrange("(k p) n -> p k n", p=128)), writes=['WOUT'])
            chk(8)
            for i in range(124):
                S.op('dve', lambda e: e.tensor_scalar(out=DG[:, i, :], in0=C('IDF'),
                                                      scalar1=SV[:, SV_DWK + i:SV_DWK + i + 1], scalar2=None,
                                                      op0=ALU.mult), reads=['CST', 'SV'], writes=['DG'])
            chk(9)
            S.op('dve', lambda e: e.memset(UP[:], 0.0), writes=['UP'])

            chk(10)
            GCOL = {'ff': 0, 'fb': 512, 'v': 1024, 'q': 1536, 'g': 2048, 'a': 2560, 'gt': 3072}

            def front(src_ap, mi):
                S.dma('sp', lambda e: e.dma_start(out=XT[:], in_=src_ap), writes=['XT'])
                S.op('act', lambda e: e.activation(out=XN[:], in_=XT[:], func=AF.Square, accum_out=SSc[:, 0:1]),
                     reads=['XT'], writes=['XN', 'SSc'])
                rsqrt_col(SSc[:, 1:2], SSc[:, 0:1], 1, 1.0 / D, name_d='SSc1', name_s='SSc')
                S.op('act', lambda e: e.activation(out=XN[:], in_=XT[:], func=AF.Identity, scale=SSc[:, 1:2]),
                     reads=['XT', 'SSc1'], writes=['XN'])
                for k in range(8):
                    S.op('pe', lambda e: e.transpose(out=PT[:, k * 128:(k + 1) * 128], in_=XN[:, k * 128:(k + 1) * 128],
                                                     identity=IDB[:]), reads=['XN', 'IDB'], writes=['PT'])
                for k in range(8):
                    if k % 2 == 0:
                        S.op('dve', lambda e: e.tensor_scalar(out=HXT[:, k, :], in0=PT[:, k * 128:(k + 1) * 128],
                                                              scalar1=A1T[:, mi, k:k + 1], scalar2=S1T[:, mi, k:k + 1],
                                                              op0=ALU.mult, op1=ALU.add),
                             reads=['PT', 'A1T', 'S1T'], writes=['HXT'])
                    else:
                        S.op('act', lambda e: e.activation(out=HXT[:, k, :], in_=PT[:, k * 128:(k + 1) * 128],
                                                           func=AF.Identity, scale=A1T[:, mi, k:k + 1],
                                                           bias=S1T[:, mi, k:k + 1]),
                             reads=['PT', 'A1T', 'S1T'], writes=['HXT'])

            def zgroup(gname, pz, pzn):
                c0 = GCOL[gname]
                gi = c0 // 512
                for k in range(8):
                    S.op('pe', lambda e: e.matmul(pz[:, :], lhsT=HXT[:, k, :], rhs=WIN[:, k, c0:c0 + 512],
                                                  start=(k == 0), stop=(k == 7)), reads=['HXT', f'WIN{gi}'],
                         writes=[pzn])

            def fprep(d, pz, pzn, full, state):
                S.op('act', lambda e: e.activation(out=T1[:], in_=pz[:, :], func=AF.Sigmoid), reads=[pzn], writes=['T1'])
                S.op('dve', lambda e: e.tensor_tensor(out=T1[:], in0=T1[:], in1=LBB[:, 2 * d + 1, :], op=ALU.mult),
                     reads=['T1', 'LBB'], writes=['T1'])
                S.op('dve', lambda e: e.tensor_tensor(out=T1[:], in0=T1[:], in1=LBB[:, 2 * d, :], op=ALU.add),
                     reads=['T1', 'LBB'], writes=['T1'])
                S.op('act', lambda e: e.activation(out=LGF[:], in_=T1[:], func=AF.Ln), reads=['T1'], writes=['LGF'])
                S.op('dve', lambda e: e.tensor_scalar(out=KK[:], in0=T1[:], scalar1=-1.0, scalar2=1.0, op0=ALU.mult,
                                                      op1=ALU.add), reads=['T1'], writes=['KK'])
                if state:
                    S.op('pe', lambda e: e.matmul(PC[:, :], lhsT=C('RF' if d == 0 else 'RB'), rhs=LGF[:],
                                                  start=True, stop=True), reads=['LGF', 'CST'], writes=['PC'])
                    S.op('act', lambda e: e.activation(out=T2[:], in_=PC[:, :], func=AF.Exp), reads=['PC'],
                         writes=['T2'])
                    S.op('dve', lambda e: e.tensor_tensor(out=KST[:], in0=KK[:], in1=T2[:], op=ALU.mult),
                         reads=['KK', 'T2'], writes=['KST'])
                for h in range(4):
                    S.op('pe', lambda e: e.matmul(PD[:, 4 * h:4 * h + 4], lhsT=LGF[:, h * 128:(h + 1) * 128],
                                                  rhs=C('CV'), start=True, stop=True), reads=['LGF', 'CST'],
                         writes=['PD'])
                S.op('act', lambda e: e.activation(out=EV[:, d, :], in_=PD[:, 0:16], func=AF.Exp), reads=['PD'],
                     writes=[f'EV{d}'])
                if full:
                    S.op('pe', lambda e: e.matmul(PC[:, :], lhsT=C('MF' if d == 0 else 'MB'), rhs=LGF[:],
                                                  start=True, stop=True), reads=['LGF', 'CST'], writes=['PC'])
                    sq, sk = (1.0, -1.0) if d == 0 else (-1.0, 1.0)
                    S.op('act', lambda e: e.activation(out=T2[:], in_=PC[:, :], func=AF.Exp, scale=sq), reads=['PC'],
                         writes=['T2'])
                    S.op('act', lambda e: e.activation(out=T3[:], in_=PC[:, :], func=AF.Exp, scale=sk), reads=['PC'],
                         writes=['T3'])
                    S.op('dve', lambda e: e.tensor_tensor(out=QK[:, d, 0, :], in0=SQ[:], in1=T2[:], op=ALU.mult),
                         reads=['SQ', 'T2'], writes=[f'QK{d}'])
                    S.op('dve', lambda e: e.tensor_tensor(out=QK[:, d, 1, :], in0=KK[:], in1=T3[:], op=ALU.mult),
                         reads=['KK', 'T3'], writes=[f'QK{d}'])

            def state_update(d):
                for h in range(4):
                    S.op('pe', lambda e: e.matmul(PC[:, h * 128:(h + 1) * 128], lhsT=KST[:, h * 128:(h + 1) * 128],
                                                  rhs=V[:, h * 128:(h + 1) * 128], start=True, stop=True),
                         reads=['KST', 'V'], writes=['PC'])
                for h in range(4):
                    S.op('dve', lambda e: e.scalar_tensor_tensor(out=SST[:, d, h * 128:(h + 1) * 128],
                                                                 in0=SST[:, d, h * 128:(h + 1) * 128],
                                                                 scalar=EV[:, d, 4 * h:4 * h + 1],
                                                                 in1=PC[:, h * 128:(h + 1) * 128], op0=ALU.mult,
                                                                 op1=ALU.add),
                         reads=[f'SST{d}', f'EV{d}', 'PC'], writes=[f'SST{d}'])

            def state_tile(src_ap, mi, d):
                front(src_ap, mi)
                zgroup('v', PB, 'PB')
                S.op('act', lambda e: e.activation(out=V[:], in_=PB[:, :], func=AF.Identity), reads=['PB'], writes=['V'])
                zgroup('ff' if d == 0 else 'fb', PA, 'PA')
                fprep(d, PA, 'PA', full=False, state=True)

            def route(t):
                LG, GM, PEN, EL, EL2 = RT[:, 0:36], RT[:, 36:40], RT[:, 40:44], RT[:, 44:76], RT[:, 76:108]
                sc = RT[:, 108:128]
                S.op('dve', lambda e: e.tensor_tensor(out=LG, in0=PO[:, 0:36], in1=RBB[:], op=ALU.add),
                     reads=['PO', 'RBB'], writes=['RT'])
                S.op('dve', lambda e: e.reduce_max(out=sc[:, 0:1], in_=RT[:, 0:4], axis=AX.X), reads=['RT'], writes=['RT'])
                S.op('dve', lambda e: e.tensor_scalar(out=GM, in0=RT[:, 0:4], scalar1=sc[:, 0:1], scalar2=None,
                                                      op0=ALU.is_equal), reads=['RT'], writes=['RT'])
                S.op('dve', lambda e: e.tensor_scalar(out=sc[:, 1:2], in0=sc[:, 0:1], scalar1=-1.0, scalar2=None,
                                                      op0=ALU.mult), reads=['RT'], writes=['RT'])
                S.op('act', lambda e: e.activation(out=sc[:, 4:8], in_=RT[:, 0:4], func=AF.Exp, bias=sc[:, 1:2],
                                                   accum_out=sc[:, 2:3]), reads=['RT'], writes=['RT'])
                S.op('dve', lambda e: e.reciprocal(out=sc[:, 3:4], in_=sc[:, 2:3]), reads=['RT'], writes=['RT'])
                S.op('dve', lambda e: e.tensor_scalar(out=PEN, in0=GM, scalar1=-1.0, scalar2=1e30, op0=ALU.add,
                                                      op1=ALU.mult), reads=['RT'], writes=['RT'])
                for g in range(4):
                    S.op('dve', lambda e: e.tensor_scalar(out=RT[:, 44 + 8 * g:52 + 8 * g], in0=RT[:, 4 + 8 * g:12 + 8 * g],
                                                          scalar1=RT[:, 36 + g:37 + g], scalar2=RT[:, 40 + g:41 + g],
                                                          op0=ALU.mult, op1=ALU.add), reads=['RT'], writes=['RT'])
                S.op('dve', lambda e: e.reduce_max(out=sc[:, 8:9], in_=EL, axis=AX.X), reads=['RT'], writes=['RT'])
                S.op('dve', lambda e: e.tensor_scalar(out=SELS[:, t, 0:32], in0=EL, scalar1=sc[:, 8:9], scalar2=None,
                                                      op0=ALU.is_equal), reads=['RT'], writes=['SELS'])
                S.op('dve', lambda e: e.scalar_tensor_tensor(out=EL2, in0=SELS[:, t, 0:32], scalar=-1e30, in1=EL,
                                                             op0=ALU.mult, op1=ALU.add), reads=['RT', 'SELS'],
                     writes=['RT'])
                S.op('dve', lambda e: e.reduce_max(out=sc[:, 9:10], in_=EL2, axis=AX.X), reads=['RT'], writes=['RT'])
                S.op('dve', lambda e: e.tensor_scalar(out=SELS[:, t, 32:64], in0=EL2, scalar1=sc[:, 9:10], scalar2=None,
                                                      op0=ALU.is_equal), reads=['RT'], writes=['SELS'])
                S.op('dve', lambda e: e.tensor_tensor(out=sc[:, 10:11], in0=sc[:, 9:10], in1=sc[:, 8:9], op=ALU.subtract),
                     reads=['RT'], writes=['RT'])
                S.op('act', lambda e: e.activation(out=sc[:, 11:12], in_=sc[:, 10:11], func=AF.Exp), reads=['RT'],
                     writes=['RT'])
                S.op('dve', lambda e: e.tensor_scalar(out=sc[:, 12:13], in0=sc[:, 11:12], scalar1=1.0, scalar2=None,
                                                      op0=ALU.add), reads=['RT'], writes=['RT'])
                S.op('dve', lambda e: e.reciprocal(out=sc[:, 13:14], in_=sc[:, 12:13]), reads=['RT'], writes=['RT'])
                S.op('dve', lambda e: e.tensor_tensor(out=WTS[:, t, 0:1], in0=sc[:, 13:14], in1=sc[:, 3:4], op=ALU.mult),
                     reads=['RT'], writes=['WTS'])
                S.op('dve', lambda e: e.tensor_tensor(out=WTS[:, t, 1:2], in0=WTS[:, t, 0:1], in1=sc[:, 11:12],
                                                      op=ALU.mult), reads=['RT', 'WTS'], writes=['WTS'])

            pending_route = []

            def full_tile(b, i):
                t = b * T + i
                front(x_d[b, i * 128:(i + 1) * 128, :], b)
                zgroup('q', PA, 'PA')
                zgroup('v', PB, 'PB')
                S.op('act', lambda e: e.activation(out=SQ[:], in_=PA[:, :], func=AF.Silu), reads=['PA'], writes=['SQ'])
                S.op('act', lambda e: e.activation(out=V[:], in_=PB[:, :], func=AF.Identity), reads=['PB'], writes=['V'])
                zgroup('fb', PA, 'PA')
                zgroup('ff', PB, 'PB')
                while pending_route:
                    route(pending_route.pop(0))
                fprep(1, PA, 'PA', full=True, state=False)
                zgroup('g', PA, 'PA')
                fprep(0, PB, 'PB', full=True, state=True)
                zgroup('gt', PB, 'PB')
                S.op('act', lambda e: e.activation(out=T1[:], in_=PA[:, :], func=AF.Silu), reads=['PA'], writes=['T1'])
                S.op('dve', lambda e: e.tensor_tensor(out=SGG[:], in0=T1[:], in1=GNB[:], op=ALU.mult),
                     reads=['T1', 'GNB'], writes=['SGG'])
                zgroup('a', PA, 'PA')
                S.op('act', lambda e: e.activation(out=T1[:], in_=PB[:, :], func=AF.Sigmoid), reads=['PB'], writes=['T1'])
                S.op('dve', lambda e: e.tensor_tensor(out=U[:], in0=PA[:, :], in1=T1[:], op=ALU.mult),
                     reads=['PA', 'T1'], writes=['U'])
                chk(20)
                for d in range(2):
                    for qk in range(2):
                        for h in range(4):
                            S.op('pe', lambda e: e.transpose(out=PT[:, (qk * 4 + h) * 128:(qk * 4 + h + 1) * 128],
                                                             in_=QK[:, d, qk, h * 128:(h + 1) * 128], identity=IDB[:]),
                                 reads=[f'QK{d}', 'IDB'], writes=['PT'])
                    eng = 'act' if d == 0 else 'dve'
                    if eng == 'act':
                        S.op('act', lambda e: e.activation(out=QKT[:, d].rearrange("p a h t -> p (a h t)"), in_=PT[:, :],
                                                           func=AF.Identity), reads=['PT'], writes=[f'QKT{d}'])
                    else:
                        S.op('dve', lambda e: e.tensor_copy(out=QKT[:, d].rearrange("p a h t -> p (a h t)"), in_=PT[:, :]),
                             reads=['PT'], writes=[f'QKT{d}'])
                for d in range(2):
                    for h in range(4):
                        S.op('pe', lambda e: e.matmul(PS[:, (d * 4 + h) * 128:(d * 4 + h + 1) * 128],
                                                      lhsT=QKT[:, d, 1, h, :], rhs=QKT[:, d, 0, h, :], start=True, stop=True),
                             reads=[f'QKT{d}'], writes=['PS'])
                for d in range(2):
                    S.op('dve', lambda e: e.tensor_tensor(out=PM[:, d].rearrange("p h t -> p (h t)"),
                                                          in0=PS[:, d * 512:(d + 1) * 512],
                                                          in1=C('MASKF' if d == 0 else 'MASKB'), op=ALU.mult),
                         reads=['PS', 'CST'], writes=[f'PM{d}'])
                chk(21)
                for h in range(4):
                    S.op('act', lambda e: e.activation(out=SFS[:, h, :], in_=SST[:, 0, h * 128:(h + 1) * 128],
                                                       func=AF.Identity, scale=EV[:, 0, 4 * h + 1:4 * h + 2]),
                         reads=['SST0', 'EV0'], writes=['SFS'])
                for h in range(4):
                    hs = slice(h * 128, (h + 1) * 128)
                    S.op('pe', lambda e: e.matmul(PO[:, hs], lhsT=PM[:, 0, h, :], rhs=V[:, hs], start=True, stop=False),
                         reads=['PM0', 'V'], writes=['PO'])
                    S.op('pe', lambda e: e.matmul(PO[:, hs], lhsT=PM[:, 1, h, :], rhs=V[:, hs], start=False, stop=False),
                         reads=['PM1', 'V'], writes=['PO'])
                    S.op('pe', lambda e: e.matmul(PO[:, hs], lhsT=QKT[:, 0, 0, h, :], rhs=SFS[:, h, :], start=False,
                                                  stop=False), reads=['QKT0', 'SFS'], writes=['PO'])
                    S.op('pe', lambda e: e.matmul(PO[:, hs], lhsT=QKT[:, 1, 0, h, :], rhs=SBS[:, i, h, :], start=False,
                                                  stop=True), reads=['QKT1', 'SBS'], writes=['PO'])
                for h in range(4):
                    S.op('act', lambda e: e.activation(out=T3[:, h * 128:(h + 1) * 128], in_=PO[:, h * 128:(h + 1) * 128],
                                                       func=AF.Square, accum_out=SSc[:, 2 + h:3 + h]),
                         reads=['PO'], writes=['T3', 'SSh'])
                S.op('act', lambda e: e.activation(out=SSc[:, 2:6], in_=SSc[:, 2:6], func=AF.Sqrt, scale=1.0 / 128,
                                                   bias=EPSB[:, 0:1]), reads=['SSh', 'EPSB'], writes=['SSh'])
                S.op('dve', lambda e: e.reciprocal(out=SSc[:, 2:6], in_=SSc[:, 2:6]), reads=['SSh'], writes=['SSh'])
                for h in range(4):
                    hs = slice(h * 128, (h + 1) * 128)
                    S.op('dve', lambda e: e.scalar_tensor_tensor(out=MIX[:, hs], in0=PO[:, hs], scalar=SSc[:, 2 + h:3 + h],
                                                                 in1=SGG[:, hs], op0=ALU.mult, op1=ALU.mult),
                         reads=['PO', 'SSh', 'SGG'], writes=['MIX'])
                state_update(0)
                if t == 0 and upto == -22:
                    dump_bf(MIX[:], 'MIX', 0, 512)
                    dump(PO[:, :] if False else SSc[:, 0:8], 'SSh', 600, 8)
                chk(22)
                for h in range(4):
                    S.op('pe', lambda e: e.transpose(out=PT[:, h * 128:(h + 1) * 128], in_=MIX[:, h * 128:(h + 1) * 128],
                                                     identity=IDB[:]), reads=['MIX', 'IDB'], writes=['PT'])
                for c in range(4):
                    S.op('pe', lambda e: e.transpose(out=PT[:, (4 + c) * 128:(5 + c) * 128], in_=U[:, c * 128:(c + 1) * 128],
                                                     identity=IDB[:]), reads=['U', 'IDB'], writes=['PT'])
                chk(28)
                S.op('act', lambda e: e.activation(out=MIXT[:, 0:4, :].rearrange("p k t -> p (k t)"), in_=PT[:, 0:512],
                                                   func=AF.Identity), reads=['PT'], writes=['MIXTa'])
                chk(29)
                if upto == -31:
                    S.op('dve', lambda e: e.tensor_copy(out=U[:, 0:64], in_=PT[:, 512:576]), reads=['PT'], writes=['U'])
                    chk(31)
                if upto == -33:
                    S.op('dve', lambda e: e.tensor_copy(out=U[:, 0:128], in_=PT[:, 512:640]), reads=['PT'], writes=['U'])
                    chk(33)
                if upto == -34:
                    S.op('dve', lambda e: e.tensor_copy(out=U[:, 0:64], in_=PT[:, 0:64]), reads=['PT'], writes=['U'])
                    chk(34)
                if upto == -35:
                    S.op('act', lambda e: e.activation(out=U[:, 0:64], in_=PT[:, 0:64], func=AF.Identity), reads=['PT'], writes=['U'])
                    chk(35)
                if upto == -36:
                    S.op('dve', lambda e: e.tensor_copy(out=MIX[:, 0:512], in_=PT[:, 0:512]), reads=['PT'], writes=['MIX'])
                    chk(36)
                if upto == -37:
                    S.op('dve', lambda e: e.tensor_copy(out=MIX[:, 0:512], in_=PT[:, 0:512]), reads=['PT', 'MIXTa'], writes=['MIX'])
                    chk(37)
                if upto == -32:
                    S.op('dve', lambda e: e.tensor_copy(out=UP[:, 0, 0, 16:80], in_=MIX[:, 0:64]), reads=['MIX'], writes=['UP'])
                    chk(32)
                for c in range(4):
                    for r in range(2):
                        src = PT[:, (4 + c) * 128 + r * 64:(4 + c) * 128 + (r + 1) * 64]
                        if (c + r) % 2 == 0:
                            S.op('dve', lambda e: e.tensor_copy(out=UP[:, c, r, 16:80], in_=src), reads=['PT'], writes=['UP'])
                        else:
                            S.op('act', lambda e: e.activation(out=UP[:, c, r, 16:80], in_=src, func=AF.Identity),
                                 reads=['PT'], writes=['UP'])
                chk(27)
                for c in range(4):
                    for k in range(31):
                        S.op('pe', lambda e: e.matmul(PD[:, c * 128:(c + 1) * 128].rearrange("p (r t) -> p r t", r=2), lhsT=DG[:, c * 31 + k, :],
                                                      rhs=UP[:, c, :, k + 1:k + 65], start=(k == 0), stop=(k == 30)),
                             reads=['DG', 'UP'], writes=['PD'])
                chk(23)
                for c in range(4):
                    S.op('act', lambda e: e.activation(out=CVS[:, c, :], in_=PD[:, c * 128:(c + 1) * 128], func=AF.Identity,
                                                       bias=SV[:, SV_DWB + c:SV_DWB + c + 1]), reads=['PD', 'SV'],
                         writes=['CVS'])
                    S.op('dve', lambda e: e.tensor_tensor(out=SQC[:, c, :], in0=CVS[:, c, :], in1=CVS[:, c, :], op=ALU.mult),
                         reads=['CVS'], writes=['SQC'])
                for c in range(4):
                    S.op('pe', lambda e: e.matmul(PC[:, 0:128], lhsT=C('ONES'), rhs=CVS[:, c, :], start=(c == 0),
                                                  stop=(c == 3)), reads=['CVS', 'CST'], writes=['PC'])
                for c in range(4):
                    S.op('pe', lambda e: e.matmul(PC[:, 128:256], lhsT=C('ONES'), rhs=SQC[:, c, :], start=(c == 0),
                                                  stop=(c == 3)), reads=['SQC', 'CST'], writes=['PC'])
                S.op('dve', lambda e: e.tensor_scalar(out=ST[:, 0, :], in0=PC[:, 0:128], scalar1=1.0 / W, scalar2=None,
                                                      op0=ALU.mult), reads=['PC'], writes=['ST'])
                S.op('dve', lambda e: e.tensor_tensor(out=ST[:, 1, :], in0=ST[:, 0, :], in1=ST[:, 0, :], op=ALU.mult),
                     reads=['ST'], writes=['ST'])
                S.op('dve', lambda e: e.scalar_tensor_tensor(out=ST[:, 2, :], in0=PC[:, 128:256], scalar=1.0 / W,
                                                             in1=ST[:, 1, :], op0=ALU.mult, op1=ALU.subtract),
                     reads=['PC', 'ST'], writes=['ST'])
                S.op('act', lambda e: e.activation(out=ST[:, 2, :], in_=ST[:, 2, :], func=AF.Sqrt, bias=EPSB[:, 0:1]),
                     reads=['ST', 'EPSB'], writes=['ST'])
                S.op('dve', lambda e: e.reciprocal(out=ST[:, 2, :], in_=ST[:, 2, :]), reads=['ST'], writes=['ST'])
                for c in range(4):
                    S.op('dve', lambda e: e.tensor_tensor(out=CVS[:, c, :], in0=CVS[:, c, :], in1=ST[:, 0, :],
                                                          op=ALU.subtract), reads=['CVS', 'ST'], writes=['CVS'])
                    S.op('dve', lambda e: e.tensor_tensor(out=CVS[:, c, :], in0=CVS[:, c, :], in1=ST[:, 2, :],
                                                          op=ALU.mult), reads=['CVS', 'ST'], writes=['CVS'])
                    S.op('dve', lambda e: e.tensor_scalar(out=CVS[:, c, :], in0=CVS[:, c, :],
                                                          scalar1=SV[:, SV_LNG + c:SV_LNG + c + 1],
                                                          scalar2=SV[:, SV_LNB + c:SV_LNB + c + 1], op0=ALU.mult,
                                                          op1=ALU.add), reads=['CVS', 'SV'], writes=['CVS'])
                    S.op('act', lambda e: e.activation(out=MIXT[:, 4 + c, :], in_=CVS[:, c, :], func=AF.Silu),
                         reads=['CVS'], writes=['MIXTb'])
                chk(24)
                for hf in range(2):
                    for k in range(8):
                        S.op('pe', lambda e: e.matmul(PS[:, hf * 512:(hf + 1) * 512], lhsT=MIXT[:, k, :],
                                                      rhs=WOUT[:, k, hf * 512:(hf + 1) * 512], start=(k == 0), stop=(k == 7)),
                             reads=['MIXTa', 'MIXTb', 'WOUT'], writes=['PS'])
                S.op('dve', lambda e: e.tensor_tensor(out=X1[:], in0=PS[:, :], in1=MBC[:, 0, :], op=ALU.mult),
                     reads=['PS', 'MBC'], writes=['X1'])
                S.op('dve', lambda e: e.tensor_tensor(out=X1[:], in0=X1[:], in1=XT[:], op=ALU.add),
                     reads=['X1', 'XT'], writes=['X1'])
                S.dma('sp', lambda e: e.dma_start(out=x1_d[t * 128:(t + 1) * 128, :], in_=X1[:]), reads=['X1'],
                      writes=[f'x1d{t}'])
                if t == 0 and upto == -25:
                    dump(X1[:, 0:512], 'X1', 0, 512)
                    dump(XT[:, 0:512], 'XT', 512, 512)
                    dump(MBC[:, 0, 0:512], 'MBC', 1024, 512)
                    dump(SQ[:, 0:512], 'SQ', 1536, 512)
                chk(25)
                S.op('act', lambda e: e.activation(out=XN[:], in_=X1[:], func=AF.Square, accum_out=SSc[:, 6:7]),
                     reads=['X1'], writes=['XN', 'SS2'])
                rsqrt_col(SSc[:, 7:8], SSc[:, 6:7], 1, 1.0 / D, name_d='SS2b', name_s='SS2')
                S.op('dve', lambda e: e.scalar_tensor_tensor(out=HX2[:], in0=X1[:], scalar=SSc[:, 7:8], in1=MBC[:, 1, :],
                                                             op0=ALU.mult, op1=ALU.mult), reads=['X1', 'SS2b', 'MBC'],
                     writes=['HX2'])
                S.op('dve', lambda e: e.tensor_tensor(out=HX2[:], in0=HX2[:], in1=MBC[:, 2, :], op=ALU.add),
                     reads=['HX2', 'MBC'], writes=['HX2'])
                S.op('act', lambda e: e.activation(out=HX2B[:], in_=HX2[:], func=AF.Identity), reads=['HX2'],
                     writes=['HX2B'])
                S.dma('sp', lambda e: e.dma_start(out=hx2_d[t * 128:(t + 1) * 128, :], in_=HX2B[:]), reads=['HX2B'],
                      writes=[f'hx2d{t}'])
                chk(26)
                for k in range(8):
                    S.op('pe', lambda e: e.transpose(out=PS[:, k * 128:(k + 1) * 128], in_=HX2[:, k * 128:(k + 1) * 128],
                                                     identity=C('IDF')), reads=['HX2', 'CST'], writes=['PS'])
                S.op('act', lambda e: e.activation(out=HX2T[:].rearrange("p k t -> p (k t)"), in_=PS[:, :],
                                                   func=AF.Identity), reads=['PS'], writes=['HX2T'])
                for k in range(8):
                    S.op('pe', lambda e: e.matmul(PO[:, 0:36], lhsT=HX2T[:, k, :], rhs=WR[:, k, :], start=(k == 0),
                                                  stop=(k == 7)), reads=['HX2T', 'WR'], writes=['PO'])
                pending_route.append(t)

            for b in range(NB):
                for d in range(2):
                    S.op('dve', lambda e: e.memset(SST[:, d, :], 0.0), writes=[f'SST{d}'])
                    order = range(CT) if d == 0 else range(CT - 1, -1, -1)
                    for ci in order:
                        state_tile(ctx_d[b, ci * 128:(ci + 1) * 128, :], 2, d)
                        chk(11)
                        state_update(d)
                        chk(12)
                if b == 0 and upto == -15:
                    dump(SST[:, 0, :], 'SST0', 0, 512)
                    dump(SST[:, 1, :], 'SST1', 512, 512)
                    chk(15)
                for i in range(T - 1, -1, -1):
                    state_tile(x_d[b, i * 128:(i + 1) * 128, :], b, 1)
                    for h in range(4):
                        S.op('act', lambda e: e.activation(out=SBS[:, i, h, :], in_=SST[:, 1, h * 128:(h + 1) * 128],
                                                           func=AF.Identity, scale=EV[:, 1, 4 * h + 2:4 * h + 3]),
                             reads=['SST1', 'EV1'], writes=['SBS'])
                    state_update(1)
                chk(13)
                chk(200 + b)
                for s in range(3):
                    S.dma('sp', lambda e: e.dma_start(out=MBC[:, s, :], in_=modbc_d[b, s]), reads=[f'modbc{b}.{s}'],
                          writes=['MBC'])
                for i in range(T):
                    full_tile(b, i)
                while pending_route:
                    route(pending_route.pop(0))
                    chk(14)
                    chk(100 + b * T + i)
            S.barrier()

        if debug and upto == 1:
            S.dma('sp', lambda e: e.dma_start(out=dbg_d[:, 2048:2048 + NT * 2], in_=WTS[:].rearrange("p t c -> p (t c)")),
                  reads=['WTS'], writes=['dbg'])
            S.barrier(['sp'])
            return nc

        with ExitStack() as es2:
            def sb2(name, shape, dt=F32):
                return es2.enter_context(nc.sbuf_tensor(name, shape, dt))
            SELT = sb2("SELT", [128, NT + 1, NE])
            CUM = sb2("CUM", [128, NT + 1, NE])
            RANK = sb2("RANK", [128, NT, NE])
            CN = sb2("CN", [128, 8, NE])
            CNI = sb2("CNI", [128, NE], I32)
            BE = sb2("BE", [128, 2, NBLK])
            POSF = sb2("POSF", [128, NT, 2])
            TMP = sb2("TMP", [128, NE])
            XB = sb2("XBs", [128, D], BF16)

            S.op('dve', lambda e: e.tensor_tensor(out=SELT[:, 0:NT, :], in0=SELS[:, :, 0:32], in1=SELS[:, :, 32:64],
                                                  op=ALU.add), reads=['SELS'], writes=['SELT'])
            S.op('dve', lambda e: e.memset(CUM[:, 0, :], 0.0), writes=['CUM'])
            for t in range(NT):
                S.op('dve', lambda e: e.tensor_tensor(out=CUM[:, t + 1, :], in0=CUM[:, t, :], in1=SELT[:, t, :], op=ALU.add),
                     reads=['CUM', 'SELT'], writes=['CUM'])
            for t in range(NT):
                S.op('pe', lambda e: e.matmul(PA[:, 0:NE], lhsT=C('RB'), rhs=SELT[:, t, :], start=True, stop=False),
                     reads=['SELT', 'CST'], writes=['PA'])
                S.op('pe', lambda e: e.matmul(PA[:, 0:NE], lhsT=C('ONES'), rhs=CUM[:, t, :], start=False, stop=True),
                     reads=['CUM', 'CST'], writes=['PA'])
                S.op('dve', lambda e: e.tensor_copy(out=RANK[:, t, :], in_=PA[:, 0:NE]), reads=['PA'], writes=['RANK'])
            S.op('pe', lambda e: e.matmul(PA[:, 0:NE], lhsT=C('ONES'), rhs=CUM[:, NT, :], start=True, stop=True),
                 reads=['CUM', 'CST'], writes=['PA'])
            S.op('dve', lambda e: e.tensor_scalar(out=CN[:, 0, :], in0=PA[:, 0:NE], scalar1=127.0, scalar2=None, op0=ALU.add),
                 reads=['PA'], writes=['CN'])
            S.op('dve', lambda e: e.tensor_copy(out=CNI[:], in_=CN[:, 0, :]), reads=['CN'], writes=['CNI'])
            S.op('dve', lambda e: e.tensor_single_scalar(out=CNI[:], in_=CNI[:], scalar=7, op=ALU.arith_shift_right),
                 reads=['CNI'], writes=['CNI'])
            S.op('dve', lambda e: e.tensor_copy(out=CN[:, 1, :], in_=CNI[:]), reads=['CNI'], writes=['CN'])
            S.op('dve', lambda e: e.tensor_copy(out=CN[:, 2, :], in_=CN[:, 1, :]), reads=['CN'], writes=['CN'])
            cur = 2
            for sh in (1, 2, 4, 8, 16):
                nxt = 5 - cur
                S.op('dve', lambda e: e.tensor_copy(out=CN[:, nxt, :], in_=CN[:, cur, :]), reads=['CN'], writes=['CN'])
                S.op('dve', lambda e: e.tensor_tensor(out=CN[:, nxt, sh:NE], in0=CN[:, cur, sh:NE], in1=CN[:, cur, 0:NE - sh],
                                                      op=ALU.add), reads=['CN'], writes=['CN'])
                cur = nxt
            PEND = CN[:, cur, :]
            S.op('dve', lambda e: e.tensor_tensor(out=CN[:, 4, :], in0=PEND, in1=CN[:, 1, :], op=ALU.subtract),
                 reads=['CN'], writes=['CN'])
            S.op('dve', lambda e: e.tensor_scalar(out=CN[:, 4, :], in0=CN[:, 4, :], scalar1=128.0, scalar2=None, op0=ALU.mult),
                 reads=['CN'], writes=['CN'])
            for t in range(NT):
                S.op('dve', lambda e: e.tensor_tensor(out=RANK[:, t, :], in0=RANK[:, t, :], in1=CN[:, 4, :], op=ALU.add),
                     reads=['RANK', 'CN'], writes=['RANK'])
                for j in range(2):
                    S.op('dve', lambda e: e.tensor_tensor(out=TMP[:], in0=RANK[:, t, :], in1=SELS[:, t, 32 * j:32 * j + 32],
                                                          op=ALU.mult), reads=['RANK', 'SELS'], writes=['TMP'])
                    S.op('dve', lambda e: e.reduce_sum(out=POSF[:, t, j:j + 1], in_=TMP[:], axis=AX.X), reads=['TMP'],
                         writes=['POSF'])
            S.op('dve', lambda e: e.tensor_copy(out=IDX[:].rearrange("p t c -> p (t c)"),
                                                in_=POSF[:].rearrange("p t c -> p (t c)")), reads=['POSF'], writes=['IDX'])
            S.op('dve', lambda e: e.memset(BE[:, 0, :], 0.0), writes=['BE'])
            for ex in range(NE):
                S.op('dve', lambda e: e.scalar_tensor_tensor(out=BE[:, 0, :], in0=C('IOTA', 0, NBLK),
                                                             scalar=CN[:, cur, ex:ex + 1], in1=BE[:, 0, :],
                                                             op0=ALU.is_ge, op1=ALU.add), reads=['CST', 'CN', 'BE'],
                     writes=['BE'])
            S.op('dve', lambda e: e.tensor_scalar(out=BE[:, 0, :], in0=BE[:, 0, :], scalar1=float(NE - 1), scalar2=None,
                                                  op0=ALU.min), reads=['BE'], writes=['BE'])
            S.op('dve', lambda e: e.memset(BE[:, 1, 0:1], 1.0), reads=[], writes=['BE1'])
            S.op('dve', lambda e: e.tensor_tensor(out=BE[:, 1, 1:NBLK], in0=BE[:, 0, 1:NBLK], in1=BE[:, 0, 0:NBLK - 1],
                                                  op=ALU.not_equal), reads=['BE', 'BE1'], writes=['BE1'])
            S.op('dve', lambda e: e.tensor_copy(out=BEI[:], in_=BE[:].rearrange("p a n -> p (a n)")), reads=['BE', 'BE1'],
                 writes=['BEI'])
            S.op('dve', lambda e: e.tensor_scalar(out=BE[:, 0, :], in0=BE[:, 0, :], scalar1=128.0, scalar2=C('PCOL', 0, 1),
                                                  op0=ALU.mult, op1=ALU.add), reads=['BE', 'CST', 'BEI'], writes=['BE'])
            S.op('dve', lambda e: e.tensor_scalar(out=BE[:, 0, :], in0=BE[:, 0, :], scalar1=-1.0e6, scalar2=None, op0=ALU.add),
                 reads=['BE'], writes=['BE'])
            S.op('dve', lambda e: e.tensor_tensor(out=BE[:, 0, :], in0=BE[:, 0, :], in1=BE[:, 1, :], op=ALU.mult),
                 reads=['BE', 'BE1'], writes=['BE'])
            S.op('dve', lambda e: e.tensor_scalar(out=BE[:, 0, :], in0=BE[:, 0, :], scalar1=1.0e6, scalar2=None, op0=ALU.add),
                 reads=['BE'], writes=['BE'])
            S.op('dve', lambda e: e.tensor_copy(out=IDXW[:], in_=BE[:, 0, :]), reads=['BE'], writes=['IDXW'])
            for t in range(NT):
                S.dma('sp', lambda e: e.dma_start(out=XB[:], in_=hx2_d[t * 128:(t + 1) * 128, :]), reads=[f'hx2d{t}'],
                      writes=['XBs'])
                for j in range(2):
                    S.dma('pool', lambda e: e.indirect_dma_start(out=xbuf_d, out_offset=bass.IndirectOffsetOnAxis(IDX[:, t, j:j + 1], 0),
                                                                 in_=XB[:], in_offset=None), reads=['XBs', 'IDX'],
                          writes=['xbuf'])
            S.barrier()

        if debug and upto == 2:
            S.dma('sp', lambda e: e.dma_start(out=dbgi_d[:, 0:NT * 2], in_=IDX[:].rearrange("p t c -> p (t c)")),
                  reads=['IDX'], writes=['dbgi'])
            S.dma('sp', lambda e: e.dma_start(out=dbgi_d[:, 256:256 + 2 * NBLK], in_=BEI[:]), reads=['BEI'], writes=['dbgi'])
            S.dma('sp', lambda e: e.dma_start(out=dbgi_d[:, 512:512 + NBLK], in_=IDXW[:]), reads=['IDXW'], writes=['dbgi'])
            S.barrier(['sp'])
            return nc

        with ExitStack() as es3:
            def sb3(name, shape, dt=F32):
                return es3.enter_context(nc.sbuf_tensor(name, shape, dt))
            WG = sb3("WG", [128, 8, W], BF16)
            WU = sb3("WU", [128, 8, W], BF16)
            WD = sb3("WD", [128, 4, D], BF16)
            XBK = [sb3(f"XBK{i}", [128, D], BF16) for i in range(2)]
            XBT = [sb3(f"XBT{i}", [128, 8, 128], BF16) for i in range(2)]
            HS = sb3("HS", [128, W])
            HT = sb3("HT", [128, 4, 128], BF16)
            YB = [sb3(f"YB{i}", [128, D]) for i in range(2)]
            weg_v = weg_d.rearrange("e (p k) n -> (e p) (k n)", k=8)
            weu_v = weu_d.rearrange("e (p k) n -> (e p) (k n)", k=8)
            wed_v = wed_d.rearrange("e (p k) n -> (e p) (k n)", k=4)
            bc_reg = nc.gpsimd.alloc_register("bc_reg")
            nc.gpsimd.reg_mov(bc_reg, NE * 128 - 1)
            def xload(j):
                p = j % 2
                S.dma('sp', lambda e: e.dma_start(out=XBK[p][:], in_=xbuf_d[j * 128:(j + 1) * 128, :]), reads=['xbuf'],
                      writes=[f'XBK{p}'])

            def transp(j):
                p = j % 2
                for k in range(8):
                    S.op('pe', lambda e: e.transpose(out=PT[:, k * 128:(k + 1) * 128], in_=XBK[p][:, k:D:8],
                                                     identity=IDB[:]), reads=[f'XBK{p}', 'IDB'], writes=['PT'])
                S.op('dve', lambda e: e.tensor_copy(out=XBT[p][:].rearrange("p k t -> p (k t)"), in_=PT[:, :]),
                     reads=['PT'], writes=[f'XBT{p}'])

            xload(0)
            transp(0)
            for j in range(NBLK):
                p = j % 2
                for (wt, wv, wn) in ((WG, weg_v, 'WG'), (WU, weu_v, 'WU'), (WD, wed_v, 'WD')):
                    S.dma('pool', lambda e: e.indirect_dma_start(out=wt[:].rearrange("p k n -> p (k n)"), out_offset=None, in_=wv,
                                                                 in_offset=bass.IndirectOffsetOnAxis(IDXW[:, j:j + 1], 0),
                                                                 bounds_check=bc_reg, oob_is_err=False),
                          reads=['IDXW'], writes=[wn])
                if j + 1 < NBLK:
                    xload(j + 1)
                for f in range(4):
                    for k in range(8):
                        S.op('pe', lambda e: e.matmul(PA[:, f * 128:(f + 1) * 128], lhsT=WG[:, k, f:W:4],
                                                      rhs=XBT[p][:, k, :], start=(k == 0), stop=(k == 7)),
                             reads=['WG', f'XBT{p}'], writes=['PA'])
                for f in range(4):
                    for k in range(8):
                        S.op('pe', lambda e: e.matmul(PB[:, f * 128:(f + 1) * 128], lhsT=WU[:, k, f:W:4],
                                                      rhs=XBT[p][:, k, :], start=(k == 0), stop=(k == 7)),
                             reads=['WU', f'XBT{p}'], writes=['PB'])
                S.op('act', lambda e: e.activation(out=HS[:], in_=PA[:, :], func=AF.Silu), reads=['PA'], writes=['HS'])
                S.op('dve', lambda e: e.tensor_tensor(out=HT[:].rearrange("p f t -> p (f t)"), in0=HS[:], in1=PB[:, :],
                                                      op=ALU.mult), reads=['HS', 'PB'], writes=['HT'])
                if j + 1 < NBLK:
                    transp(j + 1)
                for hf in range(2):
                    for f in range(4):
                        S.op('pe', lambda e: e.matmul(PS[:, hf * 512:(hf + 1) * 512], lhsT=HT[:, f, :],
                                                      rhs=WD[:, f, hf * 512:(hf + 1) * 512], start=(f == 0), stop=(f == 3)),
                             reads=['HT', 'WD'], writes=['PS'])
                S.op('act', lambda e: e.activation(out=YB[p][:], in_=PS[:, :], func=AF.Identity), reads=['PS'],
                     writes=[f'YB{p}'])
                S.dma('sp', lambda e: e.dma_start(out=ybuf_d[j * 128:(j + 1) * 128, :], in_=YB[p][:]), reads=[f'YB{p}'],
                      writes=['ybuf'])
            S.barrier()

        with ExitStack() as es4:
            def sb4(name, shape, dt=F32):
                return es4.enter_context(nc.sbuf_tensor(name, shape, dt))
            FGB = sb4("FGB", [128, D])
            G2B = sb4("G2B", [128, D])
            Y1 = [sb4(f"Y1{i}", [128, D]) for i in range(2)]
            Y2 = [sb4(f"Y2{i}", [128, D]) for i in range(2)]
            XR = [sb4(f"XR{i}", [128, D]) for i in range(2)]
            MO = sb4("MO", [128, D])
            OT = [sb4(f"OT{i}", [128, D]) for i in range(2)]
            JK = sb4("JK", [128, D], BF16)
            SF = sb4("SF", [128, 2])
            bcast_row(FGB, D, fng_d, 'FGB')
            for b in range(NB):
                S.dma('sp', lambda e: e.dma_start(out=G2B[:], in_=modbc_d[b, 3]), reads=[f'modbc{b}.3'], writes=['G2B'])
                for i in range(T):
                    t = b * T + i
                    p = t % 2
                    S.dma('pool', lambda e: e.indirect_dma_start(out=Y1[p][:], out_offset=None, in_=ybuf_d,
                                                                 in_offset=bass.IndirectOffsetOnAxis(IDX[:, t, 0:1], 0)),
                          reads=['ybuf', 'IDX'], writes=[f'Y1{p}'])
                    S.dma('pool', lambda e: e.indirect_dma_start(out=Y2[p][:], out_offset=None, in_=ybuf_d,
                                                                 in_offset=bass.IndirectOffsetOnAxis(IDX[:, t, 1:2], 0)),
                          reads=['ybuf', 'IDX'], writes=[f'Y2{p}'])
                    if t == 0:
                        S.dma('sp', lambda e: e.dma_start(out=XR[0][:], in_=x1_d[0:128, :]), reads=['x1d0'], writes=['XR0'])
                    if t + 1 < NT:
                        S.dma('sp', lambda e: e.dma_start(out=XR[1 - p][:], in_=x1_d[(t + 1) * 128:(t + 2) * 128, :]),
                              reads=[f'x1d{t + 1}'], writes=[f'XR{1 - p}'])
                    S.op('dve', lambda e: e.tensor_scalar(out=MO[:], in0=Y1[p][:], scalar1=WTS[:, t, 0:1], scalar2=None,
                                                          op0=ALU.mult), reads=[f'Y1{p}', 'WTS'], writes=['MO'])
                    S.op('dve', lambda e: e.scalar_tensor_tensor(out=MO[:], in0=Y2[p][:], scalar=WTS[:, t, 1:2], in1=MO[:],
                                                                 op0=ALU.mult, op1=ALU.add), reads=[f'Y2{p}', 'WTS', 'MO'],
                         writes=['MO'])
                    S.op('dve', lambda e: e.tensor_tensor(out=MO[:], in0=MO[:], in1=G2B[:], op=ALU.mult),
                         reads=['MO', 'G2B'], writes=['MO'])
                    S.op('dve', lambda e: e.tensor_tensor(out=MO[:], in0=MO[:], in1=XR[p][:], op=ALU.add),
                         reads=['MO', f'XR{p}'], writes=['MO'])
                    S.op('act', lambda e: e.activation(out=JK[:], in_=MO[:], func=AF.Square, accum_out=SF[:, 0:1]),
                         reads=['MO'], writes=['JK', 'SF'])
                    rsqrt_col(SF[:, 1:2], SF[:, 0:1], 1, 1.0 / D, name_d='SF1', name_s='SF')
                    S.op('dve', lambda e: e.scalar_tensor_tensor(out=OT[p][:], in0=MO[:], scalar=SF[:, 1:2], in1=FGB[:],
                                                                 op0=ALU.mult, op1=ALU.mult), reads=['MO', 'SF1', 'FGB'],
                         writes=[f'OT{p}'])
                    S.dma('sp', lambda e: e.dma_start(out=out_d[b, i * 128:(i + 1) * 128, :], in_=OT[p][:]),
                          reads=[f'OT{p}'], writes=['out'])
            S.barrier()
    except _Stop:
        pass
    return nc


def host_inputs(inputs, NB=2, cores=N_CORES):
    f = lambda a: np.ascontiguousarray(np.asarray(a, dtype=np.float32))
    x, c, ctx = f(inputs['x']), f(inputs['c']), f(inputs['ctx'])
    c_ctx = f(inputs['c_ctx'])
    sv = np.zeros((128, NSV), np.float32)
    sv[:, SV_DWB:SV_DWB + 4] = f(inputs['dw_bias'])[0].reshape(4, 128).T
    sv[:, SV_LNG:SV_LNG + 4] = f(inputs['conv_ln_g'])[0].reshape(4, 128).T
    sv[:, SV_LNB:SV_LNB + 4] = f(inputs['conv_ln_b'])[0].reshape(4, 128).T
    dwk = f(inputs['dw_kernel'])[0]
    sv[:, SV_DWK:SV_DWK + 124] = dwk.reshape(31, 4, 128).transpose(2, 1, 0).reshape(128, 124)
    shared = {
        'w_ada': f(inputs['w_ada'])[0], 'b_ada': f(inputs['b_ada'])[0][None, :],
        'w_in': f(inputs['w_in'])[0], 'w_out': f(inputs['w_out'])[0],
        'n1g': f(inputs['norm1_g'])[0][None, :], 'n2g': f(inputs['norm2_g'])[0][None, :],
        'fng': f(inputs['final_norm_g'])[None, :],
        'lbl': f(inputs['lb_logits'])[:, :2, :].reshape(1, 4 * W),
        'gn4': np.tile(f(inputs['hgrn_norm_g'])[0], 4)[None, :],
        'smallv': sv,
        'wr': np.ascontiguousarray(np.concatenate([f(inputs['router_group_w'])[0], f(inputs['router_expert_w'])[0]], axis=1)),
        'rb': np.concatenate([f(inputs['router_group_b'])[0], f(inputs['router_expert_b'])[0]])[None, :],
        'weg': f(inputs['w_expert_gate'])[0], 'weu': f(inputs['w_expert_up'])[0], 'wed': f(inputs['w_expert_down'])[0],
        'consts': CONSTS,
    }
    maps = []
    for k in range(cores):
        cT = np.zeros((128, 8, 3), np.float32)
        for b in range(NB):
            cT[:, :, b] = c[k * NB + b].reshape(8, 128).T
        cT[:, :, 2] = c_ctx.reshape(8, 128).T
        m = dict(shared)
        m['x'] = np.ascontiguousarray(x[k * NB:(k + 1) * NB])
        m['ctx'] = np.ascontiguousarray(ctx[k * NB:(k + 1) * NB])
        m['cT'] = cT
        maps.append(m)
    return maps


def kernel(**inputs):
    nc = build()
    maps = host_inputs(inputs)
    res = run_bass_kernel_spmd(nc, maps, core_ids=list(range(N_CORES)))
    return np.concatenate([np.asarray(r['out'], dtype=np.float32) for r in res.results], axis=0)
```

```python
import numpy as np
from contextlib import ExitStack
import concourse.bass as bass
import concourse.mybir as mybir
from concourse.bass_utils import run_bass_kernel_spmd

F32 = mybir.dt.float32
BF16 = mybir.dt.bfloat16
I32 = mybir.dt.int32
AF = mybir.ActivationFunctionType
ALU = mybir.AluOpType
AX = mybir.AxisListType

D = 1024
W = 512
NCOL = 3584
NE = 32
EPS = 1e-6
N_CORES = 8


class _Stop(Exception):
    pass


class Sched:
    NDS = 8

    def __init__(self, nc, es):
        self.nc = nc
        self.eng = {'pe': nc.tensor, 'act': nc.scalar, 'dve': nc.vector, 'pool': nc.gpsimd, 'sp': nc.sync}
        self.esem = {k: es.enter_context(nc.semaphore('sem_' + k)) for k in self.eng}
        self.ecnt = {k: 0 for k in self.eng}
        self.dsem = {q: [es.enter_context(nc.semaphore(f'd{q}{i}')) for i in range(self.NDS)] for q in ('sp', 'pool')}
        self.dcnt = {q: [0] * self.NDS for q in ('sp', 'pool')}
        self.drr = {'sp': 0, 'pool': 0}
        self.known = {k: {} for k in self.eng}
        self.lastw = {}
        self.rd = {}
        self.alias = {}
        self.pool_hist = []
        self.pool_depth = 2

    def _wait(self, e, ev):
        key, sem, val, src = ev
        if self.known[e].get(key, 0) >= val:
            return
        self.eng[e].wait_ge(sem, val)
        self.known[e][key] = val

    def _deps(self, e, reads, writes):
        evs = []
        for b in reads:
            if b in self.lastw:
                evs.append(self.lastw[b])
        for b in writes:
            if b in self.lastw:
                evs.append(self.lastw[b])
            evs.extend(self.rd.get(b, {}).values())
        for ev in evs:
            if ev[3] == 'pe' and e == 'pe':
                continue
            self._wait(e, ev)

    def _commit(self, ev, reads, writes):
        for b in reads:
            self.rd.setdefault(b, {})[ev[0]] = ev
        for b in writes:
            self.lastw[b] = ev
            self.rd[b] = {}

    PSUM_NAMES = ('PA', 'PB', 'PC', 'PD', 'PT', 'PS', 'PO')

    def op(self, e, fn, reads=(), writes=()):
        reads = [self.alias.get(b, b) for b in reads]
        writes = [self.alias.get(b, b) for b in writes]
        writes = writes + [b for b in reads if b in self.PSUM_NAMES and b not in writes]
        reads = [b for b in reads if b not in self.PSUM_NAMES]
        self._deps(e, reads, writes)
        ins = fn(self.eng[e])
        self.ecnt[e] += 1
        ins.then_inc(self.esem[e], 1)
        self._commit(('e_' + e, self.esem[e], self.ecnt[e], e), reads, writes)

    def dma(self, q, fn, reads=(), writes=()):
        reads = [self.alias.get(b, b) for b in reads]
        writes = [self.alias.get(b, b) for b in writes]
        i = self.drr[q]
        self.drr[q] = (i + 1) % self.NDS
        sem = self.dsem[q][i]
        key = f'd_{q}{i}'
        if self.dcnt[q][i] > 0:
            self._wait(q, (key, sem, self.dcnt[q][i], 'dma'))
        if q == 'pool' and len(self.pool_hist) >= self.pool_depth:
            self._wait(q, self.pool_hist[-self.pool_depth])
        self._deps(q, reads, writes)
        ins = fn(self.eng[q])
        self.dcnt[q][i] += 16
        ins.then_inc(sem, 16)
        ev = (key, sem, self.dcnt[q][i], 'dma')
        if q == 'pool':
            self.pool_hist.append(ev)
        self._commit(ev, reads, writes)

    def barrier(self, engines=None):
        engines = engines or list(self.eng)
        for e in engines:
            for k in self.eng:
                if k != e and self.ecnt[k] > 0:
                    self._wait(e, ('e_' + k, self.esem[k], self.ecnt[k], k))
            for q in ('sp', 'pool'):
                for i in range(self.NDS):
                    if self.dcnt[q][i] > 0:
                        self._wait(e, (f'd_{q}{i}', self.dsem[q][i], self.dcnt[q][i], 'dma'))


def make_consts():
    s = np.arange(128)[:, None]
    t = np.arange(128)[None, :]
    c = {}
    c['IDF'] = (s == t)
    c['MF'] = ((s >= 64) & (s <= t)) * 1.0 - ((s > t) & (s <= 63)) * 1.0
    c['RF'] = (s > t)
    c['MB'] = ((s >= 64) & (s < t)) * 1.0 - ((s >= t) & (s <= 63)) * 1.0
    c['RB'] = (s < t)
    c['MASKF'] = np.tile((s <= t) * 1.0, (1, 4))
    c['MASKB'] = np.tile((s >= t) * 1.0, (1, 4))
    c['ONES'] = np.ones((128, 128))
    cv = np.zeros((128, 4))
    cv[:, 0] = 1.0
    cv[:64, 1] = 1.0
    cv[64:, 2] = 1.0
    c['CV'] = cv
    c['IOTA'] = np.tile(np.arange(128)[None, :], (128, 1))
    c['PCOL'] = np.tile(np.arange(128)[:, None], (1, 2))
    offs = {}
    cols = []
    o = 0
    for k, v in c.items():
        v = np.asarray(v, np.float32)
        offs[k] = (o, v.shape[1])
        cols.append(v)
        o += v.shape[1]
    return np.concatenate(cols, axis=1).astype(np.float32), offs


CONSTS, COFF = make_consts()
NCONST = CONSTS.shape[1]
SV_DWB, SV_LNG, SV_LNB, SV_DWK, NSV = 0, 4, 8, 12, 12 + 124


def build(NB=2, T=16, CT=2, debug=False, upto=9):
    NT = NB * T
    NBLK = NT * 2 + NE
    nc = bass.Bass("TRN2", target_bir_lowering=False)

    def din(name, shape, dt=F32):
        return nc.dram_tensor(name, shape, dt, kind="ExternalInput").ap()

    x_d = din("x", [NB, T * 128, D])
    ctx_d = din("ctx", [NB, CT * 128, D])
    ct_d = din("cT", [128, 8, 3])
    wada_d = din("w_ada", [D, 6 * D])
    bada_d = din("b_ada", [1, 6 * D])
    win_d = din("w_in", [D, NCOL])
    wout_d = din("w_out", [D, D])
    n1g_d = din("n1g", [1, D])
    n2g_d = din("n2g", [1, D])
    fng_d = din("fng", [1, D])
    lbl_d = din("lbl", [1, 4 * W])
    gn4_d = din("gn4", [1, W])
    sv_d = din("smallv", [128, NSV])
    wr_d = din("wr", [D, 36])
    rb_d = din("rb", [1, 36])
    weg_d = din("weg", [NE, D, W])
    weu_d = din("weu", [NE, D, W])
    wed_d = din("wed", [NE, W, D])
    cst_d = din("consts", [128, NCONST])
    out_d = nc.dram_tensor("out", [NB, T * 128, D], F32, kind="ExternalOutput").ap()
    dk = "ExternalOutput" if debug else "Internal"
    x1_d = nc.dram_tensor("x1d", [NT * 128, D], F32, kind=dk).ap()
    hx2_d = nc.dram_tensor("hx2d", [NT * 128, D], BF16, kind=dk).ap()
    modbc_d = nc.dram_tensor("modbc", [NB, 4, 128, D], F32, kind="Internal").ap()
    xbuf_d = nc.dram_tensor("xbuf", [NBLK * 128, D], BF16, kind="Internal").ap()
    ybuf_d = nc.dram_tensor("ybuf", [NBLK * 128, D], F32, kind=dk).ap()
    if debug:
        dbg_d = nc.dram_tensor("dbg", [128, 4096], F32, kind="ExternalOutput").ap()
        dbgi_d = nc.dram_tensor("dbgi", [128, 1024], I32, kind="ExternalOutput").ap()

    try:
      with ExitStack() as es:
        S = Sched(nc, es)

        dbg_list = []

        def dump(ap, name, off, n):
            if debug:
                S.dma('sp', lambda e: e.dma_start(out=dbg_d[:, off:off + n], in_=ap), reads=[name], writes=['dbg'])

        MARK = es.enter_context(nc.sbuf_tensor("MARK", [128, 128], F32))
        DBGT = es.enter_context(nc.sbuf_tensor("DBGT", [128, 512], F32)) if debug else None

        def dump_bf(ap, name, off, n):
            if debug:
                S.op('dve', lambda e: e.tensor_copy(out=DBGT[:, 0:n], in_=ap), reads=[name], writes=['DBGT'])
                dump(DBGT[:, 0:n], 'DBGT', off, n)

        def chk(n):
            if debug and upto == -n:
                S.op('dve', lambda e: e.memset(MARK[:], float(n)), writes=['MARK'])
                dump(MARK[:], 'MARK', 3968, 128)
                S.barrier()
                raise _Stop()

        def sb(name, shape, dt=F32):
            return es.enter_context(nc.sbuf_tensor(name, shape, dt))

        def ps(name, shape, dt=F32):
            return es.enter_context(nc.psum_tensor(name, shape, dt))

        CST = sb("CST", [128, NCONST])

        def C(k, lo=0, n=None):
            o, w = COFF[k]
            n = w if n is None else n
            return CST[:, o + lo:o + lo + n]

        IDB = sb("IDB", [128, 128], BF16)
        ONEB = sb("ONEB", [128, 128], BF16)
        SV = sb("SV", [128, NSV])
        LBB = sb("LBB", [128, 4, W])
        GNB = sb("GNB", [128, W])
        A1T = sb("A1T", [128, 3, 8])
        S1T = sb("S1T", [128, 3, 8])
        WR = sb("WR", [128, 8, 36])
        RBB = sb("RBB", [128, 36])
        SELS = sb("SELS", [128, NT, 64], BF16)
        WTS = sb("WTS", [128, NT, 2])
        IDX = sb("IDX", [128, NT, 2], I32)
        BEI = sb("BEI", [128, 2 * NBLK], I32)
        IDXW = sb("IDXW", [128, NBLK], I32)
        ROW = sb("ROW", [1, 512])
        PA = ps("PA", [128, 512])
        PB = ps("PB", [128, 512])
        PC = ps("PC", [128, 512])
        PD = ps("PD", [128, 512])
        PT = ps("PT", [128, 1024], BF16)
        PS = ps("PS", [128, 1024])
        PO = ps("PO", [128, 512])

        S.dma('sp', lambda e: e.dma_start(out=CST[:], in_=cst_d), writes=['CST'])
        S.dma('sp', lambda e: e.dma_start(out=SV[:], in_=sv_d), writes=['SV'])
        S.dma('sp', lambda e: e.dma_start(out=WR[:], in_=wr_d.rearrange("(k p) n -> p k n", p=128)), writes=['WR'])
        S.op('dve', lambda e: e.tensor_copy(out=IDB[:], in_=C('IDF')), reads=['CST'], writes=['IDB'])
        S.op('dve', lambda e: e.tensor_copy(out=ONEB[:], in_=C('ONES')), reads=['CST'], writes=['ONEB'])

        def bcast_row(dst_ap, n, src_dram_ap, dstname, post=None):
            for h in range(0, n, 512):
                m = min(512, n - h)
                S.dma('sp', lambda e: e.dma_start(out=ROW[0:1, 0:m], in_=src_dram_ap[:, h:h + m]), writes=['ROW'])
                S.op('pe', lambda e: e.matmul(PC[:, 0:m], lhsT=C('ONES')[0:1, :], rhs=ROW[0:1, 0:m],
                                              start=True, stop=True), reads=['ROW', 'CST'], writes=['PC'])
                S.op('dve', lambda e: e.tensor_copy(out=dst_ap[:, h:h + m], in_=PC[:, 0:m]), reads=['PC'],
                     writes=[dstname])

        def rsqrt_col(dst, src, n, scale, cols=1, name_d=None, name_s=None):
            S.op('act', lambda e: e.activation(out=dst, in_=src, func=AF.Sqrt, scale=scale, bias=EPSB[:, 0:1]),
                 reads=[name_s, 'EPSB'], writes=[name_d])
            S.op('dve', lambda e: e.reciprocal(out=dst, in_=dst), reads=[name_d], writes=[name_d])

        EPSB = sb("EPSB", [128, 1])
        S.op('dve', lambda e: e.memset(EPSB[:], EPS), writes=['EPSB'])
        dump(CST[:, 0:512], 'CST', 0, 512)
        if debug:
            MARK2 = sb("MARK2", [128, 128])
            S.op('dve', lambda e: e.memset(MARK2[:], float(-upto)), writes=['MARK2'])
            dump(MARK2[:], 'MARK2', 3840, 128)
        chk(1)

        import os as _os2
        with ExitStack() as es0:
            def sb0(name, shape, dt=F32):
                return (es if _os2.environ.get('NOSCOPE') else es0).enter_context(nc.sbuf_tensor(name, shape, dt))
            CTt = sb0("CTt", [128, 8, 3])
            LT = sb0("LT", [128, 3, 8, 128], BF16)
            SCt = sb0("SCt", [128, 8, 3])
            WA = [sb0(f"WA{i}", [128, 8, 512], BF16) for i in range(2)]
            BAR = [sb0(f"BAR{i}", [1, 512]) for i in range(2)]
            MT = sb0("MT", [128, 3, D])
            NG = sb0("NG", [128, 2, D])
            LR = sb0("LR", [1, 4 * W])
            LR2 = sb0("LR2", [1, 4 * W])
            TMPB = sb0("TMPB", [128, D])

            S.dma('sp', lambda e: e.dma_start(out=LR[:], in_=lbl_d), writes=['LR'])
            for d in range(2):
                S.op('dve', lambda e: e.tensor_tensor(out=LR2[0:1, d * 2 * W:d * 2 * W + W],
                                                      in0=LR[0:1, d * 2 * W:d * 2 * W + W],
                                                      in1=LR[0:1, d * 2 * W + W:d * 2 * W + 2 * W], op=ALU.subtract),
                     reads=['LR'], writes=['LR2'])
                S.op('act', lambda e: e.activation(out=LR2[0:1, d * 2 * W:d * 2 * W + W],
                                                   in_=LR2[0:1, d * 2 * W:d * 2 * W + W], func=AF.Sigmoid),
                     reads=['LR2'], writes=['LR2'])
                S.op('dve', lambda e: e.tensor_scalar(out=LR2[0:1, d * 2 * W + W:d * 2 * W + 2 * W],
                                                      in0=LR2[0:1, d * 2 * W:d * 2 * W + W], scalar1=-1.0, scalar2=1.0,
                                                      op0=ALU.mult, op1=ALU.add), reads=['LR2'], writes=['LR2'])
            for q in range(4):
                S.op('pe', lambda e: e.matmul(PC[:, :], lhsT=C('ONES')[0:1, :], rhs=LR2[0:1, q * W:(q + 1) * W],
                                              start=True, stop=True), reads=['LR2', 'CST'], writes=['PC'])
                S.op('dve', lambda e: e.tensor_copy(out=LBB[:, q, :], in_=PC[:, :]), reads=['PC'], writes=['LBB'])
            chk(2)
            bcast_row(GNB, W, gn4_d, 'GNB')
            bcast_row(RBB, 36, rb_d, 'RBB')
            bcast_row(NG[:, 0, :], D, n1g_d, 'NG')
            bcast_row(NG[:, 1, :], D, n2g_d, 'NG')

            chk(3)
            S.dma('sp', lambda e: e.dma_start(out=CTt[:], in_=ct_d), writes=['CTt'])
            S.op('act', lambda e: e.activation(out=SCt[:], in_=CTt[:], func=AF.Silu), reads=['CTt'], writes=['SCt'])
            for b in range(3):
                for k in range(8):
                    S.op('dve', lambda e: e.tensor_scalar(out=LT[:, b, k, :], in0=C('ONES'), scalar1=SCt[:, k, b:b + 1],
                                                          scalar2=None, op0=ALU.mult), reads=['SCt', 'CST'],
                         writes=['LT'])

            chk(4)

            def bc_to_cols(dst, src_bc, srcname, dstname):
                for k in range(8):
                    S.op('pe', lambda e: e.transpose(out=PS[:, k * 128:(k + 1) * 128], in_=src_bc[:, k * 128:(k + 1) * 128],
                                                     identity=C('IDF')), reads=[srcname, 'CST'], writes=['PS'])
                S.op('dve', lambda e: e.tensor_copy(out=dst, in_=PS[:, 0:1024:128]), reads=['PS'], writes=[dstname])

            for m in range(6):
                nb_needed = 3 if m < 2 else 2
                for jj in range(2):
                    j = 2 * m + jj
                    wa = WA[j % 2]
                    ba = BAR[j % 2]
                    S.dma('pool', lambda e: e.dma_start(out=wa[:], in_=wada_d[:, j * 512:(j + 1) * 512]
                                                        .rearrange("(k p) n -> p k n", p=128)), writes=[f'WA{j % 2}'])
                    S.dma('sp', lambda e: e.dma_start(out=ba[:], in_=bada_d[:, j * 512:(j + 1) * 512]),
                          writes=[f'BAR{j % 2}'])
                    for b in range(nb_needed):
                        pz = PA if (b % 2 == 0) else PB
                        pzn = 'PA' if (b % 2 == 0) else 'PB'
                        for k in range(8):
                            S.op('pe', lambda e: e.matmul(pz[:, :], lhsT=LT[:, b, k, :], rhs=wa[:, k, :],
                                                          start=(k == 0), stop=False),
                                 reads=['LT', f'WA{j % 2}'], writes=[pzn])
                        S.op('pe', lambda e: e.matmul(pz[:, :], lhsT=C('ONES')[0:1, :], rhs=ba[0:1, :],
                                                      start=False, stop=True), reads=['CST', f'BAR{j % 2}'],
                             writes=[pzn])
                        S.op('act', lambda e: e.activation(out=MT[:, b, jj * 512:(jj + 1) * 512], in_=pz[:, :],
                                                           func=AF.Identity), reads=[pzn], writes=[f'MT{b}'])
                if m == 0:
                    chk(5)
                for b in range(nb_needed):
                    if m == 0:
                        bc_to_cols(S1T[:, b, :], MT[:, b, :], f'MT{b}', 'S1T')
                        chk(6)
                    elif m == 1:
                        S.op('dve', lambda e: e.scalar_tensor_tensor(out=TMPB[:], in0=MT[:, b, :], scalar=1.0,
                                                                     in1=NG[:, 0, :], op0=ALU.add, op1=ALU.mult),
                             reads=[f'MT{b}', 'NG'], writes=['TMPB'])
                        bc_to_cols(A1T[:, b, :], TMPB, 'TMPB', 'A1T')
                    elif m == 4:
                        S.op('dve', lambda e: e.scalar_tensor_tensor(out=TMPB[:], in0=MT[:, b, :], scalar=1.0,
                                                                     in1=NG[:, 1, :], op0=ALU.add, op1=ALU.mult),
                             reads=[f'MT{b}', 'NG'], writes=['TMPB'])
                        S.dma('sp', lambda e: e.dma_start(out=modbc_d[b, 1], in_=TMPB[:]), reads=['TMPB'],
                              writes=[f'modbc{b}.1'])
                    else:
                        slot = {2: 0, 3: 2, 5: 3}[m]
                        S.dma('sp', lambda e: e.dma_start(out=modbc_d[b, slot], in_=MT[:, b, :]), reads=[f'MT{b}'],
                              writes=[f'modbc{b}.{slot}'])
                chk(60 + m)
            chk(67)
            S.barrier()
            chk(66)
        if debug and upto == 0:
            dump(LBB[:, 0, :], 'LBB', 512, 512)
            dump(A1T[:].rearrange("p b k -> p (b k)"), 'A1T', 1024, 24)
            dump(S1T[:].rearrange("p b k -> p (b k)"), 'S1T', 1056, 24)
            dump(GNB[:], 'GNB', 1536, 512)
            S.barrier()
            return nc
        chk(69)
        with ExitStack() as es1:
            chk(68)
            def sb1(name, shape, dt=F32):
                return es1.enter_context(nc.sbuf_tensor(name, shape, dt))
            UP = sb1("UP", [128, 4, 2, 96], BF16)
            X1 = sb1("X1", [128, D])
            RT = sb1("RT", [128, 128])
            WIN = sb1("WIN", [128, 8, NCOL], BF16)
            WOUT = sb1("WOUT", [128, 8, D], BF16)
            import os as _os
            DG = sb1("DG", [128, int(_os.environ.get("DGN", "124")), 128], BF16)
            SBS = sb1("SBS", [128, T, 4, 128], BF16)
            MBC = sb1("MBC", [128, 3, D])
            XT = sb1("XT", [128, D])
            XN = sb1("XN", [128, D], BF16)
            HXT = sb1("HXT", [128, 8, 128], BF16)
            SSc = sb1("SSc", [128, 8])
            T1 = sb1("T1", [128, W])
            T2 = sb1("T2", [128, W])
            T3 = sb1("T3", [128, W])
            LGF = sb1("LGF", [128, W])
            KK = sb1("KK", [128, W])
            SQ = sb1("SQ", [128, W])
            V = sb1("V", [128, W], BF16)
            KST = sb1("KST", [128, W], BF16)
            QK = sb1("QK", [128, 2, 2, W], BF16)
            QKT = sb1("QKT", [128, 2, 2, 4, 128], BF16)
            EV = sb1("EV", [128, 2, 16])
            SST = sb1("SST", [128, 2, W])
            SFS = sb1("SFS", [128, 4, 128], BF16)
            MIX = sb1("MIX", [128, W], BF16)
            U = sb1("U", [128, W], BF16)
            SGG = T2
            PM = QK[:, 0].rearrange("p a (h t) -> p a h t", h=4)
            MIXT = HXT
            CVS = LGF[:].rearrange("p (c t) -> p c t", c=4)
            SQC = T1[:].rearrange("p (c t) -> p c t", c=4)
            ST = T3[:].rearrange("p (c t) -> p c t", c=4)
            HX2 = XT
            HX2B = XN
            HX2T = X1[:].rearrange("p (k t) -> p k t", k=8)
            S.alias.update({'SGG': 'T2', 'PM0': 'QK0', 'PM1': 'QK0', 'MIXTa': 'HXT', 'MIXTb': 'HXT', 'CVS': 'LGF',
                            'SQC': 'T1', 'ST': 'T3', 'HX2': 'XT', 'HX2B': 'XN', 'HX2T': 'X1'})

            chk(70)
            for g in range(7):
                if g == 1:
                    chk(71)
                S.dma('pool', lambda e: e.dma_start(out=WIN[:, :, g * 512:(g + 1) * 512],
                                                    in_=win_d[:, g * 512:(g + 1) * 512]
                                                    .rearrange("(k p) n -> p k n", p=128)), writes=[f'WIN{g}'])
            chk(7)
            for g in range(2):
                S.dma('pool', lambda e: e.dma_start(out=WOUT[:, :, g * 512:(g + 1) * 512],
                                                    in_=wout_d[:, g * 512:(g + 1) * 512]
                                                    .rearrange("(k p) n -> p k n", p=128)), writes=['WOUT'])
            chk(8)
            for i in range(124):
                S.op('dve', lambda e: e.tensor_scalar(out=DG[:, i, :], in0=C('IDF'),
                                                      scalar1=SV[:, SV_DWK + i:SV_DWK + i + 1], scalar2=None,
                                                      op0=ALU.mult), reads=['CST', 'SV'], writes=['DG'])
            chk(9)
            S.op('dve', lambda e: e.memset(UP[:], 0.0), writes=['UP'])

            chk(10)
            GCOL = {'ff': 0, 'fb': 512, 'v': 1024, 'q': 1536, 'g': 2048, 'a': 2560, 'gt': 3072}

            def front(src_ap, mi):
                S.dma('sp', lambda e: e.dma_start(out=XT[:], in_=src_ap), writes=['XT'])
                S.op('act', lambda e: e.activation(out=XN[:], in_=XT[:], func=AF.Square, accum_out=SSc[:, 0:1]),
                     reads=['XT'], writes=['XN', 'SSc'])
                rsqrt_col(SSc[:, 1:2], SSc[:, 0:1], 1, 1.0 / D, name_d='SSc1', name_s='SSc')
                S.op('act', lambda e: e.activation(out=XN[:], in_=XT[:], func=AF.Identity, scale=SSc[:, 1:2]),
                     reads=['XT', 'SSc1'], writes=['XN'])
                for k in range(8):
                    S.op('pe', lambda e: e.transpose(out=PT[:, k * 128:(k + 1) * 128], in_=XN[:, k * 128:(k + 1) * 128],
                                                     identity=IDB[:]), reads=['XN', 'IDB'], writes=['PT'])
                for k in range(8):
                    if k % 2 == 0:
                        S.op('dve', lambda e: e.tensor_scalar(out=HXT[:, k, :], in0=PT[:, k * 128:(k + 1) * 128],
                                                              scalar1=A1T[:, mi, k:k + 1], scalar2=S1T[:, mi, k:k + 1],
                                                              op0=ALU.mult, op1=ALU.add),
                             reads=['PT', 'A1T', 'S1T'], writes=['HXT'])
                    else:
                        S.op('act', lambda e: e.activation(out=HXT[:, k, :], in_=PT[:, k * 128:(k + 1) * 128],
                                                           func=AF.Identity, scale=A1T[:, mi, k:k + 1],
                                                           bias=S1T[:, mi, k:k + 1]),
                             reads=['PT', 'A1T', 'S1T'], writes=['HXT'])

            def zgroup(gname, pz, pzn):
                c0 = GCOL[gname]
                gi = c0 // 512
                for k in range(8):
                    S.op('pe', lambda e: e.matmul(pz[:, :], lhsT=HXT[:, k, :], rhs=WIN[:, k, c0:c0 + 512],
                                                  start=(k == 0), stop=(k == 7)), reads=['HXT', f'WIN{gi}'],
                         writes=[pzn])

            def fprep(d, pz, pzn, full, state):
                S.op('act', lambda e: e.activation(out=T1[:], in_=pz[:, :], func=AF.Sigmoid), reads=[pzn], writes=['T1'])
                S.op('dve', lambda e: e.tensor_tensor(out=T1[:], in0=T1[:], in1=LBB[:, 2 * d + 1, :], op=ALU.mult),
                     reads=['T1', 'LBB'], writes=['T1'])
                S.op('dve', lambda e: e.tensor_tensor(out=T1[:], in0=T1[:], in1=LBB[:, 2 * d, :], op=ALU.add),
                     reads=['T1', 'LBB'], writes=['T1'])
                S.op('act', lambda e: e.activation(out=LGF[:], in_=T1[:], func=AF.Ln), reads=['T1'], writes=['LGF'])
                S.op('dve', lambda e: e.tensor_scalar(out=KK[:], in0=T1[:], scalar1=-1.0, scalar2=1.0, op0=ALU.mult,
                                                      op1=ALU.add), reads=['T1'], writes=['KK'])
                if state:
                    S.op('pe', lambda e: e.matmul(PC[:, :], lhsT=C('RF' if d == 0 else 'RB'), rhs=LGF[:],
                                                  start=True, stop=True), reads=['LGF', 'CST'], writes=['PC'])
                    S.op('act', lambda e: e.activation(out=T2[:], in_=PC[:, :], func=AF.Exp), reads=['PC'],
                         writes=['T2'])
                    S.op('dve', lambda e: e.tensor_tensor(out=KST[:], in0=KK[:], in1=T2[:], op=ALU.mult),
                         reads=['KK', 'T2'], writes=['KST'])
                for h in range(4):
                    S.op('pe', lambda e: e.matmul(PD[:, 4 * h:4 * h + 4], lhsT=LGF[:, h * 128:(h + 1) * 128],
                                                  rhs=C('CV'), start=True, stop=True), reads=['LGF', 'CST'],
                         writes=['PD'])
                S.op('act', lambda e: e.activation(out=EV[:, d, :], in_=PD[:, 0:16], func=AF.Exp), reads=['PD'],
                     writes=[f'EV{d}'])
                if full:
                    S.op('pe', lambda e: e.matmul(PC[:, :], lhsT=C('MF' if d == 0 else 'MB'), rhs=LGF[:],
                                                  start=True, stop=True), reads=['LGF', 'CST'], writes=['PC'])
                    sq, sk = (1.0, -1.0) if d == 0 else (-1.0, 1.0)
                    S.op('act', lambda e: e.activation(out=T2[:], in_=PC[:, :], func=AF.Exp, scale=sq), reads=['PC'],
                         writes=['T2'])
                    S.op('act', lambda e: e.activation(out=T3[:], in_=PC[:, :], func=AF.Exp, scale=sk), reads=['PC'],
                         writes=['T3'])
                    S.op('dve', lambda e: e.tensor_tensor(out=QK[:, d, 0, :], in0=SQ[:], in1=T2[:], op=ALU.mult),
                         reads=['SQ', 'T2'], writes=[f'QK{d}'])
                    S.op('dve', lambda e: e.tensor_tensor(out=QK[:, d, 1, :], in0=KK[:], in1=T3[:], op=ALU.mult),
                         reads=['KK', 'T3'], writes=[f'QK{d}'])

            def state_update(d):
                for h in range(4):
                    S.op('pe', lambda e: e.matmul(PC[:, h * 128:(h + 1) * 128], lhsT=KST[:, h * 128:(h + 1) * 128],
                                                  rhs=V[:, h * 128:(h + 1) * 128], start=True, stop=True),
                         reads=['KST', 'V'], writes=['PC'])
                for h in range(4):
                    S.op('dve', lambda e: e.scalar_tensor_tensor(out=SST[:, d, h * 128:(h + 1) * 128],
                                                                 in0=SST[:, d, h * 128:(h + 1) * 128],
                                                                 scalar=EV[:, d, 4 * h:4 * h + 1],
                                                                 in1=PC[:, h * 128:(h + 1) * 128], op0=ALU.mult,
                                                                 op1=ALU.add),
                         reads=[f'SST{d}', f'EV{d}', 'PC'], writes=[f'SST{d}'])

            def state_tile(src_ap, mi, d):
                front(src_ap, mi)
                zgroup('v', PB, 'PB')
                S.op('act', lambda e: e.activation(out=V[:], in_=PB[:, :], func=AF.Identity), reads=['PB'], writes=['V'])
                zgroup('ff' if d == 0 else 'fb', PA, 'PA')
                fprep(d, PA, 'PA', full=False, state=True)

            def route(t):
                LG, GM, PEN, EL, EL2 = RT[:, 0:36], RT[:, 36:40], RT[:, 40:44], RT[:, 44:76], RT[:, 76:108]
                sc = RT[:, 108:128]
                S.op('dve', lambda e: e.tensor_tensor(out=LG, in0=PO[:, 0:36], in1=RBB[:], op=ALU.add),
                     reads=['PO', 'RBB'], writes=['RT'])
                S.op('dve', lambda e: e.reduce_max(out=sc[:, 0:1], in_=RT[:, 0:4], axis=AX.X), reads=['RT'], writes=['RT'])
                S.op('dve', lambda e: e.tensor_scalar(out=GM, in0=RT[:, 0:4], scalar1=sc[:, 0:1], scalar2=None,
                                                      op0=ALU.is_equal), reads=['RT'], writes=['RT'])
                S.op('dve', lambda e: e.tensor_scalar(out=sc[:, 1:2], in0=sc[:, 0:1], scalar1=-1.0, scalar2=None,
                                                      op0=ALU.mult), reads=['RT'], writes=['RT'])
                S.op('act', lambda e: e.activation(out=sc[:, 4:8], in_=RT[:, 0:4], func=AF.Exp, bias=sc[:, 1:2],
                                                   accum_out=sc[:, 2:3]), reads=['RT'], writes=['RT'])
                S.op('dve', lambda e: e.reciprocal(out=sc[:, 3:4], in_=sc[:, 2:3]), reads=['RT'], writes=['RT'])
                S.op('dve', lambda e: e.tensor_scalar(out=PEN, in0=GM, scalar1=-1.0, scalar2=1e30, op0=ALU.add,
                                                      op1=ALU.mult), reads=['RT'], writes=['RT'])
                for g in range(4):
                    S.op('dve', lambda e: e.tensor_scalar(out=RT[:, 44 + 8 * g:52 + 8 * g], in0=RT[:, 4 + 8 * g:12 + 8 * g],
                                                          scalar1=RT[:, 36 + g:37 + g], scalar2=RT[:, 40 + g:41 + g],
                                                          op0=ALU.mult, op1=ALU.add), reads=['RT'], writes=['RT'])
                S.op('dve', lambda e: e.reduce_max(out=sc[:, 8:9], in_=EL, axis=AX.X), reads=['RT'], writes=['RT'])
                S.op('dve', lambda e: e.tensor_scalar(out=SELS[:, t, 0:32], in0=EL, scalar1=sc[:, 8:9], scalar2=None,
                                                      op0=ALU.is_equal), reads=['RT'], writes=['SELS'])
                S.op('dve', lambda e: e.scalar_tensor_tensor(out=EL2, in0=SELS[:, t, 0:32], scalar=-1e30, in1=EL,
                                                             op0=ALU.mult, op1=ALU.add), reads=['RT', 'SELS'],
                     writes=['RT'])
                S.op('dve', lambda e: e.reduce_max(out=sc[:, 9:10], in_=EL2, axis=AX.X), reads=['RT'], writes=['RT'])
                S.op('dve', lambda e: e.tensor_scalar(out=SELS[:, t, 32:64], in0=EL2, scalar1=sc[:, 9:10], scalar2=None,
                                                      op0=ALU.is_equal), reads=['RT'], writes=['SELS'])
                S.op('dve', lambda e: e.tensor_tensor(out=sc[:, 10:11], in0=sc[:, 9:10], in1=sc[:, 8:9], op=ALU.subtract),
                     reads=['RT'], writes=['RT'])
                S.op('act', lambda e: e.activation(out=sc[:, 11:12], in_=sc[:, 10:11], func=AF.Exp), reads=['RT'],
                     writes=['RT'])
                S.op('dve', lambda e: e.tensor_scalar(out=sc[:, 12:13], in0=sc[:, 11:12], scalar1=1.0, scalar2=None,
                                                      op0=ALU.add), reads=['RT'], writes=['RT'])
                S.op('dve', lambda e: e.reciprocal(out=sc[:, 13:14], in_=sc[:, 12:13]), reads=['RT'], writes=['RT'])
                S.op('dve', lambda e: e.tensor_tensor(out=WTS[:, t, 0:1], in0=sc[:, 13:14], in1=sc[:, 3:4], op=ALU.mult),
                     reads=['RT'], writes=['WTS'])
                S.op('dve', lambda e: e.tensor_tensor(out=WTS[:, t, 1:2], in0=WTS[:, t, 0:1], in1=sc[:, 11:12],
                                                      op=ALU.mult), reads=['RT', 'WTS'], writes=['WTS'])

            pending_route = []

            def full_tile(b, i):
                t = b * T + i
                front(x_d[b, i * 128:(i + 1) * 128, :], b)
                zgroup('q', PA, 'PA')
                zgroup('v', PB, 'PB')
                S.op('act', lambda e: e.activation(out=SQ[:], in_=PA[:, :], func=AF.Silu), reads=['PA'], writes=['SQ'])
                S.op('act', lambda e: e.activation(out=V[:], in_=PB[:, :], func=AF.Identity), reads=['PB'], writes=['V'])
                zgroup('fb', PA, 'PA')
                zgroup('ff', PB, 'PB')
                while pending_route:
                    route(pending_route.pop(0))
                fprep(1, PA, 'PA', full=True, state=False)
                zgroup('g', PA, 'PA')
                fprep(0, PB, 'PB', full=True, state=True)
                zgroup('gt', PB, 'PB')
                S.op('act', lambda e: e.activation(out=T1[:], in_=PA[:, :], func=AF.Silu), reads=['PA'], writes=['T1'])
                S.op('dve', lambda e: e.tensor_tensor(out=SGG[:], in0=T1[:], in1=GNB[:], op=ALU.mult),
                     reads=['T1', 'GNB'], writes=['SGG'])
                zgroup('a', PA, 'PA')
                S.op('act', lambda e: e.activation(out=T1[:], in_=PB[:, :], func=AF.Sigmoid), reads=['PB'], writes=['T1'])
                S.op('dve', lambda e: e.tensor_tensor(out=U[:], in0=PA[:, :], in1=T1[:], op=ALU.mult),
                     reads=['PA', 'T1'], writes=['U'])
                chk(20)
                for d in range(2):
                    for qk in range(2):
                        for h in range(4):
                            S.op('pe', lambda e: e.transpose(out=PT[:, (qk * 4 + h) * 128:(qk * 4 + h + 1) * 128],
                                                             in_=QK[:, d, qk, h * 128:(h + 1) * 128], identity=IDB[:]),
                                 reads=[f'QK{d}', 'IDB'], writes=['PT'])
                    eng = 'act' if d == 0 else 'dve'
                    if eng == 'act':
                        S.op('act', lambda e: e.activation(out=QKT[:, d].rearrange("p a h t -> p (a h t)"), in_=PT[:, :],
                                                           func=AF.Identity), reads=['PT'], writes=[f'QKT{d}'])
                    else:
                        S.op('dve', lambda e: e.tensor_copy(out=QKT[:, d].rearrange("p a h t -> p (a h t)"), in_=PT[:, :]),
                             reads=['PT'], writes=[f'QKT{d}'])
                for d in range(2):
                    for h in range(4):
                        S.op('pe', lambda e: e.matmul(PS[:, (d * 4 + h) * 128:(d * 4 + h + 1) * 128],
                                                      lhsT=QKT[:, d, 1, h, :], rhs=QKT[:, d, 0, h, :], start=True, stop=True),
                             reads=[f'QKT{d}'], writes=['PS'])
                for d in range(2):
                    S.op('dve', lambda e: e.tensor_tensor(out=PM[:, d].rearrange("p h t -> p (h t)"),
                                                          in0=PS[:, d * 512:(d + 1) * 512],
                                                          in1=C('MASKF' if d == 0 else 'MASKB'), op=ALU.mult),
                         reads=['PS', 'CST'], writes=[f'PM{d}'])
                chk(21)
                for h in range(4):
                    S.op('act', lambda e: e.activation(out=SFS[:, h, :], in_=SST[:, 0, h * 128:(h + 1) * 128],
                                                       func=AF.Identity, scale=EV[:, 0, 4 * h + 1:4 * h + 2]),
                         reads=['SST0', 'EV0'], writes=['SFS'])
                for h in range(4):
                    hs = slice(h * 128, (h + 1) * 128)
                    S.op('pe', lambda e: e.matmul(PO[:, hs], lhsT=PM[:, 0, h, :], rhs=V[:, hs], start=True, stop=False),
                         reads=['PM0', 'V'], writes=['PO'])
                    S.op('pe', lambda e: e.matmul(PO[:, hs], lhsT=PM[:, 1, h, :], rhs=V[:, hs], start=False, stop=False),
                         reads=['PM1', 'V'], writes=['PO'])
                    S.op('pe', lambda e: e.matmul(PO[:, hs], lhsT=QKT[:, 0, 0, h, :], rhs=SFS[:, h, :], start=False,
                                                  stop=False), reads=['QKT0', 'SFS'], writes=['PO'])
                    S.op('pe', lambda e: e.matmul(PO[:, hs], lhsT=QKT[:, 1, 0, h, :], rhs=SBS[:, i, h, :], start=False,
                                                  stop=True), reads=['QKT1', 'SBS'], writes=['PO'])
                for h in range(4):
                    S.op('act', lambda e: e.activation(out=T3[:, h * 128:(h + 1) * 128], in_=PO[:, h * 128:(h + 1) * 128],
                                                       func=AF.Square, accum_out=SSc[:, 2 + h:3 + h]),
                         reads=['PO'], writes=['T3', 'SSh'])
                S.op('act', lambda e: e.activation(out=SSc[:, 2:6], in_=SSc[:, 2:6], func=AF.Sqrt, scale=1.0 / 128,
                                                   bias=EPSB[:, 0:1]), reads=['SSh', 'EPSB'], writes=['SSh'])
                S.op('dve', lambda e: e.reciprocal(out=SSc[:, 2:6], in_=SSc[:, 2:6]), reads=['SSh'], writes=['SSh'])
                for h in range(4):
                    hs = slice(h * 128, (h + 1) * 128)
                    S.op('dve', lambda e: e.scalar_tensor_tensor(out=MIX[:, hs], in0=PO[:, hs], scalar=SSc[:, 2 + h:3 + h],
                                                                 in1=SGG[:, hs], op0=ALU.mult, op1=ALU.mult),
                         reads=['PO', 'SSh', 'SGG'], writes=['MIX'])
                state_update(0)
                if t == 0 and upto == -22:
                    dump_bf(MIX[:], 'MIX', 0, 512)
                    dump(PO[:, :] if False else SSc[:, 0:8], 'SSh', 600, 8)
                chk(22)
                for h in range(4):
                    S.op('pe', lambda e: e.transpose(out=PT[:, h * 128:(h + 1) * 128], in_=MIX[:, h * 128:(h + 1) * 128],
                                                     identity=IDB[:]), reads=['MIX', 'IDB'], writes=['PT'])
                for c in range(4):
                    S.op('pe', lambda e: e.transpose(out=PT[:, (4 + c) * 128:(5 + c) * 128], in_=U[:, c * 128:(c + 1) * 128],
                                                     identity=IDB[:]), reads=['U', 'IDB'], writes=['PT'])
                chk(28)
                S.op('act', lambda e: e.activation(out=MIXT[:, 0:4, :].rearrange("p k t -> p (k t)"), in_=PT[:, 0:512],
                                                   func=AF.Identity), reads=['PT'], writes=['MIXTa'])
                chk(29)
                if upto == -31:
                    S.op('dve', lambda e: e.tensor_copy(out=U[:, 0:64], in_=PT[:, 512:576]), reads=['PT'], writes=['U'])
                    chk(31)
                if upto == -33:
                    S.op('dve', lambda e: e.tensor_copy(out=U[:, 0:128], in_=PT[:, 512:640]), reads=['PT'], writes=['U'])
                    chk(33)
                if upto == -34:
                    S.op('dve', lambda e: e.tensor_copy(out=U[:, 0:64], in_=PT[:, 0:64]), reads=['PT'], writes=['U'])
                    chk(34)
                if upto == -35:
                    S.op('act', lambda e: e.activation(out=U[:, 0:64], in_=PT[:, 0:64], func=AF.Identity), reads=['PT'], writes=['U'])
                    chk(35)
                if upto == -36:
                    S.op('dve', lambda e: e.tensor_copy(out=MIX[:, 0:512], in_=PT[:, 0:512]), reads=['PT'], writes=['MIX'])
                    chk(36)
                if upto == -37:
                    S.op('dve', lambda e: e.tensor_copy(out=MIX[:, 0:512], in_=PT[:, 0:512]), reads=['PT', 'MIXTa'], writes=['MIX'])
                    chk(37)
                if upto == -32:
                    S.op('dve', lambda e: e.tensor_copy(out=UP[:, 0, 0, 16:80], in_=MIX[:, 0:64]), reads=['MIX'], writes=['UP'])
                    chk(32)
                for c in range(4):
                    for r in range(2):
                        src = PT[:, (4 + c) * 128 + r * 64:(4 + c) * 128 + (r + 1) * 64]
                        if (c + r) % 2 == 0:
                            S.op('dve', lambda e: e.tensor_copy(out=UP[:, c, r, 16:80], in_=src), reads=['PT'], writes=['UP'])
                        else:
                            S.op('act', lambda e: e.activation(out=UP[:, c, r, 16:80], in_=src, func=AF.Identity),
                                 reads=['PT'], writes=['UP'])
                chk(27)
                for c in range(4):
                    for k in range(31):
                        S.op('pe', lambda e: e.matmul(PD[:, c * 128:(c + 1) * 128].rearrange("p (r t) -> p r t", r=2), lhsT=DG[:, c * 31 + k, :],
                                                      rhs=UP[:, c, :, k + 1:k + 65], start=(k == 0), stop=(k == 30)),
                             reads=['DG', 'UP'], writes=['PD'])
                chk(23)
                for c in range(4):
                    S.op('act', lambda e: e.activation(out=CVS[:, c, :], in_=PD[:, c * 128:(c + 1) * 128], func=AF.Identity,
                                                       bias=SV[:, SV_DWB + c:SV_DWB + c + 1]), reads=['PD', 'SV'],
                         writes=['CVS'])
                    S.op('dve', lambda e: e.tensor_tensor(out=SQC[:, c, :], in0=CVS[:, c, :], in1=CVS[:, c, :], op=ALU.mult),
                         reads=['CVS'], writes=['SQC'])
                for c in range(4):
                    S.op('pe', lambda e: e.matmul(PC[:, 0:128], lhsT=C('ONES'), rhs=CVS[:, c, :], start=(c == 0),
                                                  stop=(c == 3)), reads=['CVS', 'CST'], writes=['PC'])
                for c in range(4):
                    S.op('pe', lambda e: e.matmul(PC[:, 128:256], lhsT=C('ONES'), rhs=SQC[:, c, :], start=(c == 0),
                                                  stop=(c == 3)), reads=['SQC', 'CST'], writes=['PC'])
                S.op('dve', lambda e: e.tensor_scalar(out=ST[:, 0, :], in0=PC[:, 0:128], scalar1=1.0 / W, scalar2=None,
                                                      op0=ALU.mult), reads=['PC'], writes=['ST'])
                S.op('dve', lambda e: e.tensor_tensor(out=ST[:, 1, :], in0=ST[:, 0, :], in1=ST[:, 0, :], op=ALU.mult),
                     reads=['ST'], writes=['ST'])
                S.op('dve', lambda e: e.scalar_tensor_tensor(out=ST[:, 2, :], in0=PC[:, 128:256], scalar=1.0 / W,
                                                             in1=ST[:, 1, :], op0=ALU.mult, op1=ALU.subtract),
                     reads=['PC', 'ST'], writes=['ST'])
                S.op('act', lambda e: e.activation(out=ST[:, 2, :], in_=ST[:, 2, :], func=AF.Sqrt, bias=EPSB[:, 0:1]),
                     reads=['ST', 'EPSB'], writes=['ST'])
                S.op('dve', lambda e: e.reciprocal(out=ST[:, 2, :], in_=ST[:, 2, :]), reads=['ST'], writes=['ST'])
                for c in range(4):
                    S.op('dve', lambda e: e.tensor_tensor(out=CVS[:, c, :], in0=CVS[:, c, :], in1=ST[:, 0, :],
                                                          op=ALU.subtract), reads=['CVS', 'ST'], writes=['CVS'])
                    S.op('dve', lambda e: e.tensor_tensor(out=CVS[:, c, :], in0=CVS[:, c, :], in1=ST[:, 2, :],
                                                          op=ALU.mult), reads=['CVS', 'ST'], writes=['CVS'])
                    S.op('dve', lambda e: e.tensor_scalar(out=CVS[:, c, :], in0=CVS[:, c, :],
                                                          scalar1=SV[:, SV_LNG + c:SV_LNG + c + 1],
                                                          scalar2=SV[:, SV_LNB + c:SV_LNB + c + 1], op0=ALU.mult,
                                                          op1=ALU.add), reads=['CVS', 'SV'], writes=['CVS'])
                    S.op('act', lambda e: e.activation(out=MIXT[:, 4 + c, :], in_=CVS[:, c, :], func=AF.Silu),
                         reads=['CVS'], writes=['MIXTb'])
                chk(24)
                for hf in range(2):
                    for k in range(8):
                        S.op('pe', lambda e: e.matmul(PS[:, hf * 512:(hf + 1) * 512], lhsT=MIXT[:, k, :],
                                                      rhs=WOUT[:, k, hf * 512:(hf + 1) * 512], start=(k == 0), stop=(k == 7)),
                             reads=['MIXTa', 'MIXTb', 'WOUT'], writes=['PS'])
                S.op('dve', lambda e: e.tensor_tensor(out=X1[:], in0=PS[:, :], in1=MBC[:, 0, :], op=ALU.mult),
                     reads=['PS', 'MBC'], writes=['X1'])
                S.op('dve', lambda e: e.tensor_tensor(out=X1[:], in0=X1[:], in1=XT[:], op=ALU.add),
                     reads=['X1', 'XT'], writes=['X1'])
                S.dma('sp', lambda e: e.dma_start(out=x1_d[t * 128:(t + 1) * 128, :], in_=X1[:]), reads=['X1'],
                      writes=[f'x1d{t}'])
                if t == 0 and upto == -25:
                    dump(X1[:, 0:512], 'X1', 0, 512)
                    dump(XT[:, 0:512], 'XT', 512, 512)
                    dump(MBC[:, 0, 0:512], 'MBC', 1024, 512)
                    dump(SQ[:, 0:512], 'SQ', 1536, 512)
                chk(25)
                S.op('act', lambda e: e.activation(out=XN[:], in_=X1[:], func=AF.Square, accum_out=SSc[:, 6:7]),
                     reads=['X1'], writes=['XN', 'SS2'])
                rsqrt_col(SSc[:, 7:8], SSc[:, 6:7], 1, 1.0 / D, name_d='SS2b', name_s='SS2')
                S.op('dve', lambda e: e.scalar_tensor_tensor(out=HX2[:], in0=X1[:], scalar=SSc[:, 7:8], in1=MBC[:, 1, :],
                                                             op0=ALU.mult, op1=ALU.mult), reads=['X1', 'SS2b', 'MBC'],
                     writes=['HX2'])
                S.op('dve', lambda e: e.tensor_tensor(out=HX2[:], in0=HX2[:], in1=MBC[:, 2, :], op=ALU.add),
                     reads=['HX2', 'MBC'], writes=['HX2'])
                S.op('act', lambda e: e.activation(out=HX2B[:], in_=HX2[:], func=AF.Identity), reads=['HX2'],
                     writes=['HX2B'])
                S.dma('sp', lambda e: e.dma_start(out=hx2_d[t * 128:(t + 1) * 128, :], in_=HX2B[:]), reads=['HX2B'],
                      writes=[f'hx2d{t}'])
                chk(26)
                for k in range(8):
                    S.op('pe', lambda e: e.transpose(out=PS[:, k * 128:(k + 1) * 128], in_=HX2[:, k * 128:(k + 1) * 128],
                                                     identity=C('IDF')), reads=['HX2', 'CST'], writes=['PS'])
                S.op('act', lambda e: e.activation(out=HX2T[:].rearrange("p k t -> p (k t)"), in_=PS[:, :],
                                                   func=AF.Identity), reads=['PS'], writes=['HX2T'])
                for k in range(8):
                    S.op('pe', lambda e: e.matmul(PO[:, 0:36], lhsT=HX2T[:, k, :], rhs=WR[:, k, :], start=(k == 0),
                                                  stop=(k == 7)), reads=['HX2T', 'WR'], writes=['PO'])
                pending_route.append(t)

            for b in range(NB):
                for d in range(2):
                    S.op('dve', lambda e: e.memset(SST[:, d, :], 0.0), writes=[f'SST{d}'])
                    order = range(CT) if d == 0 else range(CT - 1, -1, -1)
                    for ci in order:
                        state_tile(ctx_d[b, ci * 128:(ci + 1) * 128, :], 2, d)
                        chk(11)
                        state_update(d)
                        chk(12)
                if b == 0 and upto == -15:
                    dump(SST[:, 0, :], 'SST0', 0, 512)
                    dump(SST[:, 1, :], 'SST1', 512, 512)
                    chk(15)
                for i in range(T - 1, -1, -1):
                    state_tile(x_d[b, i * 128:(i + 1) * 128, :], b, 1)
                    for h in range(4):
                        S.op('act', lambda e: e.activation(out=SBS[:, i, h, :], in_=SST[:, 1, h * 128:(h + 1) * 128],
                                                           func=AF.Identity, scale=EV[:, 1, 4 * h + 2:4 * h + 3]),
                             reads=['SST1', 'EV1'], writes=['SBS'])
                    state_update(1)
                chk(13)
                chk(200 + b)
                for s in range(3):
                    S.dma('sp', lambda e: e.dma_start(out=MBC[:, s, :], in_=modbc_d[b, s]), reads=[f'modbc{b}.{s}'],
                          writes=['MBC'])
                for i in range(T):
                    full_tile(b, i)
                while pending_route:
                    route(pending_route.pop(0))
                    chk(14)
                    chk(100 + b * T + i)
            S.barrier()

        if debug and upto == 1:
            S.dma('sp', lambda e: e.dma_start(out=dbg_d[:, 2048:2048 + NT * 2], in_=WTS[:].rearrange("p t c -> p (t c)")),
                  reads=['WTS'], writes=['dbg'])
            S.barrier(['sp'])
            return nc

        with ExitStack() as es2:
            def sb2(name, shape, dt=F32):
                return es2.enter_context(nc.sbuf_tensor(name, shape, dt))
            SELT = sb2("SELT", [128, NT + 1, NE])
            CUM = sb2("CUM", [128, NT + 1, NE])
            RANK = sb2("RANK", [128, NT, NE])
            CN = sb2("CN", [128, 8, NE])
            CNI = sb2("CNI", [128, NE], I32)
            BE = sb2("BE", [128, 2, NBLK])
            POSF = sb2("POSF", [128, NT, 2])
            TMP = sb2("TMP", [128, NE])
            XB = sb2("XBs", [128, D], BF16)

            S.op('dve', lambda e: e.tensor_tensor(out=SELT[:, 0:NT, :], in0=SELS[:, :, 0:32], in1=SELS[:, :, 32:64],
                                                  op=ALU.add), reads=['SELS'], writes=['SELT'])
            S.op('dve', lambda e: e.memset(CUM[:, 0, :], 0.0), writes=['CUM'])
            for t in range(NT):
                S.op('dve', lambda e: e.tensor_tensor(out=CUM[:, t + 1, :], in0=CUM[:, t, :], in1=SELT[:, t, :], op=ALU.add),
                     reads=['CUM', 'SELT'], writes=['CUM'])
            for t in range(NT):
                S.op('pe', lambda e: e.matmul(PA[:, 0:NE], lhsT=C('RB'), rhs=SELT[:, t, :], start=True, stop=False),
                     reads=['SELT', 'CST'], writes=['PA'])
                S.op('pe', lambda e: e.matmul(PA[:, 0:NE], lhsT=C('ONES'), rhs=CUM[:, t, :], start=False, stop=True),
                     reads=['CUM', 'CST'], writes=['PA'])
                S.op('dve', lambda e: e.tensor_copy(out=RANK[:, t, :], in_=PA[:, 0:NE]), reads=['PA'], writes=['RANK'])
            S.op('pe', lambda e: e.matmul(PA[:, 0:NE], lhsT=C('ONES'), rhs=CUM[:, NT, :], start=True, stop=True),
                 reads=['CUM', 'CST'], writes=['PA'])
            S.op('dve', lambda e: e.tensor_scalar(out=CN[:, 0, :], in0=PA[:, 0:NE], scalar1=127.0, scalar2=None, op0=ALU.add),
                 reads=['PA'], writes=['CN'])
            S.op('dve', lambda e: e.tensor_copy(out=CNI[:], in_=CN[:, 0, :]), reads=['CN'], writes=['CNI'])
            S.op('dve', lambda e: e.tensor_single_scalar(out=CNI[:], in_=CNI[:], scalar=7, op=ALU.arith_shift_right),
                 reads=['CNI'], writes=['CNI'])
            S.op('dve', lambda e: e.tensor_copy(out=CN[:, 1, :], in_=CNI[:]), reads=['CNI'], writes=['CN'])
            S.op('dve', lambda e: e.tensor_copy(out=CN[:, 2, :], in_=CN[:, 1, :]), reads=['CN'], writes=['CN'])
            cur = 2
            for sh in (1, 2, 4, 8, 16):
                nxt = 5 - cur
                S.op('dve', lambda e: e.tensor_copy(out=CN[:, nxt, :], in_=CN[:, cur, :]), reads=['CN'], writes=['CN'])
                S.op('dve', lambda e: e.tensor_tensor(out=CN[:, nxt, sh:NE], in0=CN[:, cur, sh:NE], in1=CN[:, cur, 0:NE - sh],
                                                      op=ALU.add), reads=['CN'], writes=['CN'])
                cur = nxt
            PEND = CN[:, cur, :]
            S.op('dve', lambda e: e.tensor_tensor(out=CN[:, 4, :], in0=PEND, in1=CN[:, 1, :], op=ALU.subtract),
                 reads=['CN'], writes=['CN'])
            S.op('dve', lambda e: e.tensor_scalar(out=CN[:, 4, :], in0=CN[:, 4, :], scalar1=128.0, scalar2=None, op0=ALU.mult),
                 reads=['CN'], writes=['CN'])
            for t in range(NT):
                S.op('dve', lambda e: e.tensor_tensor(out=RANK[:, t, :], in0=RANK[:, t, :], in1=CN[:, 4, :], op=ALU.add),
                     reads=['RANK', 'CN'], writes=['RANK'])
                for j in range(2):
                    S.op('dve', lambda e: e.tensor_tensor(out=TMP[:], in0=RANK[:, t, :], in1=SELS[:, t, 32 * j:32 * j + 32],
                                                          op=ALU.mult), reads=['RANK', 'SELS'], writes=['TMP'])
                    S.op('dve', lambda e: e.reduce_sum(out=POSF[:, t, j:j + 1], in_=TMP[:], axis=AX.X), reads=['TMP'],
                         writes=['POSF'])
            S.op('dve', lambda e: e.tensor_copy(out=IDX[:].rearrange("p t c -> p (t c)"),
                                                in_=POSF[:].rearrange("p t c -> p (t c)")), reads=['POSF'], writes=['IDX'])
            S.op('dve', lambda e: e.memset(BE[:, 0, :], 0.0), writes=['BE'])
            for ex in range(NE):
                S.op('dve', lambda e: e.scalar_tensor_tensor(out=BE[:, 0, :], in0=C('IOTA', 0, NBLK),
                                                             scalar=CN[:, cur, ex:ex + 1], in1=BE[:, 0, :],
                                                             op0=ALU.is_ge, op1=ALU.add), reads=['CST', 'CN', 'BE'],
                     writes=['BE'])
            S.op('dve', lambda e: e.tensor_scalar(out=BE[:, 0, :], in0=BE[:, 0, :], scalar1=float(NE - 1), scalar2=None,
                                                  op0=ALU.min), reads=['BE'], writes=['BE'])
            S.op('dve', lambda e: e.memset(BE[:, 1, 0:1], 1.0), reads=[], writes=['BE1'])
            S.op('dve', lambda e: e.tensor_tensor(out=BE[:, 1, 1:NBLK], in0=BE[:, 0, 1:NBLK], in1=BE[:, 0, 0:NBLK - 1],
                                                  op=ALU.not_equal), reads=['BE', 'BE1'], writes=['BE1'])
            S.op('dve', lambda e: e.tensor_copy(out=BEI[:], in_=BE[:].rearrange("p a n -> p (a n)")), reads=['BE', 'BE1'],
                 writes=['BEI'])
            S.op('dve', lambda e: e.tensor_scalar(out=BE[:, 0, :], in0=BE[:, 0, :], scalar1=128.0, scalar2=C('PCOL', 0, 1),
                                                  op0=ALU.mult, op1=ALU.add), reads=['BE', 'CST', 'BEI'], writes=['BE'])
            S.op('dve', lambda e: e.tensor_scalar(out=BE[:, 0, :], in0=BE[:, 0, :], scalar1=-1.0e6, scalar2=None, op0=ALU.add),
                 reads=['BE'], writes=['BE'])
            S.op('dve', lambda e: e.tensor_tensor(out=BE[:, 0, :], in0=BE[:, 0, :], in1=BE[:, 1, :], op=ALU.mult),
                 reads=['BE', 'BE1'], writes=['BE'])
            S.op('dve', lambda e: e.tensor_scalar(out=BE[:, 0, :], in0=BE[:, 0, :], scalar1=1.0e6, scalar2=None, op0=ALU.add),
                 reads=['BE'], writes=['BE'])
            S.op('dve', lambda e: e.tensor_copy(out=IDXW[:], in_=BE[:, 0, :]), reads=['BE'], writes=['IDXW'])
            for t in range(NT):
                S.dma('sp', lambda e: e.dma_start(out=XB[:], in_=hx2_d[t * 128:(t + 1) * 128, :]), reads=[f'hx2d{t}'],
                      writes=['XBs'])
                for j in range(2):
                    S.dma('pool', lambda e: e.indirect_dma_start(out=xbuf_d, out_offset=bass.IndirectOffsetOnAxis(IDX[:, t, j:j + 1], 0),
                                                                 in_=XB[:], in_offset=None), reads=['XBs', 'IDX'],
                          writes=['xbuf'])
            S.barrier()

        if debug and upto == 2:
            S.dma('sp', lambda e: e.dma_start(out=dbgi_d[:, 0:NT * 2], in_=IDX[:].rearrange("p t c -> p (t c)")),
                  reads=['IDX'], writes=['dbgi'])
            S.dma('sp', lambda e: e.dma_start(out=dbgi_d[:, 256:256 + 2 * NBLK], in_=BEI[:]), reads=['BEI'], writes=['dbgi'])
            S.dma('sp', lambda e: e.dma_start(out=dbgi_d[:, 512:512 + NBLK], in_=IDXW[:]), reads=['IDXW'], writes=['dbgi'])
            S.barrier(['sp'])
            return nc

        with ExitStack() as es3:
            def sb3(name, shape, dt=F32):
                return es3.enter_context(nc.sbuf_tensor(name, shape, dt))
            WG = sb3("WG", [128, 8, W], BF16)
            WU = sb3("WU", [128, 8, W], BF16)
            WD = sb3("WD", [128, 4, D], BF16)
            XBK = [sb3(f"XBK{i}", [128, D], BF16) for i in range(2)]
            XBT = [sb3(f"XBT{i}", [128, 8, 128], BF16) for i in range(2)]
            HS = sb3("HS", [128, W])
            HT = sb3("HT", [128, 4, 128], BF16)
            YB = [sb3(f"YB{i}", [128, D]) for i in range(2)]
            weg_v = weg_d.rearrange("e (p k) n -> (e p) (k n)", k=8)
            weu_v = weu_d.rearrange("e (p k) n -> (e p) (k n)", k=8)
            wed_v = wed_d.rearrange("e (p k) n -> (e p) (k n)", k=4)
            bc_reg = nc.gpsimd.alloc_register("bc_reg")
            nc.gpsimd.reg_mov(bc_reg, NE * 128 - 1)
            def xload(j):
                p = j % 2
                S.dma('sp', lambda e: e.dma_start(out=XBK[p][:], in_=xbuf_d[j * 128:(j + 1) * 128, :]), reads=['xbuf'],
                      writes=[f'XBK{p}'])

            def transp(j):
                p = j % 2
                for k in range(8):
                    S.op('pe', lambda e: e.transpose(out=PT[:, k * 128:(k + 1) * 128], in_=XBK[p][:, k:D:8],
                                                     identity=IDB[:]), reads=[f'XBK{p}', 'IDB'], writes=['PT'])
                S.op('dve', lambda e: e.tensor_copy(out=XBT[p][:].rearrange("p k t -> p (k t)"), in_=PT[:, :]),
                     reads=['PT'], writes=[f'XBT{p}'])

            xload(0)
            transp(0)
            for j in range(NBLK):
                p = j % 2
                for (wt, wv, wn) in ((WG, weg_v, 'WG'), (WU, weu_v, 'WU'), (WD, wed_v, 'WD')):
                    S.dma('pool', lambda e: e.indirect_dma_start(out=wt[:].rearrange("p k n -> p (k n)"), out_offset=None, in_=wv,
                                                                 in_offset=bass.IndirectOffsetOnAxis(IDXW[:, j:j + 1], 0),
                                                                 bounds_check=bc_reg, oob_is_err=False),
                          reads=['IDXW'], writes=[wn])
                if j + 1 < NBLK:
                    xload(j + 1)
                for f in range(4):
                    for k in range(8):
                        S.op('pe', lambda e: e.matmul(PA[:, f * 128:(f + 1) * 128], lhsT=WG[:, k, f:W:4],
                                                      rhs=XBT[p][:, k, :], start=(k == 0), stop=(k == 7)),
                             reads=['WG', f'XBT{p}'], writes=['PA'])
                for f in range(4):
                    for k in range(8):
                        S.op('pe', lambda e: e.matmul(PB[:, f * 128:(f + 1) * 128], lhsT=WU[:, k, f:W:4],
                                                      rhs=XBT[p][:, k, :], start=(k == 0), stop=(k == 7)),
                             reads=['WU', f'XBT{p}'], writes=['PB'])
                S.op('act', lambda e: e.activation(out=HS[:], in_=PA[:, :], func=AF.Silu), reads=['PA'], writes=['HS'])
                S.op('dve', lambda e: e.tensor_tensor(out=HT[:].rearrange("p f t -> p (f t)"), in0=HS[:], in1=PB[:, :],
                                                      op=ALU.mult), reads=['HS', 'PB'], writes=['HT'])
                if j + 1 < NBLK:
                    transp(j + 1)
                for hf in range(2):
                    for f in range(4):
                        S.op('pe', lambda e: e.matmul(PS[:, hf * 512:(hf + 1) * 512], lhsT=HT[:, f, :],
                                                      rhs=WD[:, f, hf * 512:(hf + 1) * 512], start=(f == 0), stop=(f == 3)),
                             reads=['HT', 'WD'], writes=['PS'])
                S.op('act', lambda e: e.activation(out=YB[p][:], in_=PS[:, :], func=AF.Identity), reads=['PS'],
                     writes=[f'YB{p}'])
                S.dma('sp', lambda e: e.dma_start(out=ybuf_d[j * 128:(j + 1) * 128, :], in_=YB[p][:]), reads=[f'YB{p}'],
                      writes=['ybuf'])
            S.barrier()

        with ExitStack() as es4:
            def sb4(name, shape, dt=F32):
                return es4.enter_context(nc.sbuf_tensor(name, shape, dt))
            FGB = sb4("FGB", [128, D])
            G2B = sb4("G2B", [128, D])
            Y1 = [sb4(f"Y1{i}", [128, D]) for i in range(2)]
            Y2 = [sb4(f"Y2{i}", [128, D]) for i in range(2)]
            XR = [sb4(f"XR{i}", [128, D]) for i in range(2)]
            MO = sb4("MO", [128, D])
            OT = [sb4(f"OT{i}", [128, D]) for i in range(2)]
            JK = sb4("JK", [128, D], BF16)
            SF = sb4("SF", [128, 2])
            bcast_row(FGB, D, fng_d, 'FGB')
            for b in range(NB):
                S.dma('sp', lambda e: e.dma_start(out=G2B[:], in_=modbc_d[b, 3]), reads=[f'modbc{b}.3'], writes=['G2B'])
                for i in range(T):
                    t = b * T + i
                    p = t % 2
                    S.dma('pool', lambda e: e.indirect_dma_start(out=Y1[p][:], out_offset=None, in_=ybuf_d,
                                                                 in_offset=bass.IndirectOffsetOnAxis(IDX[:, t, 0:1], 0)),
                          reads=['ybuf', 'IDX'], writes=[f'Y1{p}'])
                    S.dma('pool', lambda e: e.indirect_dma_start(out=Y2[p][:], out_offset=None, in_=ybuf_d,
                                                                 in_offset=bass.IndirectOffsetOnAxis(IDX[:, t, 1:2], 0)),
                          reads=['ybuf', 'IDX'], writes=[f'Y2{p}'])
                    if t == 0:
                        S.dma('sp', lambda e: e.dma_start(out=XR[0][:], in_=x1_d[0:128, :]), reads=['x1d0'], writes=['XR0'])
                    if t + 1 < NT:
                        S.dma('sp', lambda e: e.dma_start(out=XR[1 - p][:], in_=x1_d[(t + 1) * 128:(t + 2) * 128, :]),
                              reads=[f'x1d{t + 1}'], writes=[f'XR{1 - p}'])
                    S.op('act', lambda e: e.activation(out=MO[:], in_=Y1[p][:], func=AF.Identity, scale=WTS[:, t, 0:1]),
                         reads=[f'Y1{p}', 'WTS'], writes=['MO'])
                    S.op('dve', lambda e: e.scalar_tensor_tensor(out=MO[:], in0=Y2[p][:], scalar=WTS[:, t, 1:2], in1=MO[:],
                                                                 op0=ALU.mult, op1=ALU.add), reads=[f'Y2{p}', 'WTS', 'MO'],
                         writes=['MO'])
                    S.op('dve', lambda e: e.tensor_tensor(out=MO[:], in0=MO[:], in1=G2B[:], op=ALU.mult),
                         reads=['MO', 'G2B'], writes=['MO'])
                    S.op('dve', lambda e: e.tensor_tensor(out=MO[:], in0=MO[:], in1=XR[p][:], op=ALU.add),
                         reads=['MO', f'XR{p}'], writes=['MO'])
                    S.op('act', lambda e: e.activation(out=JK[:], in_=MO[:], func=AF.Square, accum_out=SF[:, 0:1]),
                         reads=['MO'], writes=['JK', 'SF'])
                    rsqrt_col(SF[:, 1:2], SF[:, 0:1], 1, 1.0 / D, name_d='SF1', name_s='SF')
                    S.op('dve', lambda e: e.scalar_tensor_tensor(out=OT[p][:], in0=MO[:], scalar=SF[:, 1:2], in1=FGB[:],
                                                                 op0=ALU.mult, op1=ALU.mult), reads=['MO', 'SF1', 'FGB'],
                         writes=[f'OT{p}'])
                    S.dma('sp', lambda e: e.dma_start(out=out_d[b, i * 128:(i + 1) * 128, :], in_=OT[p][:]),
                          reads=[f'OT{p}'], writes=['out'])
            S.barrier()
    except _Stop:
        pass
    return nc


def host_inputs(inputs, NB=2, cores=N_CORES):
    f = lambda a: np.ascontiguousarray(np.asarray(a, dtype=np.float32))
    x, c, ctx = f(inputs['x']), f(inputs['c']), f(inputs['ctx'])
    c_ctx = f(inputs['c_ctx'])
    sv = np.zeros((128, NSV), np.float32)
    sv[:, SV_DWB:SV_DWB + 4] = f(inputs['dw_bias'])[0].reshape(4, 128).T
    sv[:, SV_LNG:SV_LNG + 4] = f(inputs['conv_ln_g'])[0].reshape(4, 128).T
    sv[:, SV_LNB:SV_LNB + 4] = f(inputs['conv_ln_b'])[0].reshape(4, 128).T
    dwk = f(inputs['dw_kernel'])[0]
    sv[:, SV_DWK:SV_DWK + 124] = dwk.reshape(31, 4, 128).transpose(2, 1, 0).reshape(128, 124)
    shared = {
        'w_ada': f(inputs['w_ada'])[0], 'b_ada': f(inputs['b_ada'])[0][None, :],
        'w_in': f(inputs['w_in'])[0], 'w_out': f(inputs['w_out'])[0],
        'n1g': f(inputs['norm1_g'])[0][None, :], 'n2g': f(inputs['norm2_g'])[0][None, :],
        'fng': f(inputs['final_norm_g'])[None, :],
        'lbl': f(inputs['lb_logits'])[:, :2, :].reshape(1, 4 * W),
        'gn4': np.tile(f(inputs['hgrn_norm_g'])[0], 4)[None, :],
        'smallv': sv,
        'wr': np.ascontiguousarray(np.concatenate([f(inputs['router_group_w'])[0], f(inputs['router_expert_w'])[0]], axis=1)),
        'rb': np.concatenate([f(inputs['router_group_b'])[0], f(inputs['router_expert_b'])[0]])[None, :],
        'weg': f(inputs['w_expert_gate'])[0], 'weu': f(inputs['w_expert_up'])[0], 'wed': f(inputs['w_expert_down'])[0],
        'consts': CONSTS,
    }
    maps = []
    for k in range(cores):
        cT = np.zeros((128, 8, 3), np.float32)
        for b in range(NB):
            cT[:, :, b] = c[k * NB + b].reshape(8, 128).T
        cT[:, :, 2] = c_ctx.reshape(8, 128).T
        m = dict(shared)
        m['x'] = np.ascontiguousarray(x[k * NB:(k + 1) * NB])
        m['ctx'] = np.ascontiguousarray(ctx[k * NB:(k + 1) * NB])
        m['cT'] = cT
        maps.append(m)
    return maps


def kernel(**inputs):
    nc = build()
    maps = host_inputs(inputs)
    res = run_bass_kernel_spmd(nc, maps, core_ids=list(range(N_CORES)))
    return np.concatenate([np.asarray(r['out'], dtype=np.float32) for r in res.results], axis=0)
```

```python
import numpy as np
from contextlib import ExitStack
import concourse.bass as bass
import concourse.mybir as mybir
from concourse.bass_utils import run_bass_kernel_spmd

F32 = mybir.dt.float32
BF16 = mybir.dt.bfloat16
I32 = mybir.dt.int32
AF = mybir.ActivationFunctionType
ALU = mybir.AluOpType
AX = mybir.AxisListType

D = 1024
W = 512
NCOL = 3584
NE = 32
EPS = 1e-6
N_CORES = 8


class _Stop(Exception):
    pass


class Sched:
    NDS = 8

    def __init__(self, nc, es):
        self.nc = nc
        self.eng = {'pe': nc.tensor, 'act': nc.scalar, 'dve': nc.vector, 'pool': nc.gpsimd, 'sp': nc.sync}
        self.esem = {k: es.enter_context(nc.semaphore('sem_' + k)) for k in self.eng}
        self.ecnt = {k: 0 for k in self.eng}
        self.dsem = {q: [es.enter_context(nc.semaphore(f'd{q}{i}')) for i in range(self.NDS)] for q in ('sp', 'pool')}
        self.dcnt = {q: [0] * self.NDS for q in ('sp', 'pool')}
        self.drr = {'sp': 0, 'pool': 0}
        self.known = {k: {} for k in self.eng}
        self.lastw = {}
        self.rd = {}
        self.alias = {}
        self.pool_hist = []
        self.pool_depth = 2

    def _wait(self, e, ev):
        key, sem, val, src = ev
        if self.known[e].get(key, 0) >= val:
            return
        self.eng[e].wait_ge(sem, val)
        self.known[e][key] = val

    def _deps(self, e, reads, writes):
        evs = []
        for b in reads:
            if b in self.lastw:
                evs.append(self.lastw[b])
        for b in writes:
            if b in self.lastw:
                evs.append(self.lastw[b])
            evs.extend(self.rd.get(b, {}).values())
        for ev in evs:
            if ev[3] == 'pe' and e == 'pe':
                continue
            self._wait(e, ev)

    def _commit(self, ev, reads, writes):
        for b in reads:
            self.rd.setdefault(b, {})[ev[0]] = ev
        for b in writes:
            self.lastw[b] = ev
            self.rd[b] = {}

    PSUM_NAMES = ('PA', 'PB', 'PC', 'PD', 'PT', 'PS', 'PO')

    def op(self, e, fn, reads=(), writes=()):
        reads = [self.alias.get(b, b) for b in reads]
        writes = [self.alias.get(b, b) for b in writes]
        writes = writes + [b for b in reads if b in self.PSUM_NAMES and b not in writes]
        reads = [b for b in reads if b not in self.PSUM_NAMES]
        self._deps(e, reads, writes)
        ins = fn(self.eng[e])
        self.ecnt[e] += 1
        ins.then_inc(self.esem[e], 1)
        self._commit(('e_' + e, self.esem[e], self.ecnt[e], e), reads, writes)

    def dma(self, q, fn, reads=(), writes=()):
        reads = [self.alias.get(b, b) for b in reads]
        writes = [self.alias.get(b, b) for b in writes]
        i = self.drr[q]
        self.drr[q] = (i + 1) % self.NDS
        sem = self.dsem[q][i]
        key = f'd_{q}{i}'
        if self.dcnt[q][i] > 0:
            self._wait(q, (key, sem, self.dcnt[q][i], 'dma'))
        if q == 'pool' and len(self.pool_hist) >= self.pool_depth:
            self._wait(q, self.pool_hist[-self.pool_depth])
        self._deps(q, reads, writes)
        ins = fn(self.eng[q])
        self.dcnt[q][i] += 16
        ins.then_inc(sem, 16)
        ev = (key, sem, self.dcnt[q][i], 'dma')
        if q == 'pool':
            self.pool_hist.append(ev)
        self._commit(ev, reads, writes)

    def barrier(self, engines=None):
        engines = engines or list(self.eng)
        for e in engines:
            for k in self.eng:
                if k != e and self.ecnt[k] > 0:
                    self._wait(e, ('e_' + k, self.esem[k], self.ecnt[k], k))
            for q in ('sp', 'pool'):
                for i in range(self.NDS):
                    if self.dcnt[q][i] > 0:
                        self._wait(e, (f'd_{q}{i}', self.dsem[q][i], self.dcnt[q][i], 'dma'))


def make_consts():
    s = np.arange(128)[:, None]
    t = np.arange(128)[None, :]
    c = {}
    c['IDF'] = (s == t)
    c['MF'] = ((s >= 64) & (s <= t)) * 1.0 - ((s > t) & (s <= 63)) * 1.0
    c['RF'] = (s > t)
    c['MB'] = ((s >= 64) & (s < t)) * 1.0 - ((s >= t) & (s <= 63)) * 1.0
    c['RB'] = (s < t)
    c['MASKF'] = np.tile((s <= t) * 1.0, (1, 4))
    c['MASKB'] = np.tile((s >= t) * 1.0, (1, 4))
    c['ONES'] = np.ones((128, 128))
    cv = np.zeros((128, 4))
    cv[:, 0] = 1.0
    cv[:64, 1] = 1.0
    cv[64:, 2] = 1.0
    c['CV'] = cv
    c['IOTA'] = np.tile(np.arange(128)[None, :], (128, 1))
    c['PCOL'] = np.tile(np.arange(128)[:, None], (1, 2))
    offs = {}
    cols = []
    o = 0
    for k, v in c.items():
        v = np.asarray(v, np.float32)
        offs[k] = (o, v.shape[1])
        cols.append(v)
        o += v.shape[1]
    return np.concatenate(cols, axis=1).astype(np.float32), offs


CONSTS, COFF = make_consts()
NCONST = CONSTS.shape[1]
SV_DWB, SV_LNG, SV_LNB, SV_DWK, NSV = 0, 4, 8, 12, 12 + 124


def build(NB=2, T=16, CT=2, debug=False, upto=9):
    NT = NB * T
    NBLK = NT * 2 + NE
    nc = bass.Bass("TRN2", target_bir_lowering=False)

    def din(name, shape, dt=F32):
        return nc.dram_tensor(name, shape, dt, kind="ExternalInput").ap()

    x_d = din("x", [NB, T * 128, D])
    ctx_d = din("ctx", [NB, CT * 128, D])
    ct_d = din("cT", [128, 8, 3])
    wada_d = din("w_ada", [D, 6 * D])
    bada_d = din("b_ada", [1, 6 * D])
    win_d = din("w_in", [D, NCOL])
    wout_d = din("w_out", [D, D])
    n1g_d = din("n1g", [1, D])
    n2g_d = din("n2g", [1, D])
    fng_d = din("fng", [1, D])
    lbl_d = din("lbl", [1, 4 * W])
    gn4_d = din("gn4", [1, W])
    sv_d = din("smallv", [128, NSV])
    wr_d = din("wr", [D, 36])
    rb_d = din("rb", [1, 36])
    weg_d = din("weg", [NE, D, W])
    weu_d = din("weu", [NE, D, W])
    wed_d = din("wed", [NE, W, D])
    cst_d = din("consts", [128, NCONST])
    out_d = nc.dram_tensor("out", [NB, T * 128, D], F32, kind="ExternalOutput").ap()
    dk = "ExternalOutput" if debug else "Internal"
    x1_d = nc.dram_tensor("x1d", [NT * 128, D], F32, kind=dk).ap()
    hx2_d = nc.dram_tensor("hx2d", [NT * 128, D], BF16, kind=dk).ap()
    modbc_d = nc.dram_tensor("modbc", [NB, 4, 128, D], F32, kind="Internal").ap()
    xbuf_d = nc.dram_tensor("xbuf", [NBLK * 128, D], BF16, kind="Internal").ap()
    ybuf_d = nc.dram_tensor("ybuf", [NBLK * 128, D], F32, kind=dk).ap()
    if debug:
        dbg_d = nc.dram_tensor("dbg", [128, 4096], F32, kind="ExternalOutput").ap()
        dbgi_d = nc.dram_tensor("dbgi", [128, 1024], I32, kind="ExternalOutput").ap()

    try:
      with ExitStack() as es:
        S = Sched(nc, es)

        dbg_list = []

        def dump(ap, name, off, n):
            if debug:
                S.dma('sp', lambda e: e.dma_start(out=dbg_d[:, off:off + n], in_=ap), reads=[name], writes=['dbg'])

        MARK = es.enter_context(nc.sbuf_tensor("MARK", [128, 128], F32))
        DBGT = es.enter_context(nc.sbuf_tensor("DBGT", [128, 512], F32)) if debug else None

        def dump_bf(ap, name, off, n):
            if debug:
                S.op('dve', lambda e: e.tensor_copy(out=DBGT[:, 0:n], in_=ap), reads=[name], writes=['DBGT'])
                dump(DBGT[:, 0:n], 'DBGT', off, n)

        def chk(n):
            if debug and upto == -n:
                S.op('dve', lambda e: e.memset(MARK[:], float(n)), writes=['MARK'])
                dump(MARK[:], 'MARK', 3968, 128)
                S.barrier()
                raise _Stop()

        def sb(name, shape, dt=F32):
            return es.enter_context(nc.sbuf_tensor(name, shape, dt))

        def ps(name, shape, dt=F32):
            return es.enter_context(nc.psum_tensor(name, shape, dt))

        CST = sb("CST", [128, NCONST])

        def C(k, lo=0, n=None):
            o, w = COFF[k]
            n = w if n is None else n
            return CST[:, o + lo:o + lo + n]

        IDB = sb("IDB", [128, 128], BF16)
        ONEB = sb("ONEB", [128, 128], BF16)
        SV = sb("SV", [128, NSV])
        LBB = sb("LBB", [128, 4, W])
        GNB = sb("GNB", [128, W])
        A1T = sb("A1T", [128, 3, 8])
        S1T = sb("S1T", [128, 3, 8])
        WR = sb("WR", [128, 8, 36])
        RBB = sb("RBB", [128, 36])
        SELS = sb("SELS", [128, NT, 64], BF16)
        WTS = sb("WTS", [128, NT, 2])
        IDX = sb("IDX", [128, NT, 2], I32)
        BEI = sb("BEI", [128, 2 * NBLK], I32)
        IDXW = sb("IDXW", [128, NBLK], I32)
        ROW = sb("ROW", [1, 512])
        PA = ps("PA", [128, 512])
        PB = ps("PB", [128, 512])
        PC = ps("PC", [128, 512])
        PD = ps("PD", [128, 512])
        PT = ps("PT", [128, 1024], BF16)
        PS = ps("PS", [128, 1024])
        PO = ps("PO", [128, 512])

        S.dma('sp', lambda e: e.dma_start(out=CST[:], in_=cst_d), writes=['CST'])
        S.dma('sp', lambda e: e.dma_start(out=SV[:], in_=sv_d), writes=['SV'])
        S.dma('sp', lambda e: e.dma_start(out=WR[:], in_=wr_d.rearrange("(k p) n -> p k n", p=128)), writes=['WR'])
        S.op('dve', lambda e: e.tensor_copy(out=IDB[:], in_=C('IDF')), reads=['CST'], writes=['IDB'])
        S.op('dve', lambda e: e.tensor_copy(out=ONEB[:], in_=C('ONES')), reads=['CST'], writes=['ONEB'])

        def bcast_row(dst_ap, n, src_dram_ap, dstname, post=None):
            for h in range(0, n, 512):
                m = min(512, n - h)
                S.dma('sp', lambda e: e.dma_start(out=ROW[0:1, 0:m], in_=src_dram_ap[:, h:h + m]), writes=['ROW'])
                S.op('pe', lambda e: e.matmul(PC[:, 0:m], lhsT=C('ONES')[0:1, :], rhs=ROW[0:1, 0:m],
                                              start=True, stop=True), reads=['ROW', 'CST'], writes=['PC'])
                S.op('dve', lambda e: e.tensor_copy(out=dst_ap[:, h:h + m], in_=PC[:, 0:m]), reads=['PC'],
                     writes=[dstname])

        def rsqrt_col(dst, src, n, scale, cols=1, name_d=None, name_s=None):
            S.op('act', lambda e: e.activation(out=dst, in_=src, func=AF.Sqrt, scale=scale, bias=EPSB[:, 0:1]),
                 reads=[name_s, 'EPSB'], writes=[name_d])
            S.op('dve', lambda e: e.reciprocal(out=dst, in_=dst), reads=[name_d], writes=[name_d])

        EPSB = sb("EPSB", [128, 1])
        S.op('dve', lambda e: e.memset(EPSB[:], EPS), writes=['EPSB'])
        dump(CST[:, 0:512], 'CST', 0, 512)
        if debug:
            MARK2 = sb("MARK2", [128, 128])
            S.op('dve', lambda e: e.memset(MARK2[:], float(-upto)), writes=['MARK2'])
            dump(MARK2[:], 'MARK2', 3840, 128)
        chk(1)

        import os as _os2
        with ExitStack() as es0:
            def sb0(name, shape, dt=F32):
                return (es if _os2.environ.get('NOSCOPE') else es0).enter_context(nc.sbuf_tensor(name, shape, dt))
            CTt = sb0("CTt", [128, 8, 3])
            LT = sb0("LT", [128, 3, 8, 128], BF16)
            SCt = sb0("SCt", [128, 8, 3])
            WA = [sb0(f"WA{i}", [128, 8, 512], BF16) for i in range(2)]
            BAR = [sb0(f"BAR{i}", [1, 512]) for i in range(2)]
            MT = sb0("MT", [128, 3, D])
            NG = sb0("NG", [128, 2, D])
            LR = sb0("LR", [1, 4 * W])
            LR2 = sb0("LR2", [1, 4 * W])
            TMPB = sb0("TMPB", [128, D])

            S.dma('sp', lambda e: e.dma_start(out=LR[:], in_=lbl_d), writes=['LR'])
            for d in range(2):
                S.op('dve', lambda e: e.tensor_tensor(out=LR2[0:1, d * 2 * W:d * 2 * W + W],
                                                      in0=LR[0:1, d * 2 * W:d * 2 * W + W],
                                                      in1=LR[0:1, d * 2 * W + W:d * 2 * W + 2 * W], op=ALU.subtract),
                     reads=['LR'], writes=['LR2'])
                S.op('act', lambda e: e.activation(out=LR2[0:1, d * 2 * W:d * 2 * W + W],
                                                   in_=LR2[0:1, d * 2 * W:d * 2 * W + W], func=AF.Sigmoid),
                     reads=['LR2'], writes=['LR2'])
                S.op('dve', lambda e: e.tensor_scalar(out=LR2[0:1, d * 2 * W + W:d * 2 * W + 2 * W],
                                                      in0=LR2[0:1, d * 2 * W:d * 2 * W + W], scalar1=-1.0, scalar2=1.0,
                                                      op0=ALU.mult, op1=ALU.add), reads=['LR2'], writes=['LR2'])
            for q in range(4):
                S.op('pe', lambda e: e.matmul(PC[:, :], lhsT=C('ONES')[0:1, :], rhs=LR2[0:1, q * W:(q + 1) * W],
                                              start=True, stop=True), reads=['LR2', 'CST'], writes=['PC'])
                S.op('dve', lambda e: e.tensor_copy(out=LBB[:, q, :], in_=PC[:, :]), reads=['PC'], writes=['LBB'])
            chk(2)
            bcast_row(GNB, W, gn4_d, 'GNB')
            bcast_row(RBB, 36, rb_d, 'RBB')
            bcast_row(NG[:, 0, :], D, n1g_d, 'NG')
            bcast_row(NG[:, 1, :], D, n2g_d, 'NG')

            chk(3)
            S.dma('sp', lambda e: e.dma_start(out=CTt[:], in_=ct_d), writes=['CTt'])
            S.op('act', lambda e: e.activation(out=SCt[:], in_=CTt[:], func=AF.Silu), reads=['CTt'], writes=['SCt'])
            for b in range(3):
                for k in range(8):
                    S.op('dve', lambda e: e.tensor_scalar(out=LT[:, b, k, :], in0=C('ONES'), scalar1=SCt[:, k, b:b + 1],
                                                          scalar2=None, op0=ALU.mult), reads=['SCt', 'CST'],
                         writes=['LT'])

            chk(4)

            def bc_to_cols(dst, src_bc, srcname, dstname):
                for k in range(8):
                    S.op('pe', lambda e: e.transpose(out=PS[:, k * 128:(k + 1) * 128], in_=src_bc[:, k * 128:(k + 1) * 128],
                                                     identity=C('IDF')), reads=[srcname, 'CST'], writes=['PS'])
                S.op('dve', lambda e: e.tensor_copy(out=dst, in_=PS[:, 0:1024:128]), reads=['PS'], writes=[dstname])

            for m in range(6):
                nb_needed = 3 if m < 2 else 2
                for jj in range(2):
                    j = 2 * m + jj
                    wa = WA[j % 2]
                    ba = BAR[j % 2]
                    S.dma('pool', lambda e: e.dma_start(out=wa[:], in_=wada_d[:, j * 512:(j + 1) * 512]
                                                        .rearrange("(k p) n -> p k n", p=128)), writes=[f'WA{j % 2}'])
                    S.dma('sp', lambda e: e.dma_start(out=ba[:], in_=bada_d[:, j * 512:(j + 1) * 512]),
                          writes=[f'BAR{j % 2}'])
                    for b in range(nb_needed):
                        pz = PA if (b % 2 == 0) else PB
                        pzn = 'PA' if (b % 2 == 0) else 'PB'
                        for k in range(8):
                            S.op('pe', lambda e: e.matmul(pz[:, :], lhsT=LT[:, b, k, :], rhs=wa[:, k, :],
                                                          start=(k == 0), stop=False),
                                 reads=['LT', f'WA{j % 2}'], writes=[pzn])
                        S.op('pe', lambda e: e.matmul(pz[:, :], lhsT=C('ONES')[0:1, :], rhs=ba[0:1, :],
                                                      start=False, stop=True), reads=['CST', f'BAR{j % 2}'],
                             writes=[pzn])
                        S.op('act', lambda e: e.activation(out=MT[:, b, jj * 512:(jj + 1) * 512], in_=pz[:, :],
                                                           func=AF.Identity), reads=[pzn], writes=[f'MT{b}'])
                if m == 0:
                    chk(5)
                for b in range(nb_needed):
                    if m == 0:
                        bc_to_cols(S1T[:, b, :], MT[:, b, :], f'MT{b}', 'S1T')
                        chk(6)
                    elif m == 1:
                        S.op('dve', lambda e: e.scalar_tensor_tensor(out=TMPB[:], in0=MT[:, b, :], scalar=1.0,
                                                                     in1=NG[:, 0, :], op0=ALU.add, op1=ALU.mult),
                             reads=[f'MT{b}', 'NG'], writes=['TMPB'])
                        bc_to_cols(A1T[:, b, :], TMPB, 'TMPB', 'A1T')
                    elif m == 4:
                        S.op('dve', lambda e: e.scalar_tensor_tensor(out=TMPB[:], in0=MT[:, b, :], scalar=1.0,
                                                                     in1=NG[:, 1, :], op0=ALU.add, op1=ALU.mult),
                             reads=[f'MT{b}', 'NG'], writes=['TMPB'])
                        S.dma('sp', lambda e: e.dma_start(out=modbc_d[b, 1], in_=TMPB[:]), reads=['TMPB'],
                              writes=[f'modbc{b}.1'])
                    else:
                        slot = {2: 0, 3: 2, 5: 3}[m]
                        S.dma('sp', lambda e: e.dma_start(out=modbc_d[b, slot], in_=MT[:, b, :]), reads=[f'MT{b}'],
                              writes=[f'modbc{b}.{slot}'])
                chk(60 + m)
            chk(67)
            S.barrier()
            chk(66)
        if debug and upto == 0:
            dump(LBB[:, 0, :], 'LBB', 512, 512)
            dump(A1T[:].rearrange("p b k -> p (b k)"), 'A1T', 1024, 24)
            dump(S1T[:].rearrange("p b k -> p (b k)"), 'S1T', 1056, 24)
            dump(GNB[:], 'GNB', 1536, 512)
            S.barrier()
            return nc
        chk(69)
        with ExitStack() as es1:
            chk(68)
            def sb1(name, shape, dt=F32):
                return es1.enter_context(nc.sbuf_tensor(name, shape, dt))
            UP = sb1("UP", [128, 4, 2, 96], BF16)
            X1 = sb1("X1", [128, D])
            RT = sb1("RT", [128, 128])
            WIN = sb1("WIN", [128, 8, NCOL], BF16)
            WOUT = sb1("WOUT", [128, 8, D], BF16)
            import os as _os
            DG = sb1("DG", [128, int(_os.environ.get("DGN", "124")), 128], BF16)
            SBS = sb1("SBS", [128, T, 4, 128], BF16)
            MBC = sb1("MBC", [128, 3, D])
            XT = sb1("XT", [128, D])
            XN = sb1("XN", [128, D], BF16)
            HXT = sb1("HXT", [128, 8, 128], BF16)
            SSc = sb1("SSc", [128, 8])
            T1 = sb1("T1", [128, W])
            T2 = sb1("T2", [128, W])
            T3 = sb1("T3", [128, W])
            LGF = sb1("LGF", [128, W])
            KK = sb1("KK", [128, W])
            SQ = sb1("SQ", [128, W])
            V = sb1("V", [128, W], BF16)
            KST = sb1("KST", [128, W], BF16)
            QK = sb1("QK", [128, 2, 2, W], BF16)
            QKT = sb1("QKT", [128, 2, 2, 4, 128], BF16)
            EV = sb1("EV", [128, 2, 16])
            SST = sb1("SST", [128, 2, W])
            SFS = sb1("SFS", [128, 4, 128], BF16)
            MIX = sb1("MIX", [128, W], BF16)
            U = sb1("U", [128, W], BF16)
            SGG = T2
            PM = QK[:, 0].rearrange("p a (h t) -> p a h t", h=4)
            MIXT = HXT
            CVS = LGF[:].rearrange("p (c t) -> p c t", c=4)
            SQC = T1[:].rearrange("p (c t) -> p c t", c=4)
            ST = T3[:].rearrange("p (c t) -> p c t", c=4)
            HX2 = XT
            HX2B = XN
            HX2T = X1[:].rearrange("p (k t) -> p k t", k=8)
            S.alias.update({'SGG': 'T2', 'PM0': 'QK0', 'PM1': 'QK0', 'MIXTa': 'HXT', 'MIXTb': 'HXT', 'CVS': 'LGF',
                            'SQC': 'T1', 'ST': 'T3', 'HX2': 'XT', 'HX2B': 'XN', 'HX2T': 'X1'})

            chk(70)
            for g in range(7):
                if g == 1:
                    chk(71)
                S.dma('pool', lambda e: e.dma_start(out=WIN[:, :, g * 512:(g + 1) * 512],
                                                    in_=win_d[:, g * 512:(g + 1) * 512]
                                                    .rearrange("(k p) n -> p k n", p=128)), writes=[f'WIN{g}'])
            chk(7)
            for g in range(2):
                S.dma('pool', lambda e: e.dma_start(out=WOUT[:, :, g * 512:(g + 1) * 512],
                                                    in_=wout_d[:, g * 512:(g + 1) * 512]
                                                    .rearrange("(k p) n -> p k n", p=128)), writes=['WOUT'])
            chk(8)
            for i in range(124):
                S.op('dve', lambda e: e.tensor_scalar(out=DG[:, i, :], in0=C('IDF'),
                                                      scalar1=SV[:, SV_DWK + i:SV_DWK + i + 1], scalar2=None,
                                                      op0=ALU.mult), reads=['CST', 'SV'], writes=['DG'])
            chk(9)
            S.op('dve', lambda e: e.memset(UP[:], 0.0), writes=['UP'])

            chk(10)
            GCOL = {'ff': 0, 'fb': 512, 'v': 1024, 'q': 1536, 'g': 2048, 'a': 2560, 'gt': 3072}

            def front(src_ap, mi):
                S.dma('sp', lambda e: e.dma_start(out=XT[:], in_=src_ap), writes=['XT'])
                S.op('act', lambda e: e.activation(out=XN[:], in_=XT[:], func=AF.Square, accum_out=SSc[:, 0:1]),
                     reads=['XT'], writes=['XN', 'SSc'])
                rsqrt_col(SSc[:, 1:2], SSc[:, 0:1], 1, 1.0 / D, name_d='SSc1', name_s='SSc')
                S.op('act', lambda e: e.activation(out=XN[:], in_=XT[:], func=AF.Identity, scale=SSc[:, 1:2]),
                     reads=['XT', 'SSc1'], writes=['XN'])
                for k in range(8):
                    S.op('pe', lambda e: e.transpose(out=PT[:, k * 128:(k + 1) * 128], in_=XN[:, k * 128:(k + 1) * 128],
                                                     identity=IDB[:]), reads=['XN', 'IDB'], writes=['PT'])
                for k in range(8):
                    if k % 2 == 0:
                        S.op('dve', lambda e: e.tensor_scalar(out=HXT[:, k, :], in0=PT[:, k * 128:(k + 1) * 128],
                                                              scalar1=A1T[:, mi, k:k + 1], scalar2=S1T[:, mi, k:k + 1],
                                                              op0=ALU.mult, op1=ALU.add),
                             reads=['PT', 'A1T', 'S1T'], writes=['HXT'])
                    else:
                        S.op('act', lambda e: e.activation(out=HXT[:, k, :], in_=PT[:, k * 128:(k + 1) * 128],
                                                           func=AF.Identity, scale=A1T[:, mi, k:k + 1],
                                                           bias=S1T[:, mi, k:k + 1]),
                             reads=['PT', 'A1T', 'S1T'], writes=['HXT'])

            def zgroup(gname, pz, pzn):
                c0 = GCOL[gname]
                gi = c0 // 512
                for k in range(8):
                    S.op('pe', lambda e: e.matmul(pz[:, :], lhsT=HXT[:, k, :], rhs=WIN[:, k, c0:c0 + 512],
                                                  start=(k == 0), stop=(k == 7)), reads=['HXT', f'WIN{gi}'],
                         writes=[pzn])

            def fprep(d, pz, pzn, full, state):
                S.op('act', lambda e: e.activation(out=T1[:], in_=pz[:, :], func=AF.Sigmoid), reads=[pzn], writes=['T1'])
                S.op('dve', lambda e: e.tensor_tensor(out=T1[:], in0=T1[:], in1=LBB[:, 2 * d + 1, :], op=ALU.mult),
                     reads=['T1', 'LBB'], writes=['T1'])
                S.op('dve', lambda e: e.tensor_tensor(out=T1[:], in0=T1[:], in1=LBB[:, 2 * d, :], op=ALU.add),
                     reads=['T1', 'LBB'], writes=['T1'])
                S.op('act', lambda e: e.activation(out=LGF[:], in_=T1[:], func=AF.Ln), reads=['T1'], writes=['LGF'])
                S.op('dve', lambda e: e.tensor_scalar(out=KK[:], in0=T1[:], scalar1=-1.0, scalar2=1.0, op0=ALU.mult,
                                                      op1=ALU.add), reads=['T1'], writes=['KK'])
                if state:
                    S.op('pe', lambda e: e.matmul(PC[:, :], lhsT=C('RF' if d == 0 else 'RB'), rhs=LGF[:],
                                                  start=True, stop=True), reads=['LGF', 'CST'], writes=['PC'])
                    S.op('act', lambda e: e.activation(out=T2[:], in_=PC[:, :], func=AF.Exp), reads=['PC'],
                         writes=['T2'])
                    S.op('dve', lambda e: e.tensor_tensor(out=KST[:], in0=KK[:], in1=T2[:], op=ALU.mult),
                         reads=['KK', 'T2'], writes=['KST'])
                for h in range(4):
                    S.op('pe', lambda e: e.matmul(PD[:, 4 * h:4 * h + 4], lhsT=LGF[:, h * 128:(h + 1) * 128],
                                                  rhs=C('CV'), start=True, stop=True), reads=['LGF', 'CST'],
                         writes=['PD'])
                S.op('act', lambda e: e.activation(out=EV[:, d, :], in_=PD[:, 0:16], func=AF.Exp), reads=['PD'],
                     writes=[f'EV{d}'])
                if full:
                    S.op('pe', lambda e: e.matmul(PC[:, :], lhsT=C('MF' if d == 0 else 'MB'), rhs=LGF[:],
                                                  start=True, stop=True), reads=['LGF', 'CST'], writes=['PC'])
                    sq, sk = (1.0, -1.0) if d == 0 else (-1.0, 1.0)
                    S.op('act', lambda e: e.activation(out=T2[:], in_=PC[:, :], func=AF.Exp, scale=sq), reads=['PC'],
                         writes=['T2'])
                    S.op('act', lambda e: e.activation(out=T3[:], in_=PC[:, :], func=AF.Exp, scale=sk), reads=['PC'],
                         writes=['T3'])
                    S.op('dve', lambda e: e.tensor_tensor(out=QK[:, d, 0, :], in0=SQ[:], in1=T2[:], op=ALU.mult),
                         reads=['SQ', 'T2'], writes=[f'QK{d}'])
                    S.op('dve', lambda e: e.tensor_tensor(out=QK[:, d, 1, :], in0=KK[:], in1=T3[:], op=ALU.mult),
                         reads=['KK', 'T3'], writes=[f'QK{d}'])

            def state_update(d):
                for h in range(4):
                    S.op('pe', lambda e: e.matmul(PC[:, h * 128:(h + 1) * 128], lhsT=KST[:, h * 128:(h + 1) * 128],
                                                  rhs=V[:, h * 128:(h + 1) * 128], start=True, stop=True),
                         reads=['KST', 'V'], writes=['PC'])
                for h in range(4):
                    S.op('dve', lambda e: e.scalar_tensor_tensor(out=SST[:, d, h * 128:(h + 1) * 128],
                                                                 in0=SST[:, d, h * 128:(h + 1) * 128],
                                                                 scalar=EV[:, d, 4 * h:4 * h + 1],
                                                                 in1=PC[:, h * 128:(h + 1) * 128], op0=ALU.mult,
                                                                 op1=ALU.add),
                         reads=[f'SST{d}', f'EV{d}', 'PC'], writes=[f'SST{d}'])

            def state_tile(src_ap, mi, d):
                front(src_ap, mi)
                zgroup('v', PB, 'PB')
                S.op('act', lambda e: e.activation(out=V[:], in_=PB[:, :], func=AF.Identity), reads=['PB'], writes=['V'])
                zgroup('ff' if d == 0 else 'fb', PA, 'PA')
                fprep(d, PA, 'PA', full=False, state=True)

            def route(t):
                LG, GM, PEN, EL, EL2 = RT[:, 0:36], RT[:, 36:40], RT[:, 40:44], RT[:, 44:76], RT[:, 76:108]
                sc = RT[:, 108:128]
                S.op('dve', lambda e: e.tensor_tensor(out=LG, in0=PO[:, 0:36], in1=RBB[:], op=ALU.add),
                     reads=['PO', 'RBB'], writes=['RT'])
                S.op('dve', lambda e: e.reduce_max(out=sc[:, 0:1], in_=RT[:, 0:4], axis=AX.X), reads=['RT'], writes=['RT'])
                S.op('dve', lambda e: e.tensor_scalar(out=GM, in0=RT[:, 0:4], scalar1=sc[:, 0:1], scalar2=None,
                                                      op0=ALU.is_equal), reads=['RT'], writes=['RT'])
                S.op('dve', lambda e: e.tensor_scalar(out=sc[:, 1:2], in0=sc[:, 0:1], scalar1=-1.0, scalar2=None,
                                                      op0=ALU.mult), reads=['RT'], writes=['RT'])
                S.op('act', lambda e: e.activation(out=sc[:, 4:8], in_=RT[:, 0:4], func=AF.Exp, bias=sc[:, 1:2],
                                                   accum_out=sc[:, 2:3]), reads=['RT'], writes=['RT'])
                S.op('dve', lambda e: e.reciprocal(out=sc[:, 3:4], in_=sc[:, 2:3]), reads=['RT'], writes=['RT'])
                S.op('dve', lambda e: e.tensor_scalar(out=PEN, in0=GM, scalar1=-1.0, scalar2=1e30, op0=ALU.add,
                                                      op1=ALU.mult), reads=['RT'], writes=['RT'])
                for g in range(4):
                    S.op('dve', lambda e: e.tensor_scalar(out=RT[:, 44 + 8 * g:52 + 8 * g], in0=RT[:, 4 + 8 * g:12 + 8 * g],
                                                          scalar1=RT[:, 36 + g:37 + g], scalar2=RT[:, 40 + g:41 + g],
                                                          op0=ALU.mult, op1=ALU.add), reads=['RT'], writes=['RT'])
                S.op('dve', lambda e: e.reduce_max(out=sc[:, 8:9], in_=EL, axis=AX.X), reads=['RT'], writes=['RT'])
                S.op('dve', lambda e: e.tensor_scalar(out=SELS[:, t, 0:32], in0=EL, scalar1=sc[:, 8:9], scalar2=None,
                                                      op0=ALU.is_equal), reads=['RT'], writes=['SELS'])
                S.op('dve', lambda e: e.scalar_tensor_tensor(out=EL2, in0=SELS[:, t, 0:32], scalar=-1e30, in1=EL,
                                                             op0=ALU.mult, op1=ALU.add), reads=['RT', 'SELS'],
                     writes=['RT'])
                S.op('dve', lambda e: e.reduce_max(out=sc[:, 9:10], in_=EL2, axis=AX.X), reads=['RT'], writes=['RT'])
                S.op('dve', lambda e: e.tensor_scalar(out=SELS[:, t, 32:64], in0=EL2, scalar1=sc[:, 9:10], scalar2=None,
                                                      op0=ALU.is_equal), reads=['RT'], writes=['SELS'])
                S.op('dve', lambda e: e.tensor_tensor(out=sc[:, 10:11], in0=sc[:, 9:10], in1=sc[:, 8:9], op=ALU.subtract),
                     reads=['RT'], writes=['RT'])
                S.op('act', lambda e: e.activation(out=sc[:, 11:12], in_=sc[:, 10:11], func=AF.Exp), reads=['RT'],
                     writes=['RT'])
                S.op('dve', lambda e: e.tensor_scalar(out=sc[:, 12:13], in0=sc[:, 11:12], scalar1=1.0, scalar2=None,
                                                      op0=ALU.add), reads=['RT'], writes=['RT'])
                S.op('dve', lambda e: e.reciprocal(out=sc[:, 13:14], in_=sc[:, 12:13]), reads=['RT'], writes=['RT'])
                S.op('dve', lambda e: e.tensor_tensor(out=WTS[:, t, 0:1], in0=sc[:, 13:14], in1=sc[:, 3:4], op=ALU.mult),
                     reads=['RT'], writes=['WTS'])
                S.op('dve', lambda e: e.tensor_tensor(out=WTS[:, t, 1:2], in0=WTS[:, t, 0:1], in1=sc[:, 11:12],
                                                      op=ALU.mult), reads=['RT', 'WTS'], writes=['WTS'])

            pending_route = []

            def full_tile(b, i):
                t = b * T + i
                front(x_d[b, i * 128:(i + 1) * 128, :], b)
                zgroup('q', PA, 'PA')
                zgroup('v', PB, 'PB')
                S.op('act', lambda e: e.activation(out=SQ[:], in_=PA[:, :], func=AF.Silu), reads=['PA'], writes=['SQ'])
                S.op('act', lambda e: e.activation(out=V[:], in_=PB[:, :], func=AF.Identity), reads=['PB'], writes=['V'])
                zgroup('fb', PA, 'PA')
                zgroup('ff', PB, 'PB')
                while pending_route:
                    route(pending_route.pop(0))
                fprep(1, PA, 'PA', full=True, state=False)
                zgroup('g', PA, 'PA')
                fprep(0, PB, 'PB', full=True, state=True)
                zgroup('gt', PB, 'PB')
                S.op('act', lambda e: e.activation(out=T1[:], in_=PA[:, :], func=AF.Silu), reads=['PA'], writes=['T1'])
                S.op('dve', lambda e: e.tensor_tensor(out=SGG[:], in0=T1[:], in1=GNB[:], op=ALU.mult),
                     reads=['T1', 'GNB'], writes=['SGG'])
                zgroup('a', PA, 'PA')
                S.op('act', lambda e: e.activation(out=T1[:], in_=PB[:, :], func=AF.Sigmoid), reads=['PB'], writes=['T1'])
                S.op('dve', lambda e: e.tensor_tensor(out=U[:], in0=PA[:, :], in1=T1[:], op=ALU.mult),
                     reads=['PA', 'T1'], writes=['U'])
                chk(20)
                for d in range(2):
                    for qk in range(2):
                        for h in range(4):
                            S.op('pe', lambda e: e.transpose(out=PT[:, (qk * 4 + h) * 128:(qk * 4 + h + 1) * 128],
                                                             in_=QK[:, d, qk, h * 128:(h + 1) * 128], identity=IDB[:]),
                                 reads=[f'QK{d}', 'IDB'], writes=['PT'])
                    eng = 'act' if d == 0 else 'dve'
                    if eng == 'act':
                        S.op('act', lambda e: e.activation(out=QKT[:, d].rearrange("p a h t -> p (a h t)"), in_=PT[:, :],
                                                           func=AF.Identity), reads=['PT'], writes=[f'QKT{d}'])
                    else:
                        S.op('dve', lambda e: e.tensor_copy(out=QKT[:, d].rearrange("p a h t -> p (a h t)"), in_=PT[:, :]),
                             reads=['PT'], writes=[f'QKT{d}'])
                for d in range(2):
                    for h in range(4):
                        S.op('pe', lambda e: e.matmul(PS[:, (d * 4 + h) * 128:(d * 4 + h + 1) * 128],
                                                      lhsT=QKT[:, d, 1, h, :], rhs=QKT[:, d, 0, h, :], start=True, stop=True),
                             reads=[f'QKT{d}'], writes=['PS'])
                for d in range(2):
                    S.op('dve', lambda e: e.tensor_tensor(out=PM[:, d].rearrange("p h t -> p (h t)"),
                                                          in0=PS[:, d * 512:(d + 1) * 512],
                                                          in1=C('MASKF' if d == 0 else 'MASKB'), op=ALU.mult),
                         reads=['PS', 'CST'], writes=[f'PM{d}'])
                chk(21)
                for h in range(4):
                    S.op('act', lambda e: e.activation(out=SFS[:, h, :], in_=SST[:, 0, h * 128:(h + 1) * 128],
                                                       func=AF.Identity, scale=EV[:, 0, 4 * h + 1:4 * h + 2]),
                         reads=['SST0', 'EV0'], writes=['SFS'])
                for h in range(4):
                    hs = slice(h * 128, (h + 1) * 128)
                    S.op('pe', lambda e: e.matmul(PO[:, hs], lhsT=PM[:, 0, h, :], rhs=V[:, hs], start=True, stop=False),
                         reads=['PM0', 'V'], writes=['PO'])
                    S.op('pe', lambda e: e.matmul(PO[:, hs], lhsT=PM[:, 1, h, :], rhs=V[:, hs], start=False, stop=False),
                         reads=['PM1', 'V'], writes=['PO'])
                    S.op('pe', lambda e: e.matmul(PO[:, hs], lhsT=QKT[:, 0, 0, h, :], rhs=SFS[:, h, :], start=False,
                                                  stop=False), reads=['QKT0', 'SFS'], writes=['PO'])
                    S.op('pe', lambda e: e.matmul(PO[:, hs], lhsT=QKT[:, 1, 0, h, :], rhs=SBS[:, i, h, :], start=False,
                                                  stop=True), reads=['QKT1', 'SBS'], writes=['PO'])
                for h in range(4):
                    S.op('act', lambda e: e.activation(out=T3[:, h * 128:(h + 1) * 128], in_=PO[:, h * 128:(h + 1) * 128],
                                                       func=AF.Square, accum_out=SSc[:, 2 + h:3 + h]),
                         reads=['PO'], writes=['T3', 'SSh'])
                S.op('act', lambda e: e.activation(out=SSc[:, 2:6], in_=SSc[:, 2:6], func=AF.Sqrt, scale=1.0 / 128,
                                                   bias=EPSB[:, 0:1]), reads=['SSh', 'EPSB'], writes=['SSh'])
                S.op('dve', lambda e: e.reciprocal(out=SSc[:, 2:6], in_=SSc[:, 2:6]), reads=['SSh'], writes=['SSh'])
                for h in range(4):
                    hs = slice(h * 128, (h + 1) * 128)
                    S.op('dve', lambda e: e.scalar_tensor_tensor(out=MIX[:, hs], in0=PO[:, hs], scalar=SSc[:, 2 + h:3 + h],
                                                                 in1=SGG[:, hs], op0=ALU.mult, op1=ALU.mult),
                         reads=['PO', 'SSh', 'SGG'], writes=['MIX'])
                state_update(0)
                if t == 0 and upto == -22:
                    dump_bf(MIX[:], 'MIX', 0, 512)
                    dump(PO[:, :] if False else SSc[:, 0:8], 'SSh', 600, 8)
                chk(22)
                for h in range(4):
                    S.op('pe', lambda e: e.transpose(out=PT[:, h * 128:(h + 1) * 128], in_=MIX[:, h * 128:(h + 1) * 128],
                                                     identity=IDB[:]), reads=['MIX', 'IDB'], writes=['PT'])
                for c in range(4):
                    S.op('pe', lambda e: e.transpose(out=PT[:, (4 + c) * 128:(5 + c) * 128], in_=U[:, c * 128:(c + 1) * 128],
                                                     identity=IDB[:]), reads=['U', 'IDB'], writes=['PT'])
                chk(28)
                S.op('act', lambda e: e.activation(out=MIXT[:, 0:4, :].rearrange("p k t -> p (k t)"), in_=PT[:, 0:512],
                                                   func=AF.Identity), reads=['PT'], writes=['MIXTa'])
                chk(29)
                if upto == -31:
                    S.op('dve', lambda e: e.tensor_copy(out=U[:, 0:64], in_=PT[:, 512:576]), reads=['PT'], writes=['U'])
                    chk(31)
                if upto == -33:
                    S.op('dve', lambda e: e.tensor_copy(out=U[:, 0:128], in_=PT[:, 512:640]), reads=['PT'], writes=['U'])
                    chk(33)
                if upto == -34:
                    S.op('dve', lambda e: e.tensor_copy(out=U[:, 0:64], in_=PT[:, 0:64]), reads=['PT'], writes=['U'])
                    chk(34)
                if upto == -35:
                    S.op('act', lambda e: e.activation(out=U[:, 0:64], in_=PT[:, 0:64], func=AF.Identity), reads=['PT'], writes=['U'])
                    chk(35)
                if upto == -36:
                    S.op('dve', lambda e: e.tensor_copy(out=MIX[:, 0:512], in_=PT[:, 0:512]), reads=['PT'], writes=['MIX'])
                    chk(36)
                if upto == -37:
                    S.op('dve', lambda e: e.tensor_copy(out=MIX[:, 0:512], in_=PT[:, 0:512]), reads=['PT', 'MIXTa'], writes=['MIX'])
                    chk(37)
                if upto == -32:
                    S.op('dve', lambda e: e.tensor_copy(out=UP[:, 0, 0, 16:80], in_=MIX[:, 0:64]), reads=['MIX'], writes=['UP'])
                    chk(32)
                for c in range(4):
                    for r in range(2):
                        src = PT[:, (4 + c) * 128 + r * 64:(4 + c) * 128 + (r + 1) * 64]
                        if (c + r) % 2 == 0:
                            S.op('dve', lambda e: e.tensor_copy(out=UP[:, c, r, 16:80], in_=src), reads=['PT'], writes=['UP'])
                        else:
                            S.op('act', lambda e: e.activation(out=UP[:, c, r, 16:80], in_=src, func=AF.Identity),
                                 reads=['PT'], writes=['UP'])
                chk(27)
                for c in range(4):
                    for k in range(31):
                        S.op('pe', lambda e: e.matmul(PD[:, c * 128:(c + 1) * 128].rearrange("p (r t) -> p r t", r=2), lhsT=DG[:, c * 31 + k, :],
                                                      rhs=UP[:, c, :, k + 1:k + 65], start=(k == 0), stop=(k == 30)),
                             reads=['DG', 'UP'], writes=['PD'])
                chk(23)
                for c in range(4):
                    S.op('act', lambda e: e.activation(out=CVS[:, c, :], in_=PD[:, c * 128:(c + 1) * 128], func=AF.Identity,
                                                       bias=SV[:, SV_DWB + c:SV_DWB + c + 1]), reads=['PD', 'SV'],
                         writes=['CVS'])
                    S.op('dve', lambda e: e.tensor_tensor(out=SQC[:, c, :], in0=CVS[:, c, :], in1=CVS[:, c, :], op=ALU.mult),
                         reads=['CVS'], writes=['SQC'])
                for c in range(4):
                    S.op('pe', lambda e: e.matmul(PC[:, 0:128], lhsT=C('ONES'), rhs=CVS[:, c, :], start=(c == 0),
                                                  stop=(c == 3)), reads=['CVS', 'CST'], writes=['PC'])
                for c in range(4):
                    S.op('pe', lambda e: e.matmul(PC[:, 128:256], lhsT=C('ONES'), rhs=SQC[:, c, :], start=(c == 0),
                                                  stop=(c == 3)), reads=['SQC', 'CST'], writes=['PC'])
                S.op('dve', lambda e: e.tensor_scalar(out=ST[:, 0, :], in0=PC[:, 0:128], scalar1=1.0 / W, scalar2=None,
                                                      op0=ALU.mult), reads=['PC'], writes=['ST'])
                S.op('dve', lambda e: e.tensor_tensor(out=ST[:, 1, :], in0=ST[:, 0, :], in1=ST[:, 0, :], op=ALU.mult),
                     reads=['ST'], writes=['ST'])
                S.op('dve', lambda e: e.scalar_tensor_tensor(out=ST[:, 2, :], in0=PC[:, 128:256], scalar=1.0 / W,
                                                             in1=ST[:, 1, :], op0=ALU.mult, op1=ALU.subtract),
                     reads=['PC', 'ST'], writes=['ST'])
                S.op('act', lambda e: e.activation(out=ST[:, 2, :], in_=ST[:, 2, :], func=AF.Sqrt, bias=EPSB[:, 0:1]),
                     reads=['ST', 'EPSB'], writes=['ST'])
                S.op('dve', lambda e: e.reciprocal(out=ST[:, 2, :], in_=ST[:, 2, :]), reads=['ST'], writes=['ST'])
                for c in range(4):
                    S.op('dve', lambda e: e.tensor_tensor(out=CVS[:, c, :], in0=CVS[:, c, :], in1=ST[:, 0, :],
                                                          op=ALU.subtract), reads=['CVS', 'ST'], writes=['CVS'])
                    S.op('dve', lambda e: e.tensor_tensor(out=CVS[:, c, :], in0=CVS[:, c, :], in1=ST[:, 2, :],
                                                          op=ALU.mult), reads=['CVS', 'ST'], writes=['CVS'])
                    S.op('dve', lambda e: e.tensor_scalar(out=CVS[:, c, :], in0=CVS[:, c, :],
                                                          scalar1=SV[:, SV_LNG + c:SV_LNG + c + 1],
                                                          scalar2=SV[:, SV_LNB + c:SV_LNB + c + 1], op0=ALU.mult,
                                                          op1=ALU.add), reads=['CVS', 'SV'], writes=['CVS'])
                    S.op('act', lambda e: e.activation(out=MIXT[:, 4 + c, :], in_=CVS[:, c, :], func=AF.Silu),
                         reads=['CVS'], writes=['MIXTb'])
                chk(24)
                for hf in range(2):
                    for k in range(8):
                        S.op('pe', lambda e: e.matmul(PS[:, hf * 512:(hf + 1) * 512], lhsT=MIXT[:, k, :],
                                                      rhs=WOUT[:, k, hf * 512:(hf + 1) * 512], start=(k == 0), stop=(k == 7)),
                             reads=['MIXTa', 'MIXTb', 'WOUT'], writes=['PS'])
                S.op('dve', lambda e: e.tensor_tensor(out=X1[:], in0=PS[:, :], in1=MBC[:, 0, :], op=ALU.mult),
                     reads=['PS', 'MBC'], writes=['X1'])
                S.op('dve', lambda e: e.tensor_tensor(out=X1[:], in0=X1[:], in1=XT[:], op=ALU.add),
                     reads=['X1', 'XT'], writes=['X1'])
                S.dma('sp', lambda e: e.dma_start(out=x1_d[t * 128:(t + 1) * 128, :], in_=X1[:]), reads=['X1'],
                      writes=[f'x1d{t}'])
                if t == 0 and upto == -25:
                    dump(X1[:, 0:512], 'X1', 0, 512)
                    dump(XT[:, 0:512], 'XT', 512, 512)
                    dump(MBC[:, 0, 0:512], 'MBC', 1024, 512)
                    dump(SQ[:, 0:512], 'SQ', 1536, 512)
                chk(25)
                S.op('act', lambda e: e.activation(out=XN[:], in_=X1[:], func=AF.Square, accum_out=SSc[:, 6:7]),
                     reads=['X1'], writes=['XN', 'SS2'])
                rsqrt_col(SSc[:, 7:8], SSc[:, 6:7], 1, 1.0 / D, name_d='SS2b', name_s='SS2')
                S.op('dve', lambda e: e.scalar_tensor_tensor(out=HX2[:], in0=X1[:], scalar=SSc[:, 7:8], in1=MBC[:, 1, :],
                                                             op0=ALU.mult, op1=ALU.mult), reads=['X1', 'SS2b', 'MBC'],
                     writes=['HX2'])
                S.op('dve', lambda e: e.tensor_tensor(out=HX2[:], in0=HX2[:], in1=MBC[:, 2, :], op=ALU.add),
                     reads=['HX2', 'MBC'], writes=['HX2'])
                S.op('act', lambda e: e.activation(out=HX2B[:], in_=HX2[:], func=AF.Identity), reads=['HX2'],
                     writes=['HX2B'])
                S.dma('sp', lambda e: e.dma_start(out=hx2_d[t * 128:(t + 1) * 128, :], in_=HX2B[:]), reads=['HX2B'],
                      writes=[f'hx2d{t}'])
                chk(26)
                for k in range(8):
                    S.op('pe', lambda e: e.transpose(out=PS[:, k * 128:(k + 1) * 128], in_=HX2[:, k * 128:(k + 1) * 128],
                                                     identity=C('IDF')), reads=['HX2', 'CST'], writes=['PS'])
                S.op('act', lambda e: e.activation(out=HX2T[:].rearrange("p k t -> p (k t)"), in_=PS[:, :],
                                                   func=AF.Identity), reads=['PS'], writes=['HX2T'])
                for k in range(8):
                    S.op('pe', lambda e: e.matmul(PO[:, 0:36], lhsT=HX2T[:, k, :], rhs=WR[:, k, :], start=(k == 0),
                                                  stop=(k == 7)), reads=['HX2T', 'WR'], writes=['PO'])
                pending_route.append(t)

            for b in range(NB):
                for d in range(2):
                    S.op('dve', lambda e: e.memset(SST[:, d, :], 0.0), writes=[f'SST{d}'])
                    order = range(CT) if d == 0 else range(CT - 1, -1, -1)
                    for ci in order:
                        state_tile(ctx_d[b, ci * 128:(ci + 1) * 128, :], 2, d)
                        chk(11)
                        state_update(d)
                        chk(12)
                if b == 0 and upto == -15:
                    dump(SST[:, 0, :], 'SST0', 0, 512)
                    dump(SST[:, 1, :], 'SST1', 512, 512)
                    chk(15)
                for i in range(T - 1, -1, -1):
                    state_tile(x_d[b, i * 128:(i + 1) * 128, :], b, 1)
                    for h in range(4):
                        S.op('act', lambda e: e.activation(out=SBS[:, i, h, :], in_=SST[:, 1, h * 128:(h + 1) * 128],
                                                           func=AF.Identity, scale=EV[:, 1, 4 * h + 2:4 * h + 3]),
                             reads=['SST1', 'EV1'], writes=['SBS'])
                    state_update(1)
                chk(13)
                chk(200 + b)
                for s in range(3):
                    S.dma('sp', lambda e: e.dma_start(out=MBC[:, s, :], in_=modbc_d[b, s]), reads=[f'modbc{b}.{s}'],
                          writes=['MBC'])
                for i in range(T):
                    full_tile(b, i)
                while pending_route:
                    route(pending_route.pop(0))
                    chk(14)
                    chk(100 + b * T + i)
            S.barrier()

        if debug and upto == 1:
            S.dma('sp', lambda e: e.dma_start(out=dbg_d[:, 2048:2048 + NT * 2], in_=WTS[:].rearrange("p t c -> p (t c)")),
                  reads=['WTS'], writes=['dbg'])
            S.barrier(['sp'])
            return nc

        with ExitStack() as es2:
            def sb2(name, shape, dt=F32):
                return es2.enter_context(nc.sbuf_tensor(name, shape, dt))
            SELT = sb2("SELT", [128, NT + 1, NE])
            CUM = sb2("CUM", [128, NT + 1, NE])
            RANK = sb2("RANK", [128, NT, NE])
            CN = sb2("CN", [128, 8, NE])
            CNI = sb2("CNI", [128, NE], I32)
            BE = sb2("BE", [128, 2, NBLK])
            POSF = sb2("POSF", [128, NT, 2])
            TMP = sb2("TMP", [128, NE])
            XB = sb2("XBs", [128, D], BF16)

            S.op('dve', lambda e: e.tensor_tensor(out=SELT[:, 0:NT, :], in0=SELS[:, :, 0:32], in1=SELS[:, :, 32:64],
                                                  op=ALU.add), reads=['SELS'], writes=['SELT'])
            S.op('dve', lambda e: e.memset(CUM[:, 0, :], 0.0), writes=['CUM'])
            for t in range(NT):
                S.op('dve', lambda e: e.tensor_tensor(out=CUM[:, t + 1, :], in0=CUM[:, t, :], in1=SELT[:, t, :], op=ALU.add),
                     reads=['CUM', 'SELT'], writes=['CUM'])
            for t in range(NT):
                S.op('pe', lambda e: e.matmul(PA[:, 0:NE], lhsT=C('RB'), rhs=SELT[:, t, :], start=True, stop=False),
                     reads=['SELT', 'CST'], writes=['PA'])
                S.op('pe', lambda e: e.matmul(PA[:, 0:NE], lhsT=C('ONES'), rhs=CUM[:, t, :], start=False, stop=True),
                     reads=['CUM', 'CST'], writes=['PA'])
                S.op('dve', lambda e: e.tensor_copy(out=RANK[:, t, :], in_=PA[:, 0:NE]), reads=['PA'], writes=['RANK'])
            S.op('pe', lambda e: e.matmul(PA[:, 0:NE], lhsT=C('ONES'), rhs=CUM[:, NT, :], start=True, stop=True),
                 reads=['CUM', 'CST'], writes=['PA'])
            S.op('dve', lambda e: e.tensor_scalar(out=CN[:, 0, :], in0=PA[:, 0:NE], scalar1=127.0, scalar2=None, op0=ALU.add),
                 reads=['PA'], writes=['CN'])
            S.op('dve', lambda e: e.tensor_copy(out=CNI[:], in_=CN[:, 0, :]), reads=['CN'], writes=['CNI'])
            S.op('dve', lambda e: e.tensor_single_scalar(out=CNI[:], in_=CNI[:], scalar=7, op=ALU.arith_shift_right),
                 reads=['CNI'], writes=['CNI'])
            S.op('dve', lambda e: e.tensor_copy(out=CN[:, 1, :], in_=CNI[:]), reads=['CNI'], writes=['CN'])
            S.op('dve', lambda e: e.tensor_copy(out=CN[:, 2, :], in_=CN[:, 1, :]), reads=['CN'], writes=['CN'])
            cur = 2
            for sh in (1, 2, 4, 8, 16):
                nxt = 5 - cur
                S.op('dve', lambda e: e.tensor_copy(out=CN[:, nxt, :], in_=CN[:, cur, :]), reads=['CN'], writes=['CN'])
                S.op('dve', lambda e: e.tensor_tensor(out=CN[:, nxt, sh:NE], in0=CN[:, cur, sh:NE], in1=CN[:, cur, 0:NE - sh],
                                                      op=ALU.add), reads=['CN'], writes=['CN'])
                cur = nxt
            PEND = CN[:, cur, :]
            S.op('dve', lambda e: e.tensor_tensor(out=CN[:, 4, :], in0=PEND, in1=CN[:, 1, :], op=ALU.subtract),
                 reads=['CN'], writes=['CN'])
            S.op('dve', lambda e: e.tensor_scalar(out=CN[:, 4, :], in0=CN[:, 4, :], scalar1=128.0, scalar2=None, op0=ALU.mult),
                 reads=['CN'], writes=['CN'])
            for t in range(NT):
                S.op('dve', lambda e: e.tensor_tensor(out=RANK[:, t, :], in0=RANK[:, t, :], in1=CN[:, 4, :], op=ALU.add),
                     reads=['RANK', 'CN'], writes=['RANK'])
                for j in range(2):
                    S.op('dve', lambda e: e.tensor_tensor(out=TMP[:], in0=RANK[:, t, :], in1=SELS[:, t, 32 * j:32 * j + 32],
                                                          op=ALU.mult), reads=['RANK', 'SELS'], writes=['TMP'])
                    S.op('dve', lambda e: e.reduce_sum(out=POSF[:, t, j:j + 1], in_=TMP[:], axis=AX.X), reads=['TMP'],
                         writes=['POSF'])
            S.op('dve', lambda e: e.tensor_copy(out=IDX[:].rearrange("p t c -> p (t c)"),
                                                in_=POSF[:].rearrange("p t c -> p (t c)")), reads=['POSF'], writes=['IDX'])
            S.op('dve', lambda e: e.memset(BE[:, 0, :], 0.0), writes=['BE'])
            for ex in range(NE):
                S.op('dve', lambda e: e.scalar_tensor_tensor(out=BE[:, 0, :], in0=C('IOTA', 0, NBLK),
                                                             scalar=CN[:, cur, ex:ex + 1], in1=BE[:, 0, :],
                                                             op0=ALU.is_ge, op1=ALU.add), reads=['CST', 'CN', 'BE'],
                     writes=['BE'])
            S.op('dve', lambda e: e.tensor_scalar(out=BE[:, 0, :], in0=BE[:, 0, :], scalar1=float(NE - 1), scalar2=None,
                                                  op0=ALU.min), reads=['BE'], writes=['BE'])
            S.op('dve', lambda e: e.memset(BE[:, 1, 0:1], 1.0), reads=[], writes=['BE1'])
            S.op('dve', lambda e: e.tensor_tensor(out=BE[:, 1, 1:NBLK], in0=BE[:, 0, 1:NBLK], in1=BE[:, 0, 0:NBLK - 1],
                                                  op=ALU.not_equal), reads=['BE', 'BE1'], writes=['BE1'])
            S.op('dve', lambda e: e.tensor_copy(out=BEI[:], in_=BE[:].rearrange("p a n -> p (a n)")), reads=['BE', 'BE1'],
                 writes=['BEI'])
            S.op('dve', lambda e: e.tensor_scalar(out=BE[:, 0, :], in0=BE[:, 0, :], scalar1=128.0, scalar2=C('PCOL', 0, 1),
                                                  op0=ALU.mult, op1=ALU.add), reads=['BE', 'CST', 'BEI'], writes=['BE'])
            S.op('dve', lambda e: e.tensor_scalar(out=BE[:, 0, :], in0=BE[:, 0, :], scalar1=-1.0e6, scalar2=None, op0=ALU.add),
                 reads=['BE'], writes=['BE'])
            S.op('dve', lambda e: e.tensor_tensor(out=BE[:, 0, :], in0=BE[:, 0, :], in1=BE[:, 1, :], op=ALU.mult),
                 reads=['BE', 'BE1'], writes=['BE'])
            S.op('dve', lambda e: e.tensor_scalar(out=BE[:, 0, :], in0=BE[:, 0, :], scalar1=1.0e6, scalar2=None, op0=ALU.add),
                 reads=['BE'], writes=['BE'])
            S.op('dve', lambda e: e.tensor_copy(out=IDXW[:], in_=BE[:, 0, :]), reads=['BE'], writes=['IDXW'])
            for t in range(NT):
                S.dma('sp', lambda e: e.dma_start(out=XB[:], in_=hx2_d[t * 128:(t + 1) * 128, :]), reads=[f'hx2d{t}'],
                      writes=['XBs'])
                for j in range(2):
                    S.dma('pool', lambda e: e.indirect_dma_start(out=xbuf_d, out_offset=bass.IndirectOffsetOnAxis(IDX[:, t, j:j + 1], 0),
                                                                 in_=XB[:], in_offset=None), reads=['XBs', 'IDX'],
                          writes=['xbuf'])
            S.barrier()

        if debug and upto == 2:
            S.dma('sp', lambda e: e.dma_start(out=dbgi_d[:, 0:NT * 2], in_=IDX[:].rearrange("p t c -> p (t c)")),
                  reads=['IDX'], writes=['dbgi'])
            S.dma('sp', lambda e: e.dma_start(out=dbgi_d[:, 256:256 + 2 * NBLK], in_=BEI[:]), reads=['BEI'], writes=['dbgi'])
            S.dma('sp', lambda e: e.dma_start(out=dbgi_d[:, 512:512 + NBLK], in_=IDXW[:]), reads=['IDXW'], writes=['dbgi'])
            S.barrier(['sp'])
            return nc

        with ExitStack() as es3:
            def sb3(name, shape, dt=F32):
                return es3.enter_context(nc.sbuf_tensor(name, shape, dt))
            WG = sb3("WG", [128, 8, W], BF16)
            WU = sb3("WU", [128, 8, W], BF16)
            WD = sb3("WD", [128, 4, D], BF16)
            XBK = [sb3(f"XBK{i}", [128, D], BF16) for i in range(2)]
            XBT = [sb3(f"XBT{i}", [128, 8, 128], BF16) for i in range(2)]
            HS = sb3("HS", [128, W])
            HT = sb3("HT", [128, 4, 128], BF16)
            YB = [sb3(f"YB{i}", [128, D]) for i in range(2)]
            weg_v = weg_d.rearrange("e (p k) n -> (e p) (k n)", k=8)
            weu_v = weu_d.rearrange("e (p k) n -> (e p) (k n)", k=8)
            wed_v = wed_d.rearrange("e (p k) n -> (e p) (k n)", k=4)
            bc_reg = nc.gpsimd.alloc_register("bc_reg")
            nc.gpsimd.reg_mov(bc_reg, NE * 128 - 1)
            def xload(j):
                p = j % 2
                S.dma('sp', lambda e: e.dma_start(out=XBK[p][:], in_=xbuf_d[j * 128:(j + 1) * 128, :]), reads=['xbuf'],
                      writes=[f'XBK{p}'])

            def transp(j):
                p = j % 2
                for k in range(8):
                    S.op('pe', lambda e: e.transpose(out=PT[:, k * 128:(k + 1) * 128], in_=XBK[p][:, k:D:8],
                                                     identity=IDB[:]), reads=[f'XBK{p}', 'IDB'], writes=['PT'])
                S.op('dve', lambda e: e.tensor_copy(out=XBT[p][:].rearrange("p k t -> p (k t)"), in_=PT[:, :]),
                     reads=['PT'], writes=[f'XBT{p}'])

            xload(0)
            transp(0)
            for j in range(NBLK):
                p = j % 2
                for (wt, wv, wn) in ((WG, weg_v, 'WG'), (WU, weu_v, 'WU'), (WD, wed_v, 'WD')):
                    S.dma('pool', lambda e: e.indirect_dma_start(out=wt[:].rearrange("p k n -> p (k n)"), out_offset=None, in_=wv,
                                                                 in_offset=bass.IndirectOffsetOnAxis(IDXW[:, j:j + 1], 0),
                                                                 bounds_check=bc_reg, oob_is_err=False),
                          reads=['IDXW'], writes=[wn])
                if j + 1 < NBLK:
                    xload(j + 1)
                for f in range(4):
                    for k in range(8):
                        S.op('pe', lambda e: e.matmul(PA[:, f * 128:(f + 1) * 128], lhsT=WG[:, k, f:W:4],
                                                      rhs=XBT[p][:, k, :], start=(k == 0), stop=(k == 7)),
                             reads=['WG', f'XBT{p}'], writes=['PA'])
                for f in range(4):
                    for k in range(8):
                        S.op('pe', lambda e: e.matmul(PB[:, f * 128:(f + 1) * 128], lhsT=WU[:, k, f:W:4],
                                                      rhs=XBT[p][:, k, :], start=(k == 0), stop=(k == 7)),
                             reads=['WU', f'XBT{p}'], writes=['PB'])
                S.op('act', lambda e: e.activation(out=HS[:], in_=PA[:, :], func=AF.Silu), reads=['PA'], writes=['HS'])
                S.op('dve', lambda e: e.tensor_tensor(out=HT[:].rearrange("p f t -> p (f t)"), in0=HS[:], in1=PB[:, :],
                                                      op=ALU.mult), reads=['HS', 'PB'], writes=['HT'])
                if j + 1 < NBLK:
                    transp(j + 1)
                for hf in range(2):
                    for f in range(4):
                        S.op('pe', lambda e: e.matmul(PS[:, hf * 512:(hf + 1) * 512], lhsT=HT[:, f, :],
                                                      rhs=WD[:, f, hf * 512:(hf + 1) * 512], start=(f == 0), stop=(f == 3)),
                             reads=['HT', 'WD'], writes=['PS'])
                S.op('act', lambda e: e.activation(out=YB[p][:], in_=PS[:, :], func=AF.Identity), reads=['PS'],
                     writes=[f'YB{p}'])
                S.dma('sp', lambda e: e.dma_start(out=ybuf_d[j * 128:(j + 1) * 128, :], in_=YB[p][:]), reads=[f'YB{p}'],
                      writes=['ybuf'])
            S.barrier()

        with ExitStack() as es4:
            def sb4(name, shape, dt=F32):
                return es4.enter_context(nc.sbuf_tensor(name, shape, dt))
            FGB = sb4("FGB", [128, D])
            G2B = sb4("G2B", [128, D])
            Y1 = [sb4(f"Y1{i}", [128, D]) for i in range(2)]
            Y2 = [sb4(f"Y2{i}", [128, D]) for i in range(2)]
            XR = [sb4(f"XR{i}", [128, D]) for i in range(2)]
            MO = [sb4(f"MO{i}", [128, D]) for i in range(2)]
            OT = [sb4(f"OT{i}", [128, D]) for i in range(2)]
            JK = sb4("JK", [128, D], BF16)
            SFa = sb4("SF", [128, 4])
            bcast_row(FGB, D, fng_d, 'FGB')
            for b in range(NB):
                S.dma('sp', lambda e: e.dma_start(out=G2B[:], in_=modbc_d[b, 3]), reads=[f'modbc{b}.3'], writes=['G2B'])
                for i in range(T):
                    t = b * T + i
                    p = t % 2
                    SF = SFa[:, 2 * p:2 * p + 2]
                    S.dma('pool', lambda e: e.indirect_dma_start(out=Y1[p][:], out_offset=None, in_=ybuf_d,
                                                                 in_offset=bass.IndirectOffsetOnAxis(IDX[:, t, 0:1], 0)),
                          reads=['ybuf', 'IDX'], writes=[f'Y1{p}'])
                    S.dma('pool', lambda e: e.indirect_dma_start(out=Y2[p][:], out_offset=None, in_=ybuf_d,
                                                                 in_offset=bass.IndirectOffsetOnAxis(IDX[:, t, 1:2], 0)),
                          reads=['ybuf', 'IDX'], writes=[f'Y2{p}'])
                    if t == 0:
                        S.dma('sp', lambda e: e.dma_start(out=XR[0][:], in_=x1_d[0:128, :]), reads=['x1d0'], writes=['XR0'])
                    if t + 1 < NT:
                        S.dma('sp', lambda e: e.dma_start(out=XR[1 - p][:], in_=x1_d[(t + 1) * 128:(t + 2) * 128, :]),
                              reads=[f'x1d{t + 1}'], writes=[f'XR{1 - p}'])
                    S.op('act', lambda e: e.activation(out=MO[p][:], in_=Y1[p][:], func=AF.Identity, scale=WTS[:, t, 0:1]),
                         reads=[f'Y1{p}', 'WTS'], writes=[f'MO{p}'])
                    S.op('dve', lambda e: e.scalar_tensor_tensor(out=MO[p][:], in0=Y2[p][:], scalar=WTS[:, t, 1:2], in1=MO[p][:],
                                                                 op0=ALU.mult, op1=ALU.add), reads=[f'Y2{p}', 'WTS', f'MO{p}'],
                         writes=[f'MO{p}'])
                    S.op('dve', lambda e: e.tensor_tensor(out=MO[p][:], in0=MO[p][:], in1=G2B[:], op=ALU.mult),
                         reads=[f'MO{p}', 'G2B'], writes=[f'MO{p}'])
                    S.op('dve', lambda e: e.tensor_tensor(out=MO[p][:], in0=MO[p][:], in1=XR[p][:], op=ALU.add),
                         reads=[f'MO{p}', f'XR{p}'], writes=[f'MO{p}'])
                    S.op('act', lambda e: e.activation(out=JK[:], in_=MO[p][:], func=AF.Square, accum_out=SF[:, 0:1]),
                         reads=[f'MO{p}'], writes=['JK', f'SF{p}'])
                    rsqrt_col(SF[:, 1:2], SF[:, 0:1], 1, 1.0 / D, name_d=f'SF1{p}', name_s=f'SF{p}')
                    S.op('dve', lambda e: e.scalar_tensor_tensor(out=OT[p][:], in0=MO[p][:], scalar=SF[:, 1:2], in1=FGB[:],
                                                                 op0=ALU.mult, op1=ALU.mult), reads=[f'MO{p}', f'SF1{p}', 'FGB'],
                         writes=[f'OT{p}'])
                    S.dma('sp', lambda e: e.dma_start(out=out_d[b, i * 128:(i + 1) * 128, :], in_=OT[p][:]),
                          reads=[f'OT{p}'], writes=['out'])
            S.barrier()
    except _Stop:
        pass
    return nc


def host_inputs(inputs, NB=2, cores=N_CORES):
    f = lambda a: np.ascontiguousarray(np.asarray(a, dtype=np.float32))
    x, c, ctx = f(inputs['x']), f(inputs['c']), f(inputs['ctx'])
    c_ctx = f(inputs['c_ctx'])
    sv = np.zeros((128, NSV), np.float32)
    sv[:, SV_DWB:SV_DWB + 4] = f(inputs['dw_bias'])[0].reshape(4, 128).T
    sv[:, SV_LNG:SV_LNG + 4] = f(inputs['conv_ln_g'])[0].reshape(4, 128).T
    sv[:, SV_LNB:SV_LNB + 4] = f(inputs['conv_ln_b'])[0].reshape(4, 128).T
    dwk = f(inputs['dw_kernel'])[0]
    sv[:, SV_DWK:SV_DWK + 124] = dwk.reshape(31, 4, 128).transpose(2, 1, 0).reshape(128, 124)
    shared = {
        'w_ada': f(inputs['w_ada'])[0], 'b_ada': f(inputs['b_ada'])[0][None, :],
        'w_in': f(inputs['w_in'])[0], 'w_out': f(inputs['w_out'])[0],
        'n1g': f(inputs['norm1_g'])[0][None, :], 'n2g': f(inputs['norm2_g'])[0][None, :],
        'fng': f(inputs['final_norm_g'])[None, :],
        'lbl': f(inputs['lb_logits'])[:, :2, :].reshape(1, 4 * W),
        'gn4': np.tile(f(inputs['hgrn_norm_g'])[0], 4)[None, :],
        'smallv': sv,
        'wr': np.ascontiguousarray(np.concatenate([f(inputs['router_group_w'])[0], f(inputs['router_expert_w'])[0]], axis=1)),
        'rb': np.concatenate([f(inputs['router_group_b'])[0], f(inputs['router_expert_b'])[0]])[None, :],
        'weg': f(inputs['w_expert_gate'])[0], 'weu': f(inputs['w_expert_up'])[0], 'wed': f(inputs['w_expert_down'])[0],
        'consts': CONSTS,
    }
    maps = []
    for k in range(cores):
        cT = np.zeros((128, 8, 3), np.float32)
        for b in range(NB):
            cT[:, :, b] = c[k * NB + b].reshape(8, 128).T
        cT[:, :, 2] = c_ctx.reshape(8, 128).T
        m = dict(shared)
        m['x'] = np.ascontiguousarray(x[k * NB:(k + 1) * NB])
        m['ctx'] = np.ascontiguousarray(ctx[k * NB:(k + 1) * NB])
        m['cT'] = cT
        maps.append(m)
    return maps


def kernel(**inputs):
    nc = build()
    maps = host_inputs(inputs)
    res = run_bass_kernel_spmd(nc, maps, core_ids=list(range(N_CORES)))
    return np.concatenate([np.asarray(r['out'], dtype=np.float32) for r in res.results], axis=0)
```
